# Optimizing a Trainium2 kernel written in Bass

```python
import math
import jax, jax.numpy as jnp
from jax import lax
import numpy as np

D_MODEL = 2048
BATCH = 1
SEQ = 16384
DEPTH = 1

CHUNK = 64
Q_BLOCK = 128
N_MEM = 256

SB_HEADS = 8
SB_HEAD_DIM = 128
DSA_HEADS = 8
DSA_HEAD_DIM = 128
IDX_HEADS = 8
IDX_HEAD_DIM = 64
DSA_TOPK_MAX = 256
MEM_HEADS = 4
MEM_HEAD_DIM = 256

N_BRANCH = 3
SB_W = SB_HEADS * SB_HEAD_DIM
DSA_W = DSA_HEADS * DSA_HEAD_DIM
MEM_W = MEM_HEADS * MEM_HEAD_DIM
BRANCH_WIDTH = 1024
IDX_Q_W = IDX_HEADS * IDX_HEAD_DIM
GATE_W = N_BRANCH * D_MODEL
D_IN = 3 * SB_W + 3 * DSA_W + MEM_W + IDX_Q_W + IDX_HEAD_DIM + IDX_HEADS + GATE_W

D_FF = 5504
CONV_WIDTH = 3
ROPE_THETA = 10000.0
NORM_EPS = 1e-6

kernel_name = "hybrid_sb_dsa_mem_convffn_block"


def _rmsnorm(x, g):
    x32 = x.astype(jnp.float32)
    y = x32 * lax.rsqrt(jnp.mean(x32 * x32, axis=-1, keepdims=True) + NORM_EPS)
    return (y * g.astype(jnp.float32)).astype(x.dtype)


def _rope(x, positions):
    half = x.shape[-1] // 2
    inv = ROPE_THETA ** (-jnp.arange(half, dtype=jnp.float32) / half)
    ang = positions.astype(jnp.float32)[..., None] * inv
    cos = jnp.cos(ang)[:, :, None, :]
    sin = jnp.sin(ang)[:, :, None, :]
    x1 = x[..., :half].astype(jnp.float32)
    x2 = x[..., half:].astype(jnp.float32)
    out = jnp.concatenate([x1 * cos - x2 * sin, x2 * cos + x1 * sin], axis=-1)
    return out.astype(x.dtype)


def _to_blocks(a):
    B, S = a.shape[:2]
    return a.reshape((B, S // Q_BLOCK, Q_BLOCK) + a.shape[2:]).swapaxes(0, 1)


def _from_blocks(a):
    nb, B, qb, H, Dh = a.shape
    return a.swapaxes(0, 1).reshape(B, nb * qb, H * Dh)


def _stick_breaking(q, k, v):
    B, S, H, Dh = q.shape
    nb = S // Q_BLOCK
    starts = jnp.arange(nb, dtype=jnp.int32) * Q_BLOCK
    key_pos = jnp.arange(S, dtype=jnp.int32)
    scale = Dh ** -0.5

    def block(args):
        qb, t0 = args
        qpos = t0 + jnp.arange(Q_BLOCK, dtype=jnp.int32)
        visible = key_pos[None, :] < qpos[:, None]
        z = jnp.einsum('bqhd,bkhd->bhqk', qb, k).astype(jnp.float32) * scale
        sp = jnp.where(visible, jax.nn.softplus(z), 0.0)
        tail = lax.cumsum(sp, axis=3, reverse=True) - sp
        a = jnp.where(visible, jnp.exp(jax.nn.log_sigmoid(z) - tail), 0.0)
        return jnp.einsum('bhqk,bkhd->bqhd', a.astype(v.dtype), v)

    out = lax.map(block, (_to_blocks(q), starts))
    return _from_blocks(out)


def _dsa(q, k, v, q_idx, k_idx, w_idx, topk):
    B, S, H, Dh = q.shape
    nb = S // Q_BLOCK
    starts = jnp.arange(nb, dtype=jnp.int32) * Q_BLOCK
    key_chunk = jnp.arange(S, dtype=jnp.int32) // CHUNK
    idx_scale = IDX_HEAD_DIM ** -0.5
    scale = Dh ** -0.5

    def block(args):
        qb, qib, wib, t0 = args
        qchunk = (t0 + jnp.arange(Q_BLOCK, dtype=jnp.int32)) // CHUNK
        admissible = key_chunk[None, :] <= qchunk[:, None]
        logits = jnp.einsum('bqhd,bkd->bqhk', qib, k_idx).astype(jnp.float32) * idx_scale
        score = jnp.einsum('bqhk,bqh->bqk', jax.nn.relu(logits), wib.astype(jnp.float32))
        score = jnp.where(admissible[None], score, -jnp.inf)
        _, sel = lax.top_k(score, topk)
        valid = (sel // CHUNK) <= qchunk[None, :, None]
        k_sel = jax.vmap(lambda kk, ii: kk[ii])(k, sel)
        v_sel = jax.vmap(lambda vv, ii: vv[ii])(v, sel)
        s = jnp.einsum('bqhd,bqkhd->bhqk', qb, k_sel).astype(jnp.float32) * scale
        s = jnp.where(valid[:, None], s, -jnp.inf)
        p = jax.nn.softmax(s, axis=-1).astype(v.dtype)
        return jnp.einsum('bhqk,bqkhd->bqhd', p, v_sel)

    out = lax.map(block, (_to_blocks(q), _to_blocks(q_idx), _to_blocks(w_idx), starts))
    return _from_blocks(out)


def _mem_attn(q, mk, mv):
    B, S, H, Dh = q.shape
    s = jnp.einsum('bqhd,bmhd->bhqm', q, mk).astype(jnp.float32) * (Dh ** -0.5)
    p = jax.nn.softmax(s, axis=-1).astype(mv.dtype)
    return jnp.einsum('bhqm,bmhd->bqhd', p, mv).reshape(B, S, H * Dh)


def _causal_dwconv(u, w, b):
    S = u.shape[1]
    up = jnp.pad(u, ((0, 0), (CONV_WIDTH - 1, 0), (0, 0)))
    out = b
    for j in range(CONV_WIDTH):
        out = out + up[:, j:j + S] * w[j]
    return out


def _layer(x, mem, positions, attn_norm, mem_norm, w_in, b_gate, dsa_q_norm, dsa_k_norm,
           mem_q_norm, mem_k_norm, w_mem_kv, w_branch, w_out, ffn_norm, w_up, conv_w,
           conv_b, w_down, topk):
    B, S, D = x.shape
    h = _rmsnorm(x, attn_norm)
    proj = h @ w_in
    sizes = [SB_W, SB_W, SB_W, DSA_W, DSA_W, DSA_W, MEM_W, IDX_Q_W, IDX_HEAD_DIM,
             IDX_HEADS, GATE_W]
    cuts = [int(c) for c in np.cumsum(sizes)[:-1]]
    qa, ka, va, qb, kb, vb, qm, qi, ki, wi, gl = jnp.split(proj, cuts, axis=-1)

    sb_shape = (B, S, SB_HEADS, SB_HEAD_DIM)
    o_sb = _stick_breaking(qa.reshape(sb_shape), ka.reshape(sb_shape), va.reshape(sb_shape))

    dsa_shape = (B, S, DSA_HEADS, DSA_HEAD_DIM)
    qb = _rope(_rmsnorm(qb.reshape(dsa_shape), dsa_q_norm), positions)
    kb = _rope(_rmsnorm(kb.reshape(dsa_shape), dsa_k_norm), positions)
    vb = vb.reshape(dsa_shape)
    qi = _rope(qi.reshape(B, S, IDX_HEADS, IDX_HEAD_DIM), positions)
    ki = _rope(ki[:, :, None, :], positions)[:, :, 0, :]
    wi = wi * (IDX_HEADS ** -0.5)
    o_dsa = _dsa(qb, kb, vb, qi, ki, wi, topk)

    mh = _rmsnorm(mem, mem_norm)
    mk, mv = jnp.split(mh @ w_mem_kv, 2, axis=-1)
    mem_shape = (B, mem.shape[1], MEM_HEADS, MEM_HEAD_DIM)
    mk = _rmsnorm(mk.reshape(mem_shape), mem_k_norm)
    mv = mv.reshape(mem_shape)
    qm = _rmsnorm(qm.reshape(B, S, MEM_HEADS, MEM_HEAD_DIM), mem_q_norm)
    o_mem = _mem_attn(qm, mk, mv)

    gates = jax.nn.sigmoid((gl + b_gate).astype(jnp.float32)).astype(x.dtype)
    gates = gates.reshape(B, S, N_BRANCH, D)
    merged = (gates[:, :, 0] * (o_sb @ w_branch[0])
              + gates[:, :, 1] * (o_dsa @ w_branch[1])
              + gates[:, :, 2] * (o_mem @ w_branch[2]))
    x = x + merged @ w_out

    h2 = _rmsnorm(x, ffn_norm)
    u = _causal_dwconv(h2 @ w_up, conv_w, conv_b)
    g, val = jnp.split(u, 2, axis=-1)
    return x + (jax.nn.silu(g) * val) @ w_down


def setup_inputs(seed: int = 0) -> dict:
    key = jax.random.key(seed)
    ks = jax.random.split(key, 20)
    f32 = jnp.float32
    nrm = lambda k, shape, s: jax.random.normal(k, shape, f32) * s
    gain = lambda k, shape: 1.0 + 0.02 * jax.random.normal(k, shape, f32)
    offset = jax.random.randint(ks[2], (BATCH, 1), 0, 64, dtype=jnp.int32) * CHUNK
    positions = offset + jnp.arange(SEQ, dtype=jnp.int32)[None, :]
    return {
        "x": nrm(ks[0], (BATCH, SEQ, D_MODEL), 1.0),
        "mem": nrm(ks[1], (BATCH, N_MEM, D_MODEL), 1.0),
        "positions": positions,
        "attn_norm": gain(ks[3], (DEPTH, D_MODEL)),
        "mem_norm": gain(ks[4], (DEPTH, D_MODEL)),
        "w_in": nrm(ks[5], (DEPTH, D_MODEL, D_IN), D_MODEL ** -0.5),
        "b_gate": nrm(ks[6], (DEPTH, GATE_W), 0.02),
        "dsa_q_norm": gain(ks[7], (DEPTH, DSA_HEAD_DIM)),
        "dsa_k_norm": gain(ks[8], (DEPTH, DSA_HEAD_DIM)),
        "mem_q_norm": gain(ks[9], (DEPTH, MEM_HEAD_DIM)),
        "mem_k_norm": gain(ks[10], (DEPTH, MEM_HEAD_DIM)),
        "w_mem_kv": nrm(ks[11], (DEPTH, D_MODEL, 2 * MEM_W), D_MODEL ** -0.5),
        "w_branch": nrm(ks[12], (DEPTH, N_BRANCH, BRANCH_WIDTH, D_MODEL), BRANCH_WIDTH ** -0.5),
        "w_out": nrm(ks[13], (DEPTH, D_MODEL, D_MODEL), D_MODEL ** -0.5),
        "ffn_norm": gain(ks[14], (DEPTH, D_MODEL)),
        "w_up": nrm(ks[15], (DEPTH, D_MODEL, 2 * D_FF), D_MODEL ** -0.5),
        "conv_w": nrm(ks[16], (DEPTH, CONV_WIDTH, 2 * D_FF), CONV_WIDTH ** -0.5),
        "conv_b": nrm(ks[17], (DEPTH, 2 * D_FF), 0.02),
        "w_down": nrm(ks[18], (DEPTH, D_FF, D_MODEL), D_FF ** -0.5),
    }


def reference(x, mem, positions, attn_norm, mem_norm, w_in, b_gate, dsa_q_norm, dsa_k_norm,
              mem_q_norm, mem_k_norm, w_mem_kv, w_branch, w_out, ffn_norm, w_up, conv_w,
              conv_b, w_down):
    topk = min(DSA_TOPK_MAX, x.shape[1] // 4)
    for l in range(DEPTH):
        x = _layer(x, mem, positions, attn_norm[l], mem_norm[l], w_in[l], b_gate[l],
                   dsa_q_norm[l], dsa_k_norm[l], mem_q_norm[l], mem_k_norm[l],
                   w_mem_kv[l], w_branch[l], w_out[l], ffn_norm[l], w_up[l], conv_w[l],
                   conv_b[l], w_down[l], topk)
    return x
```

```python
import contextlib
import numpy as np
import concourse.bass as bass
import concourse.mybir as mybir
from concourse.bass_utils import run_bass_kernel_spmd

F32 = mybir.dt.float32
BF16 = mybir.dt.bfloat16
I32 = mybir.dt.int32
AF = mybir.ActivationFunctionType
ALU = mybir.AluOpType
AX = mybir.AxisListType

D = 2048
SEQ = 16384
D_IN = 13896
D_FF = 5504
EPS = 1e-6
NCORE = 8
SEG = 1024
QW = 128 + SEG
NQ = 2 * QW

ENGS = ["pe", "act", "dve", "pool", "sp"]
EPOCH = 30000
ATTACH_WAITS = True
SAME_ENGINE_INORDER = ("pe",)


class Buf:
    __slots__ = ("name", "writes", "reads", "dsem", "ndma", "fresh", "qs", "swq", "grp")

    def __init__(self, name):
        self.name = name
        self.writes = {}
        self.reads = {}
        self.dsem = None
        self.ndma = 0
        self.fresh = False
        self.qs = set()
        self.swq = False
        self.grp = None


class TT:
    def __init__(self, h, name):
        self.h = h
        self.b = Buf(name)

    def __getitem__(self, k):
        return self.h[k]


class Sched:
    def __init__(self, nc, sems):
        self.nc = nc
        self.free_sems = list(sems)
        self.stream = {e: [] for e in ENGS}
        self.cnt = {e: 0 for e in ENGS}
        self.esems = {e: [] for e in ENGS}
        self.waited = {e: {} for e in ENGS}
        self.dtok = {}
        self.recycled = []
        self.dead_keys = set()
        self.grp_cur = {}
        self.snap = {e: [] for e in ENGS}
        self.dsnap = {}
        self.deferred = None
        self.cgrp = Buf("constgroup")
        self.free_sw = []
        self.free_hw = []
        self.sem_inc = {}
        self.barbufs = [Buf("bar_" + e) for e in ENGS]
        self.nops = 0

    def _sem_of(self, key):
        if key[0] == "e":
            lst = self.esems[key[1]]
            while len(lst) <= key[2]:
                lst.append(self.free_sems.pop())
            return lst[key[2]]
        return key[1]

    def _need(self, eng, deps):
        w = self.waited[eng]
        for key, val in sorted(deps.items(), key=lambda kv: -kv[1]):
            if w.get(key, 0) >= val:
                continue
            w[key] = val
            self.stream[eng].append(("wait", self._sem_of(key), val))
            if key[0] == "e":
                sn = self.snap[key[1]][key[2] * EPOCH + val - 1]
            else:
                sn = self.dsnap.get((key, val))
            if sn:
                for k2, v2 in sn.items():
                    if w.get(k2, 0) < v2:
                        w[k2] = v2

    def _collect(self, reads, writes, eng):
        deps = {}
        for b in reads:
            for k, v in b.writes.items():
                if eng == "pe" and k[0] == "e" and k[1] == "pe":
                    continue
                if deps.get(k, 0) < v:
                    deps[k] = v
        for b in writes:
            for src in (b.writes, b.reads):
                for k, v in src.items():
                    if eng in SAME_ENGINE_INORDER and k[0] == "e" and k[1] == eng:
                        continue
                    if deps.get(k, 0) < v:
                        deps[k] = v
        for k in deps:
            if k in self.grp_cur:
                deps[k] = self.grp_cur[k]
        return deps

    deferred_started = False

    def begin_const(self):
        self.deferred = []
        self.deferred_started = False

    def end_const(self):
        d, self.deferred = self.deferred, None
        for a in d:
            self.op(*a)

    def op(self, eng, fn, reads=(), writes=()):
        if self.deferred is not None:
            self.deferred.append((eng, fn, reads, writes))
            return
        reads = [r.b if isinstance(r, TT) else r for r in reads]
        writes = [r.b if isinstance(r, TT) else r for r in writes]
        self._need(eng, self._collect(reads, writes, eng))
        self.cnt[eng] += 1
        c = self.cnt[eng]
        key = ("e", eng, (c - 1) // EPOCH)
        val = (c - 1) % EPOCH + 1
        self.stream[eng].append(("op", fn, self._sem_of(key)))
        self.snap[eng].append(dict(self.waited[eng]))
        self.nops += 1
        for b in reads:
            if b.reads.get(key, 0) < val:
                b.reads[key] = val
        for b in writes:
            b.writes = {key: val}
            b.reads = {}

    def dma(self, q, out_ap, in_ap, sb, reads=(), writes=()):
        reads = [r.b if isinstance(r, TT) else r for r in reads]
        writes = [r.b if isinstance(r, TT) else r for r in writes]
        sb = sb.b if isinstance(sb, TT) else sb
        self._need(q, self._collect(reads, writes, None))
        own = sb.grp if sb.grp is not None else sb
        if own.dsem is None:
            own.dsem = self.free_sems.pop()
        if sb.grp is not None:
            assert self.deferred is not None, "constant loads must be inside begin_const/end_const"
            if own.ndma:
                self._need(q, {("d", own.dsem, 0): 16 * own.ndma} if not self.deferred_started else {})
                self.deferred_started = True
        own.ndma += 1
        key = ("d", own.dsem, 0)
        val = 16 * own.ndma
        self.dtok[key] = val
        if sb.grp is not None:
            self.grp_cur[key] = val
        self.stream[q].append(("dma", out_ap, in_ap, own.dsem))
        self.dsnap[(key, val)] = dict(self.waited[q])
        self.nops += 1
        for b in reads:
            if b.reads.get(key, 0) < val:
                b.reads[key] = val
        for b in writes:
            keep = {k: v for k, v in b.writes.items() if k[0] == "d"}
            keep[key] = val
            b.writes = keep
            b.reads = {}

    def _all_tokens(self):
        deps = dict(self.dtok)
        for e in ENGS:
            c = self.cnt[e]
            if c:
                deps[("e", e, (c - 1) // EPOCH)] = (c - 1) % EPOCH + 1
        return deps

    def barrier(self):
        deps = self._all_tokens()
        for e in ENGS:
            self._need(e, deps)

    def recycle(self, b):
        return

    def emit(self):
        nc = self.nc
        names = {"pe": "tensor", "act": "scalar", "dve": "vector", "pool": "gpsimd", "sp": "sync"}
        with nc.Block() as block:
            def run(name):
                def body(e):
                    pend = None
                    for it in self.stream[name]:
                        if it[0] == "wait":
                            if pend is not None:
                                e.wait_ge(pend[0], pend[1])
                            pend = (it[1], it[2])
                            continue
                        if pend is not None and not ATTACH_WAITS:
                            e.wait_ge(pend[0], pend[1])
                            pend = None
                        if it[0] == "op":
                            ins = it[1](e)
                        else:
                            ins = e.dma_start(out=it[1], in_=it[2])
                        if pend is not None:
                            ins._wait_ge(pend[0], pend[1])
                            pend = None
                        ins.then_inc(it[-1], 1 if it[0] == "op" else 16)
                    if pend is not None:
                        e.wait_ge(pend[0], pend[1])
                return body
            for name in ENGS:
                if self.stream[name]:
                    getattr(block, names[name])(run(name))
        self.stream = {e: [] for e in ENGS}


class Builder:
    def __init__(self, cfg):
        self.cfg = cfg
        self.nc = bass.Bass("TRN2", target_bir_lowering=False)
        self.rr = 0
        self.SEQ = cfg.get("seq", SEQ)

    def din(self, name, shape, dt=F32):
        return self.nc.dram_tensor(name, list(shape), dt, kind="ExternalInput").ap()

    def dscratch(self, name, shape, dt=BF16):
        kind = "ExternalOutput" if name in self.cfg.get("debug_out", ()) else "Internal"
        t = self.nc.dram_tensor(name, list(shape), dt, kind=kind).ap()
        return TT(t, name)

    def sb(self, es, name, shape, dt, const=False):
        self.uid = getattr(self, "uid", 0) + 1
        name = f"{name}_{self.uid}"
        t = TT(es.enter_context(self.nc.sbuf_tensor(name, list(shape), dt)), name)
        if const:
            t.b.grp = self.S.cgrp
        return t

    def next_ps(self):
        p = self.ps[self.rr % len(self.ps)]
        self.rr += 1
        return p

    def mm(self, out, lhsT, rhs, start, stop, R, W):
        self.S.op("pe", lambda e: e.matmul(out, lhsT, rhs, start=start, stop=stop), R, W)

    def tr(self, out, in_, ident, R, W):
        self.S.op("pe", lambda e: e.transpose(out, in_, ident), R, W)

    def act(self, out, in_, func, R, W, **kw):
        self.S.op("act", lambda e: e.activation(out=out, in_=in_, func=func, **kw), R, W)

    def tsc(self, eng, out, in0, s1, s2, op0, op1, R, W, **kw):
        if op1 is None:
            self.S.op(eng, lambda e: e.tensor_scalar(out=out, in0=in0, scalar1=s1, scalar2=None, op0=op0, **kw), R, W)
        else:
            self.S.op(eng, lambda e: e.tensor_scalar(out=out, in0=in0, scalar1=s1, scalar2=s2, op0=op0, op1=op1, **kw), R, W)

    def tt(self, eng, out, in0, in1, op, R, W):
        self.S.op(eng, lambda e: e.tensor_tensor(out=out, in0=in0, in1=in1, op=op), R, W)

    def stt(self, eng, out, in0, scalar, in1, op0, op1, R, W):
        self.S.op(eng, lambda e: e.scalar_tensor_tensor(out=out, in0=in0, scalar=scalar, in1=in1, op0=op0, op1=op1), R, W)

    def cp(self, eng, out, in_, R, W):
        if eng == "act":
            self.S.op("act", lambda e: e.copy(out=out, in_=in_), R, W)
        else:
            self.S.op(eng, lambda e: e.tensor_copy(out=out, in_=in_), R, W)

    def dma(self, q, out, in_, sbt, R, W):
        self.S.dma(q, out, in_, sbt, R, W)

    def evac(self, out, in_, R, W):
        self.ev = getattr(self, "ev", 0) + 1
        self.cp("act" if self.ev % 2 else "dve", out, in_, R, W)

    def build(self):
        nc, cfg = self.nc, self.cfg
        A = self
        A.x_all = A.din("x_all", [A.SEQ, D])
        A.pos_pt = A.din("pos_pt", [128, 128], I32)
        A.x_q = A.din("x_q", [NQ, D])
        A.posq_pt = A.din("posq_pt", [128, NQ // 128], I32)
        A.qidx_row = A.din("qidx_row", [1, NQ])
        A.qidx_pt = A.din("qidx_pt", [128, NQ // 128])
        A.kidx_pt = A.din("kidx_pt", [128, 128])
        A.kidx_row = A.din("kidx_row", [1, A.SEQ])
        A.flags = A.din("flags", [1, 2])
        A.bg_pt = A.din("bg_pt", [128, 48])
        A.cwb = A.din("cwb", [128, 86, 4])
        A.qlim_pt = A.din("qlim_pt", [128, NQ // 128])
        A.c_ident = A.din("c_ident", [128, 128])
        A.c_tri = A.din("c_tri", [128, 128])
        A.c_inv64 = A.din("c_inv64", [1, 64])
        A.mem = A.din("mem", [256, D])
        A.attn_norm = A.din("attn_norm", [1, D])
        A.mem_norm = A.din("mem_norm", [1, D])
        A.w_in = A.din("w_in", [D, D_IN])
        A.b_gate = A.din("b_gate", [1, 3 * D])
        A.dsa_q_norm = A.din("dsa_q_norm", [1, 128])
        A.dsa_k_norm = A.din("dsa_k_norm", [1, 128])
        A.mem_q_norm = A.din("mem_q_norm", [1, 256])
        A.mem_k_norm = A.din("mem_k_norm", [1, 256])
        A.w_mem_kv = A.din("w_mem_kv", [D, 2048])
        A.w_branch = A.din("w_branch", [3, 1024, D])
        A.w_out = A.din("w_out", [D, D])
        A.ffn_norm = A.din("ffn_norm", [1, D])
        A.w_up = A.din("w_up", [D, 2 * D_FF])
        A.conv_w = A.din("conv_w", [3, 2 * D_FF])
        A.conv_b = A.din("conv_b", [1, 2 * D_FF])
        A.w_down = A.din("w_down", [D_FF, D])
        A.out = nc.dram_tensor("out", [2 * SEG, D], F32, kind="ExternalOutput").ap()
        A.kaT = A.dscratch("kaT", [8, 128, A.SEQ])
        A.va = A.dscratch("va", [A.SEQ, 1024])
        A.kbT = A.dscratch("kbT", [8, 128, A.SEQ])
        A.vb = A.dscratch("vb", [A.SEQ, 1024])
        A.kiT = A.dscratch("kiT", [64, A.SEQ])

        with contextlib.ExitStack() as top:
            sems = [top.enter_context(nc.semaphore(f"s{i}")) for i in range(101)]
            A.S = Sched(nc, sems)
            A.ps = [TT(top.enter_context(nc.psum_tensor(f"ps{i}", [128, 512], F32)), f"ps{i}") for i in range(8)]
            A.S.begin_const()
            A.ident = A.sb(top, "ident", [128, 128], BF16)
            A.identf = A.sb(top, "identf", [128, 128], F32, const=True)
            A.dma("sp", A.identf[:], A.c_ident[:, :], A.identf, [], [A.identf])
            A.cp("dve", A.ident[:], A.identf[:], [A.identf], [A.ident])
            A.cst = A.sb(top, "cst", [128, 4], F32)
            for j, v in enumerate((-float(np.pi), D * EPS, 128 * EPS, 256 * EPS)):
                A.S.op("pool", lambda e, j=j, v=v: e.memset(A.cst[:, j:j + 1], float(v)), [], [A.cst])
            A.invt = A.sb(top, "invt", [128, 64], F32, const=True)
            A.dma("sp", A.invt[:], A.c_inv64.broadcast_to([128, 64]), A.invt, [], [A.invt])
            A.S.end_const()
            for ph in cfg["phases"]:
                getattr(A, "phase_" + ph)()
                A.S.barrier()
                A.S.emit()
        return nc

    def load_w_bf16(self, dst, dst_cols, w_ap, col0, ncols, stage):
        A = self
        wv = w_ap.rearrange("(c p) n -> p c n", p=128)
        nchunk = wv.shape[1]
        step = stage[0].h.shape[2]
        i = getattr(A, "_wst", 0)
        for j0 in range(0, ncols, step):
            n = min(step, ncols - j0)
            st = stage[i % len(stage)]
            i += 1
            for cc0 in range(0, nchunk, 16):
                cc1 = min(nchunk, cc0 + 16)
                A.dma("sp" if i % 2 else "act", st[:, cc0:cc1, 0:n], wv[:, cc0:cc1, col0 + j0:col0 + j0 + n], st, [], [st])
            A.cp("pool", dst[:, 0:nchunk, dst_cols + j0:dst_cols + j0 + n], st[:, 0:nchunk, 0:n], [st], [dst])
        A._wst = i

    def rope_tables(self, T, pos4, R, gains=None):
        A = self
        v = lambda t: t[:].rearrange("p (s j) -> p s j", s=4)
        t, ti, tf, fr, frc, c, sn, cs = (T[k] for k in ("t", "ti", "tf", "fr", "frc", "c", "sin", "cos"))
        A.tt("dve", v(t), A.invt[:].unsqueeze(1).broadcast_to([128, 4, 64]),
             pos4.unsqueeze(2).broadcast_to([128, 4, 64]), ALU.mult, list(R) + [A.invt], [t])
        A.cp("dve", ti[:], t[:], [t], [ti])
        A.cp("dve", tf[:], ti[:], [ti], [tf])
        A.tt("dve", fr[:], t[:], tf[:], ALU.subtract, [t, tf], [fr])
        A.tsc("dve", frc[:], fr[:], 0.25, None, ALU.add, None, [fr], [frc])
        A.tsc("dve", c[:], frc[:], 0.5, None, ALU.is_gt, None, [frc], [c])
        A.tt("dve", frc[:], frc[:], c[:], ALU.subtract, [frc, c], [frc])
        A.act(sn[:], fr[:], AF.Sin, [fr], [sn], scale=float(2 * np.pi))
        A.act(cs[:], frc[:], AF.Sin, [frc], [cs], scale=float(2 * np.pi))
        if gains is not None:
            gt, gap = gains
            g1 = gap[:, 0:64].unsqueeze(1).broadcast_to([128, 4, 64])
            g2 = gap[:, 64:128].unsqueeze(1).broadcast_to([128, 4, 64])
            A.tt("pool", v(T["A"]), v(cs), g1, ALU.mult, [cs, gt], [T["A"]])
            A.tt("pool", v(T["B"]), v(sn), g2, ALU.mult, [sn, gt], [T["B"]])
            A.tt("pool", v(T["C"]), v(cs), g2, ALU.mult, [cs, gt], [T["C"]])
            A.tt("pool", v(T["D"]), v(sn), g1, ALU.mult, [sn, gt], [T["D"]])

    def rope_apply(self, out, y, nh, hd, tabs, tabR, R, W, tmp):
        A = self
        half = hd // 2
        tA, tB, tC, tD = tabs
        t1, t2 = tmp
        bc = lambda t: t.unsqueeze(1).broadcast_to([128, nh, half])
        y1, y2 = y[:, :, 0:half], y[:, :, half:hd]
        v1 = t1[:, 0:nh * half].rearrange("p (h d) -> p h d", h=nh)
        v2 = t2[:, 0:nh * half].rearrange("p (h d) -> p h d", h=nh)
        tabR = list(tabR)
        A.tt("dve", v1, y1, bc(tA), ALU.mult, list(R) + tabR, [t1])
        A.tt("pool", v2, y2, bc(tB), ALU.mult, list(R) + tabR, [t2])
        A.tt("dve", out[:, :, 0:half], v1, v2, ALU.subtract, [t1, t2], list(W))
        A.tt("dve", v1, y2, bc(tC), ALU.mult, list(R) + tabR, [t1])
        A.tt("pool", v2, y1, bc(tD), ALU.mult, list(R) + tabR, [t2])
        A.tt("dve", out[:, :, half:hd], v1, v2, ALU.add, [t1, t2], list(W))

    def norm_tile(self, xt, gb, junk, ss, rs, h, ncols_scale, R_x):
        A = self
        A.S.op("pool", lambda e: e.memset(ss[:, 0:1], 0.0), [], [ss])
        A.act(junk[:], xt[:], AF.Square, [xt] + list(R_x), [junk, ss], accum_out=ss[:, 0:1])
        A.rsqrt(rs[:, 0:1], ss[:, 0:1], 1, rs, ss)
        A.stt("dve", h[:], xt[:], rs[:, 0:1], gb[:], ALU.mult, ALU.mult, [xt, rs, gb], [h])

    def rsqrt(self, out, in_, eps_col, out_t, in_t):
        A = self
        A.act(out, in_, AF.Sqrt, [in_t, A.cst], [out_t], bias=A.cst[:, eps_col:eps_col + 1])
        A.S.op("dve", lambda e: e.reciprocal(out=out, in_=out), [out_t], [out_t])

    def transpose_to(self, hT, col0, h, nchunks=16):
        A = self
        for c0 in range(0, nchunks, 8):
            p = A.next_ps()
            pv = p[:].bitcast(BF16).rearrange("p (a b) -> p a b", a=8)
            n = min(8, nchunks - c0)
            for c in range(n):
                A.tr(pv[:, c, :], h[:, (c0 + c) * 128:(c0 + c + 1) * 128], A.ident[:], [h, A.ident], [p])
            A.evac(hT[:, c0:c0 + n, col0:col0 + 128], pv[:, 0:n, :], [p], [hT])

    def phase_p1a(self):
        self.kv_pass("a")

    def phase_p1b(self):
        self.kv_pass("b")

    def kv_pass(self, which):
        A, nc, cfg = self, self.nc, self.cfg
        NT = cfg.get("nt1", A.SEQ // 512)
        isb = which == "b"
        ncols = 2112 if isb else 2048
        with contextlib.ExitStack() as es:
            W = A.sb(es, "W1", [128, 16, ncols], BF16)
            A.S.begin_const()
            gb = A.sb(es, "gb", [128, D], F32, const=True)
            A.dma("sp", gb[:], A.attn_norm.broadcast_to([128, D]), gb, [], [gb])
            A.tsc("dve", gb[:], gb[:], float(np.sqrt(D)), None, ALU.mult, None, [gb], [gb])
            A.S.end_const()
            with contextlib.ExitStack() as es2:
                stage = [A.sb(es2, f"wst{i}", [128, 16, 256], F32) for i in range(2)]
                if isb:
                    A.load_w_bf16(W, 0, A.w_in, 4096, 1024, stage)
                    A.load_w_bf16(W, 1024, A.w_in, 5120, 1024, stage)
                    A.load_w_bf16(W, 2048, A.w_in, 7680, 64, stage)
                else:
                    A.load_w_bf16(W, 0, A.w_in, 1024, 1024, stage)
                    A.load_w_bf16(W, 1024, A.w_in, 2048, 1024, stage)
                A.S.barrier()
            xt = [A.sb(es, f"xt{i}", [128, D], F32) for i in range(2)]
            ss = [A.sb(es, f"ss{i}", [128, 1], F32) for i in range(2)]
            rs = [A.sb(es, f"rs{i}", [128, 1], F32) for i in range(2)]
            h = [A.sb(es, f"h{i}", [128, D], BF16) for i in range(2)]
            hT = [A.sb(es, f"hT{i}", [128, 16, 512], BF16) for i in range(2)]
            kst = [A.sb(es, f"kst{i}", [128, 8, 512], BF16) for i in range(2)]
            vst = [A.sb(es, f"vst{i}", [128, 4, 1024], BF16) for i in range(2)]
            if isb:
                A.S.begin_const()
                posi = A.sb(es, "posi", [128, 128], I32, const=True)
                posf = A.sb(es, "posf", [128, 128], F32)
                A.dma("sp", posi[:], A.pos_pt[:, :], posi, [], [posi])
                A.cp("dve", posf[:], posi[:], [posi], [posf])
                A.gains = A.sb(es, "gains", [128, 128], F32, const=True)
                A.dma("sp", A.gains[:], A.dsa_k_norm.broadcast_to([128, 128]), A.gains, [], [A.gains])
                A.tsc("dve", A.gains[:], A.gains[:], float(np.sqrt(128.0)), None, ALU.mult, None, [A.gains], [A.gains])
                A.S.end_const()
                RT = {k: A.sb(es, "rt_" + k, [128, 256], I32 if k == "ti" else F32)
                      for k in ("t", "ti", "tf", "fr", "frc", "c", "sin", "cos", "A", "B", "C", "D")}
                sq = A.sb(es, "sq", [128, 512], F32)
                ssq = A.sb(es, "ssq", [128, 8], F32)
                rs8 = A.sb(es, "rs8", [128, 8], F32)
                yraw = A.sb(es, "yraw", [128, 1024], F32)
                y = A.sb(es, "y", [128, 1024], F32)
                kbr = A.sb(es, "kbr", [128, 1024], BF16)
                t1 = A.sb(es, "t1", [128, 512], F32)
                t2 = A.sb(es, "t2", [128, 512], F32)
                kiraw = A.sb(es, "kiraw", [128, 64], F32)
                kir = A.sb(es, "kir", [128, 64], BF16)
                kist = [A.sb(es, f"kist{i}", [64, 512], BF16) for i in range(2)]
            kT_dst = (A.kbT if isb else A.kaT)
            v_dst = (A.vb if isb else A.va)
            kT_view = kT_dst.h.rearrange("h d t -> d h t")
            for ti in range(NT):
                tok0 = 512 * ti
                hTt = hT[ti % 2]
                for s in range(4):
                    g = 4 * ti + s
                    x_ = xt[g % 2]
                    A.dma("sp", x_[:], A.x_all[tok0 + 128 * s: tok0 + 128 * (s + 1), :], x_, [], [x_])
                    A.norm_tile(x_, gb, h[g % 2], ss[g % 2], rs[g % 2], h[g % 2], D, [])
                    A.transpose_to(hTt, s * 128, h[g % 2])
                ks = kst[ti % 2]
                vs = vst[ti % 2]
                if not isb:
                    for hh in range(8):
                        p = A.next_ps()
                        for c in range(16):
                            A.mm(p[:], W[:, c, hh * 128:(hh + 1) * 128], hTt[:, c, :], c == 0, c == 15, [W, hTt], [p])
                        A.evac(ks[:, hh, :], p[:], [p], [ks])
                else:
                    A.rope_tables(RT, posf[:, 4 * ti:4 * ti + 4], [posf], (A.gains, A.gains))
                    rv = lambda k, s: RT[k][:, s * 64:(s + 1) * 64]
                    for s in range(4):
                        g = 4 * ti + s
                        for cc in range(2):
                            p = A.next_ps()
                            for c in range(16):
                                A.mm(p[:], hTt[:, c, s * 128:(s + 1) * 128], W[:, c, cc * 512:(cc + 1) * 512], c == 0, c == 15, [W, hTt], [p])
                            A.act(sq[:], p[:], AF.Square, [p], [sq])
                            A.S.op("dve", lambda e, o=ssq[:, cc * 4:(cc + 1) * 4], i=sq[:].rearrange("p (h d) -> p h d", h=4):
                                   e.tensor_reduce(out=o, in_=i, axis=AX.X, op=ALU.add), [sq], [ssq])
                            A.cp("dve", yraw[:, cc * 512:(cc + 1) * 512], p[:], [p], [yraw])
                        A.rsqrt(rs8[:], ssq[:], 2, rs8, ssq)
                        y3 = y[:].rearrange("p (h d) -> p h d", h=8)
                        A.tt("dve", y3, yraw[:].rearrange("p (h d) -> p h d", h=8),
                             rs8[:].unsqueeze(2).broadcast_to([128, 8, 128]), ALU.mult, [yraw, rs8], [y])
                        kb3 = kbr[:].rearrange("p (h d) -> p h d", h=8)
                        A.rope_apply(kb3, y3, 8, 128, [rv(k, s) for k in "ABCD"], [RT[k] for k in "ABCD"], [y], [kbr], (t1, t2))
                        p = A.next_ps()
                        pv = p[:].bitcast(BF16).rearrange("p (a b) -> p a b", a=8)
                        for hh in range(8):
                            A.tr(pv[:, hh, :], kbr[:, hh * 128:(hh + 1) * 128], A.ident[:], [kbr, A.ident], [p])
                        A.evac(ks[:, :, s * 128:(s + 1) * 128], pv, [p], [ks])
                        c32 = RT["cos"][:, s * 64:(s + 1) * 64:2]
                        s32 = RT["sin"][:, s * 64:(s + 1) * 64:2]
                        p = A.next_ps()
                        for c in range(16):
                            A.mm(p[:, 0:64], hTt[:, c, s * 128:(s + 1) * 128], W[:, c, 2048:2112], c == 0, c == 15, [W, hTt], [p])
                        A.cp("act", kiraw[:], p[:, 0:64], [p], [kiraw])
                        A.rope_apply(kir[:].rearrange("p (h d) -> p h d", h=1), kiraw[:].rearrange("p (h d) -> p h d", h=1),
                                     1, 64, [c32, s32, c32, s32], [RT["cos"], RT["sin"]], [kiraw], [kir], (t1, t2))
                        p = A.next_ps()
                        pv = p[:].bitcast(BF16)
                        A.tr(pv[0:64, 0:128], kir[:], A.ident[:], [kir, A.ident], [p])
                        A.evac(kist[ti % 2][:, s * 128:(s + 1) * 128], pv[0:64, 0:128], [p], [kist[ti % 2]])
                    A.dma("pool", A.kiT.h[:, tok0:tok0 + 512], kist[ti % 2][:], kist[ti % 2], [kist[ti % 2]], [A.kiT])
                A.dma("pool", kT_view[:, :, tok0:tok0 + 512], ks[:], ks, [ks], [kT_dst])
                vc0 = 1024
                for s in range(4):
                    for cc in range(2):
                        p = A.next_ps()
                        for c in range(16):
                            A.mm(p[:], hTt[:, c, s * 128:(s + 1) * 128], W[:, c, vc0 + cc * 512: vc0 + (cc + 1) * 512], c == 0, c == 15, [W, hTt], [p])
                        A.evac(vs[:, s, cc * 512:(cc + 1) * 512], p[:], [p], [vs])
                A.dma("pool", v_dst.h[tok0:tok0 + 512, :].rearrange("(s p) n -> p s n", p=128), vs[:], vs, [vs], [v_dst])

    def phase_p2(self):
        A, nc, cfg = self, self.nc, self.cfg
        NB = NQ // 128
        A.hTq = A.dscratch("hTq", [128, 16, NQ])
        A.qaT = A.dscratch("qaT", [8, 128, NQ])
        A.qbT = A.dscratch("qbT", [8, 128, NQ])
        A.qmT = A.dscratch("qmT", [8, 128, NQ])
        A.qiT = A.dscratch("qiT", [8, 64, NQ])
        A.wi = A.dscratch("wi", [NQ, 8], F32)
        ttiles = [(t0, min(512, NQ - t0)) for t0 in range(0, NQ, 512)]
        with contextlib.ExitStack() as es:
            hTq = A.sb(es, "hTq", [128, 16, NQ], BF16)
            Wb = [A.sb(es, f"Wb{i}", [128, 16, 512], BF16) for i in range(2)]
            stage = [A.sb(es, f"wst{i}", [128, 16, 128], F32) for i in range(2)]
            TB = {k: A.sb(es, "qt_" + k, [128, NB * 64], F32) for k in ("sin", "cos", "A", "B", "C", "D")}
            A.S.begin_const()
            gq = A.sb(es, "gq", [128, 128], F32, const=True)
            gm = A.sb(es, "gm", [128, 256], F32, const=True)
            A.dma("sp", gq[:], A.dsa_q_norm.broadcast_to([128, 128]), gq, [], [gq])
            A.tsc("dve", gq[:], gq[:], float(np.sqrt(128.0)), None, ALU.mult, None, [gq], [gq])
            A.dma("sp", gm[:], A.mem_q_norm.broadcast_to([128, 256]), gm, [], [gm])
            A.tsc("dve", gm[:], gm[:], float(np.sqrt(256.0)), None, ALU.mult, None, [gm], [gm])
            with contextlib.ExitStack() as es2:
                posi = A.sb(es2, "posqi", [128, NB], I32, const=True)
                posf = A.sb(es2, "posqf", [128, NB], F32)
                A.dma("sp", posi[:], A.posq_pt[:, :], posi, [], [posi])
                A.cp("dve", posf[:], posi[:], [posi], [posf])
                gb = A.sb(es2, "gbq", [128, D], F32, const=True)
                A.dma("sp", gb[:], A.attn_norm.broadcast_to([128, D]), gb, [], [gb])
                A.tsc("dve", gb[:], gb[:], float(np.sqrt(D)), None, ALU.mult, None, [gb], [gb])
                A.S.end_const()
                RT = {k: A.sb(es2, "rq_" + k, [128, 256], I32 if k == "ti" else F32)
                      for k in ("t", "ti", "tf", "fr", "frc", "c", "sin", "cos", "A", "B", "C", "D")}
                for b0 in range(0, NB, 4):
                    n = min(4, NB - b0)
                    pos4 = posf[:, b0:b0 + 4] if n == 4 else posf[:, NB - 4:NB]
                    bb = b0 if n == 4 else NB - 4
                    A.rope_tables(RT, pos4, [posf], (gq, gq))
                    for k in TB:
                        A.cp("pool", TB[k][:, bb * 64:(bb + 4) * 64], RT[k][:], [RT[k]], [TB[k]])
                xt = [A.sb(es2, f"xq{i}", [128, D], F32) for i in range(2)]
                ss = [A.sb(es2, f"ssq{i}", [128, 1], F32) for i in range(2)]
                rs = [A.sb(es2, f"rsq{i}", [128, 1], F32) for i in range(2)]
                h = [A.sb(es2, f"hq{i}", [128, D], BF16) for i in range(2)]
                for b in range(NB):
                    x_ = xt[b % 2]
                    A.dma("sp", x_[:], A.x_q[128 * b:128 * (b + 1), :], x_, [], [x_])
                    A.norm_tile(x_, gb, h[b % 2], ss[b % 2], rs[b % 2], h[b % 2], D, [])
                    A.transpose_to(hTq, b * 128, h[b % 2])
                A.dma("pool", A.hTq.h[:, :, :], hTq[:], hTq, [hTq], [A.hTq])
                A.S.barrier()
            qst = A.sb(es, "qst", [128, 4, NQ], BF16)
            sq = A.sb(es, "sqq", [128, 512], F32)
            ssq = A.sb(es, "ssqq", [128, 4], F32)
            rs4 = A.sb(es, "rs4", [128, 4], F32)
            yraw = A.sb(es, "yrawq", [128, 512], F32)
            y = A.sb(es, "yq", [128, 512], F32)
            yb = A.sb(es, "ybq", [128, 512], BF16)
            t1 = A.sb(es, "t1q", [128, 256], F32)
            t2 = A.sb(es, "t2q", [128, 256], F32)
            qist = [A.sb(es, f"qist{i}", [64, 8, 128], BF16) for i in range(2)]
            wist = A.sb(es, "wist", [128, NB, 8], F32)
            wcount = [0]

            def load_block(col0, ncols):
                w = Wb[wcount[0] % 2]
                wcount[0] += 1
                A.load_w_bf16(w, 0, A.w_in, col0, ncols, stage)
                return w

            def tok_major(w, ncols, b):
                p = A.next_ps()
                for c in range(16):
                    A.mm(p[:, 0:ncols], hTq[:, c, b * 128:(b + 1) * 128], w[:, c, 0:ncols], c == 0, c == 15, [w, hTq], [p])
                return p

            for blk in range(2):
                w = load_block(blk * 512, 512)
                for hh in range(4):
                    for (t0, tw) in ttiles:
                        p = A.next_ps()
                        for c in range(16):
                            A.mm(p[:, 0:tw], w[:, c, hh * 128:(hh + 1) * 128], hTq[:, c, t0:t0 + tw], c == 0, c == 15, [w, hTq], [p])
                        A.evac(qst[:, hh, t0:t0 + tw], p[:, 0:tw], [p], [qst])
                A.dma("pool", A.qaT.h[4 * blk:4 * blk + 4].rearrange("h d t -> d h t"), qst[:], qst, [qst], [A.qaT])
            for blk in range(2):
                w = load_block(3072 + blk * 512, 512)
                for b in range(NB):
                    p = tok_major(w, 512, b)
                    A.act(sq[:], p[:], AF.Square, [p], [sq])
                    A.S.op("dve", lambda e, o=ssq[:], i=sq[:].rearrange("p (h d) -> p h d", h=4):
                           e.tensor_reduce(out=o, in_=i, axis=AX.X, op=ALU.add), [sq], [ssq])
                    A.cp("dve", yraw[:], p[:], [p], [yraw])
                    A.rsqrt(rs4[:], ssq[:], 2, rs4, ssq)
                    y3 = y[:].rearrange("p (h d) -> p h d", h=4)
                    A.tt("dve", y3, yraw[:].rearrange("p (h d) -> p h d", h=4),
                         rs4[:].unsqueeze(2).broadcast_to([128, 4, 128]), ALU.mult, [yraw, rs4], [y])
                    yb3 = yb[:].rearrange("p (h d) -> p h d", h=4)
                    tabs = [TB[k][:, b * 64:(b + 1) * 64] for k in "ABCD"]
                    A.rope_apply(yb3, y3, 4, 128, tabs, [TB[k] for k in "ABCD"], [y], [yb], (t1, t2))
                    p2 = A.next_ps()
                    pv = p2[:].bitcast(BF16).rearrange("p (a b) -> p a b", a=8)
                    for hh in range(4):
                        A.tr(pv[:, hh, :], yb[:, hh * 128:(hh + 1) * 128], A.ident[:], [yb, A.ident], [p2])
                    A.evac(qst[:, :, b * 128:(b + 1) * 128], pv[:, 0:4, :], [p2], [qst])
                A.dma("pool", A.qbT.h[4 * blk:4 * blk + 4].rearrange("h d t -> d h t"), qst[:], qst, [qst], [A.qbT])
            for blk in range(2):
                w = load_block(6144 + blk * 512, 512)
                for b in range(NB):
                    p = tok_major(w, 512, b)
                    A.act(sq[:], p[:], AF.Square, [p], [sq])
                    A.S.op("dve", lambda e, o=ssq[:, 0:2], i=sq[:].rearrange("p (h d) -> p h d", h=2):
                           e.tensor_reduce(out=o, in_=i, axis=AX.X, op=ALU.add), [sq], [ssq])
                    A.cp("dve", yraw[:], p[:], [p], [yraw])
                    A.rsqrt(rs4[:, 0:2], ssq[:, 0:2], 3, rs4, ssq)
                    y3 = y[:].rearrange("p (h d) -> p h d", h=2)
                    A.tt("dve", y3, yraw[:].rearrange("p (h d) -> p h d", h=2),
                         rs4[:, 0:2].unsqueeze(2).broadcast_to([128, 2, 256]), ALU.mult, [yraw, rs4], [y])
                    A.tt("pool", yb[:].rearrange("p (h d) -> p h d", h=2), y3,
                         gm[:].unsqueeze(1).broadcast_to([128, 2, 256]), ALU.mult, [y, gm], [yb])
                    p2 = A.next_ps()
                    pv = p2[:].bitcast(BF16).rearrange("p (a b) -> p a b", a=8)
                    for hh in range(4):
                        A.tr(pv[:, hh, :], yb[:, hh * 128:(hh + 1) * 128], A.ident[:], [yb, A.ident], [p2])
                    A.evac(qst[:, :, b * 128:(b + 1) * 128], pv[:, 0:4, :], [p2], [qst])
                A.dma("pool", A.qmT.h[4 * blk:4 * blk + 4].rearrange("h d t -> d h t"), qst[:], qst, [qst], [A.qmT])
            w = load_block(7168, 512)
            for b in range(NB):
                p = tok_major(w, 512, b)
                A.cp("act", yraw[:], p[:], [p], [yraw])
                c32 = TB["cos"][:, b * 64:(b + 1) * 64:2]
                s32 = TB["sin"][:, b * 64:(b + 1) * 64:2]
                A.rope_apply(yb[:].rearrange("p (h d) -> p h d", h=8), yraw[:].rearrange("p (h d) -> p h d", h=8),
                             8, 64, [c32, s32, c32, s32], [TB["cos"], TB["sin"]], [yraw], [yb], (t1, t2))
                p2 = A.next_ps()
                pv = p2[:].bitcast(BF16).rearrange("p (a b) -> p a b", a=8)
                for hh in range(8):
                    A.tr(pv[0:64, hh, :], yb[:, hh * 64:(hh + 1) * 64], A.ident[:], [yb, A.ident], [p2])
                qs = qist[b % 2]
                A.evac(qs[:], pv[0:64, :, :], [p2], [qs])
                A.dma("pool", A.qiT.h.rearrange("h d t -> d h t")[:, :, b * 128:(b + 1) * 128], qs[:], qs, [qs], [A.qiT])
            w = load_block(7744, 8)
            for b in range(NB):
                p = tok_major(w, 8, b)
                A.evac(wist[:, b, :], p[:, 0:8], [p], [wist])
            A.dma("pool", A.wi.h.rearrange("(b p) h -> p b h", p=128), wist[:], wist, [wist], [A.wi])

    def q_tiles(self):
        nkt = self.SEQ // 128
        out = []
        for slot, (segmax, segmin) in enumerate(((7, 0), (15, 8))):
            base = slot * QW
            for (off, w, toff) in ((0, 128, -128), (128, 512, 0), (640, 512, 512)):
                qmax = SEG * segmax + toff + w
                qmin = max(SEG * segmin + toff, 0)
                nk = min((qmax + 127) // 128, nkt)
                out.append((base + off, w, nk, min(qmin // 128, nk) if self.SEQ == SEQ else 0))
        return out

    def phase_p3(self):
        A, nc, cfg = self, self.nc, self.cfg
        A.osbT = A.dscratch("osbT", [8, 128, NQ])
        heads = cfg.get("heads", range(8))
        nkt = A.SEQ // 128
        SC = float(128 ** -0.5)
        with contextlib.ExitStack() as es:
            kT = [A.sb(es, f"kTa{i}", [128, A.SEQ], BF16) for i in range(2)]
            vv = [A.sb(es, f"vva{i}", [128, nkt, 128], BF16) for i in range(2)]
            qT = [A.sb(es, f"qTa{i}", [128, NQ], BF16) for i in range(2)]
            ost = [A.sb(es, f"osta{i}", [128, NQ], BF16) for i in range(2)]
            A.S.begin_const()
            qrow = A.sb(es, "qrow", [128, NQ], F32, const=True)
            kidx = A.sb(es, "kidx", [128, 128], F32, const=True)
            A.dma("sp", qrow[:], A.qidx_row.broadcast_to([128, NQ]), qrow, [], [qrow])
            A.dma("sp", kidx[:], A.kidx_pt[:, :], kidx, [], [kidx])
            trif = A.sb(es, "trif", [128, 128], F32, const=True)
            tri = A.sb(es, "tri", [128, 128], BF16)
            ones = A.sb(es, "ones", [128, 128], BF16)
            one1 = A.sb(es, "one1", [128, 1], F32)
            A.dma("sp", trif[:], A.c_tri[:, :], trif, [], [trif])
            A.cp("dve", tri[:], trif[:], [trif], [tri])
            A.S.op("pool", lambda e: e.memset(ones[:], 1.0), [], [ones])
            A.S.op("pool", lambda e: e.memset(one1[:], 1.0), [], [one1])
            A.S.end_const()
            eb = [A.sb(es, f"eb{i}", [128, 512], F32) for i in range(2)]
            spb = [A.sb(es, f"spb{i}", [128, 512], BF16) for i in range(2)]
            sps = [A.sb(es, f"sps{i}", [128, 512], BF16) for i in range(2)]
            tb = [A.sb(es, f"tb{i}", [128, 512], F32) for i in range(2)]
            ab = [A.sb(es, f"ab{i}", [128, 512], BF16) for i in range(2)]
            it = 0
            for hi, hh in enumerate(heads):
                k_, v_, q_, o_ = kT[hi % 2], vv[hi % 2], qT[hi % 2], ost[hi % 2]
                nsp = 4
                for j in range(nsp):
                    c0, c1 = A.SEQ * j // nsp, A.SEQ * (j + 1) // nsp
                    A.dma("sp" if j % 2 else "act", k_[:, c0:c1], A.kaT.h[hh, :, c0:c1], k_, [A.kaT], [k_])
                nsv = max(4, nkt // 8)
                for j in range(nsv):
                    t0, t1 = nkt * j // nsv, nkt * (j + 1) // nsv
                    A.dma("act" if j % 2 else "sp", v_[:, t0:t1, :],
                          A.va.h[t0 * 128:t1 * 128, hh * 128:(hh + 1) * 128].rearrange("(t p) d -> p t d", p=128),
                          v_, [A.va], [v_])
                A.dma("sp", q_[:], A.qaT.h[hh, :, :], q_, [A.qaT], [q_])
                for (q0, tw, nk, kmask) in A.q_tiles():
                    po = A.next_ps()
                    for i, kt in enumerate(range(nk - 1, -1, -1)):
                        first, last = i == 0, kt == 0
                        e_, sp_, t_, a_ = eb[it % 2], spb[it % 2], tb[it % 2], ab[it % 2]
                        ss_new, ss_old = sps[it % 2], sps[(it + 1) % 2]
                        it += 1
                        pz = A.next_ps()
                        if pz is po:
                            pz = A.next_ps()
                        A.mm(pz[:, 0:tw], k_[:, kt * 128:(kt + 1) * 128], q_[:, q0:q0 + tw], True, True, [k_, q_], [pz])
                        A.act(e_[:, 0:tw], pz[:, 0:tw], AF.Exp, [pz], [e_], scale=SC)
                        if kt >= kmask:
                            A.stt("dve", e_[:, 0:tw], qrow[:, q0:q0 + tw], kidx[:, kt:kt + 1], e_[:, 0:tw],
                                  ALU.is_gt, ALU.mult, [qrow, kidx, e_], [e_])
                        A.act(sp_[:, 0:tw], e_[:, 0:tw], AF.Ln, [e_, one1], [sp_], bias=one1[:, 0:1])
                        pc = A.next_ps()
                        if pc is po:
                            pc = A.next_ps()
                        A.mm(pc[:, 0:tw], tri[:], sp_[:, 0:tw], True, first, [tri, sp_], [pc])
                        if first:
                            A.cp("pool", ss_new[:, 0:tw], sp_[:, 0:tw], [sp_], [ss_new])
                        else:
                            A.mm(pc[:, 0:tw], ones[:], ss_old[:, 0:tw], False, True, [ones, ss_old], [pc])
                            if not last:
                                A.tt("pool", ss_new[:, 0:tw], ss_old[:, 0:tw], sp_[:, 0:tw], ALU.add, [ss_old, sp_], [ss_new])
                        A.act(t_[:, 0:tw], pc[:, 0:tw], AF.Exp, [pc], [t_], scale=-1.0)
                        A.tt("dve", a_[:, 0:tw], e_[:, 0:tw], t_[:, 0:tw], ALU.mult, [e_, t_], [a_])
                        A.mm(po[:, 0:tw], v_[:, kt, :], a_[:, 0:tw], first, last, [v_, a_], [po])
                    A.evac(o_[:, q0:q0 + tw], po[:, 0:tw], [po], [o_])
                A.dma("pool", A.osbT.h[hh, :, :], o_[:], o_, [o_], [A.osbT])

    def q_blocks(self):
        out = []
        for slot, segmax in enumerate((7, 15)):
            for j in range(-1, 8):
                qmax0 = SEG * segmax + 128 * j
                out.append((slot * 9 + j + 1, min(qmax0 + 128, self.SEQ)))
        return out

    def ps_free(self, excl):
        while True:
            p = self.next_ps()
            if all(p is not x for x in excl):
                return p

    def phase_p4(self):
        A, nc, cfg = self, self.nc, self.cfg
        A.odsaT = A.dscratch("odsaT", [8, 128, NQ])
        NB = NQ // 128
        blocks = [qb for qb in A.q_blocks() if qb[0] in cfg.get("qblocks", range(NB))]
        SCD = float(128 ** -0.5)
        NITER = cfg.get("niter", 27)
        with contextlib.ExitStack() as es:
            A.S.begin_const()
            kiT = A.sb(es, "kiT", [64, A.SEQ], BF16, const=True)
            A.dma("sp", kiT[:], A.kiT.h[:, :], kiT, [A.kiT], [kiT])
            Sb = A.sb(es, "Sb", [128, A.SEQ], F32)
            mask = A.sb(es, "maskb", [128, A.SEQ], BF16)
            iota = A.sb(es, "iota", [128, 512], F32, const=True)
            A.dma("sp", iota[:], A.kidx_row[:, 0:512].broadcast_to([128, 512]), iota, [], [iota])
            qlim = A.sb(es, "qlim", [128, NB], F32, const=True)
            A.dma("sp", qlim[:], A.qlim_pt[:, :], qlim, [], [qlim])
            ones = A.sb(es, "ones4", [128, 128], BF16)
            A.S.op("pool", lambda e: e.memset(ones[:], 1.0), [], [ones])
            zeros = A.sb(es, "zeros4", [128, 512], BF16)
            A.S.op("pool", lambda e: e.memset(zeros[:], 0.0), [], [zeros])
            wi = A.sb(es, "wi_sb", [128, NB, 8], F32, const=True)
            A.dma("sp", wi[:], A.wi.h.rearrange("(b p) h -> p b h", p=128), wi, [A.wi], [wi])
            A.S.end_const()
            qi = [A.sb(es, f"qi{i}", [64, 8, 128], BF16) for i in range(2)]
            qb_ = [A.sb(es, f"qbq{i}", [128, 8, 128], BF16) for i in range(2)]
            Dg = [A.sb(es, f"Dg{i}", [128, 8, 128], BF16) for i in range(2)]
            rl = [A.sb(es, f"rl{i}", [128, 512], BF16) for i in range(3)]
            pen = [A.sb(es, f"pen{i}", [128, 512], F32) for i in range(2)]
            am = A.sb(es, "am", [128, 32], F32)
            st = {k: A.sb(es, "bs_" + k, [128, 1], F32) for k in ("lo", "w", "mid", "cnt", "ge", "amx")}
            kc = [A.sb(es, f"kc{i}", [128, 8, 512], BF16) for i in range(2)]
            vc = [A.sb(es, f"vc{i}", [128, 4, 1024], BF16) for i in range(2)]
            pe_ = [A.sb(es, f"pexp{i}", [128, 512], BF16) for i in range(2)]
            pm = [A.sb(es, f"pm{i}", [128, 512], BF16) for i in range(2)]
            rinv = A.sb(es, "rinv", [128, 512], F32)
            ost = [A.sb(es, f"ostd{i}", [128, 8, 128], BF16) for i in range(2)]
            kT_view = A.kbT.h.rearrange("h d t -> d h t")
            qiT_view = A.qiT.h.rearrange("h d t -> d h t")
            qbT_view = A.qbT.h.rearrange("h d t -> d h t")
            cnt_i = 0
            for bi, (b, L) in enumerate(blocks):
                nch = (L + 511) // 512
                qi_, qq, dg = qi[bi % 2], qb_[bi % 2], Dg[bi % 2]
                A.dma("sp", qi_[:], qiT_view[:, :, b * 128:(b + 1) * 128], qi_, [A.qiT], [qi_])
                A.dma("sp", qq[:], qbT_view[:, :, b * 128:(b + 1) * 128], qq, [A.qbT], [qq])
                for h in range(8):
                    A.tsc("dve", dg[:, h, :], A.ident[:], wi[:, b, h:h + 1], None, ALU.mult, None, [A.ident, wi], [dg])
                for ci in range(nch):
                    c0 = ci * 512
                    cw = min(512, L - c0)
                    pS = A.next_ps()
                    for h in range(8):
                        pl = A.ps_free([pS])
                        A.mm(pl[:, 0:cw], qi_[:, h, :], kiT[:, c0:c0 + cw], True, True, [qi_, kiT], [pl])
                        r_ = rl[cnt_i % 3]
                        cnt_i += 1
                        if h % 2 == 0:
                            A.act(r_[:, 0:cw], pl[:, 0:cw], AF.Relu, [pl], [r_])
                        else:
                            A.tsc("dve", r_[:, 0:cw], pl[:, 0:cw], 0.0, None, ALU.max, None, [pl], [r_])
                        A.mm(pS[:, 0:cw], dg[:, h, :], r_[:, 0:cw], h == 0, h == 7, [dg, r_], [pS])
                    A.S.op("dve", lambda e, o=am[:, ci:ci + 1], i=pS[:, 0:cw]:
                           e.tensor_reduce(out=o, in_=i, axis=AX.X, op=ALU.max, apply_absolute_value=True), [pS], [am])
                    pn = pen[ci % 2]
                    A.tsc("dve", pn[:, 0:cw], iota[:, 0:cw], float(c0), qlim[:, b:b + 1], ALU.add, ALU.is_ge, [iota, qlim], [pn])
                    A.stt("dve", Sb[:, c0:c0 + cw], pn[:, 0:cw], -1.0e30, pS[:, 0:cw], ALU.mult, ALU.add, [pn, pS], [Sb])
                lo, w_, mid, cnt, ge, amx = (st[k] for k in ("lo", "w", "mid", "cnt", "ge", "amx"))
                A.S.op("dve", lambda e, i=am[:, 0:nch]: e.tensor_reduce(out=amx[:], in_=i, axis=AX.X, op=ALU.max), [am], [amx])
                A.tsc("dve", lo[:], amx[:], -1.0001, -1e-6, ALU.mult, ALU.add, [amx], [lo])
                A.tsc("dve", w_[:], amx[:], 2.0004, 4e-6, ALU.mult, ALU.add, [amx], [w_])
                for it in range(NITER):
                    A.tsc("dve", w_[:], w_[:], 0.5, None, ALU.mult, None, [w_], [w_])
                    A.tt("dve", mid[:], lo[:], w_[:], ALU.add, [lo, w_], [mid])
                    A.tsc("dve", mask[:, 0:L], Sb[:, 0:L], mid[:, 0:1], 0.0, ALU.is_ge, ALU.add, [Sb, mid], [mask, cnt], accum_out=cnt[:, 0:1])
                    A.tsc("dve", ge[:], cnt[:], 255.5, None, ALU.is_ge, None, [cnt], [ge])
                    A.stt("dve", lo[:], ge[:], w_[:, 0:1], lo[:], ALU.mult, ALU.add, [ge, w_, lo], [lo])
                A.tsc("dve", mask[:, 0:L], Sb[:, 0:L], lo[:, 0:1], None, ALU.is_ge, None, [Sb, lo], [mask])
                po = [A.next_ps(), A.next_ps()]
                pr = [A.next_ps(), A.next_ps()]
                acc = po + pr
                nkt = L // 128
                for hg in range(2):
                    A.mm(po[hg][:], zeros[:, 0:128], zeros[:], True, False, [zeros], [po[hg]])
                for ci in range(nch):
                    c0 = ci * 512
                    cw = min(512, L - c0)
                    k_, v_ = kc[ci % 2], vc[ci % 2]
                    A.dma("sp", k_[:, :, 0:cw], kT_view[:, :, c0:c0 + cw], k_, [A.kbT], [k_])
                    A.dma("act", v_[:, 0:cw // 128, :], A.vb.h[c0:c0 + cw, :].rearrange("(t p) n -> p t n", p=128), v_, [A.vb], [v_])
                    pmk = A.ps_free(acc)
                    pmv = pmk[:].bitcast(BF16).rearrange("p (a b) -> p a b", a=8)
                    for j in range(cw // 128):
                        A.tr(pmv[:, j, :], mask[:, c0 + j * 128:c0 + (j + 1) * 128], A.ident[:], [mask, A.ident], [pmk])
                    for j in range(cw // 128):
                        kt = ci * 4 + j
                        first, last = kt == 0, kt == nkt - 1
                        for hg in range(2):
                            ps = A.ps_free(acc + [pmk])
                            for hh in range(4):
                                h = hg * 4 + hh
                                A.mm(ps[:, hh * 128:(hh + 1) * 128], k_[:, h, j * 128:(j + 1) * 128], qq[:, h, :], True, True, [k_, qq], [ps])
                            pe1, pm1 = pe_[(kt * 2 + hg) % 2], pm[(kt * 2 + hg) % 2]
                            A.act(pe1[:], ps[:], AF.Exp, [ps], [pe1], scale=SCD)
                            A.tt("dve", pm1[:].rearrange("p (h q) -> p h q", h=4), pe1[:].rearrange("p (h q) -> p h q", h=4),
                                 pmv[:, j, :].unsqueeze(1).broadcast_to([128, 4, 128]), ALU.mult, [pe1, pmk], [pm1])
                            for hh in range(4):
                                h = hg * 4 + hh
                                A.mm(po[hg][:, hh * 128:(hh + 1) * 128], v_[:, j, h * 128:(h + 1) * 128], pm1[:, hh * 128:(hh + 1) * 128],
                                     False, last and hh == 3, [v_, pm1], [po[hg]])
                            A.mm(pr[hg][:], ones[:], pm1[:], first, last, [ones, pm1], [pr[hg]])
                o_ = ost[bi % 2]
                for hg in range(2):
                    A.S.op("dve", lambda e, p_=pr[hg][:]: e.reciprocal(out=rinv[:], in_=p_), [pr[hg]], [rinv])
                    A.tt("dve", o_[:, hg * 4:(hg + 1) * 4, :], po[hg][:].rearrange("p (h q) -> p h q", h=4),
                         rinv[:].rearrange("p (h q) -> p h q", h=4), ALU.mult, [po[hg], rinv], [o_])
                A.dma("pool", A.odsaT.h.rearrange("h d t -> d h t")[:, :, b * 128:(b + 1) * 128], o_[:], o_, [o_], [A.odsaT])

    def phase_p5(self):
        A, nc, cfg = self, self.nc, self.cfg
        A.omemT = A.dscratch("omemT", [8, 128, NQ])
        SCM = float(256 ** -0.5)
        ttiles = [(t0, min(512, NQ - t0)) for t0 in range(0, NQ, 512)]
        with contextlib.ExitStack() as es:
            mkT = A.sb(es, "mkT", [128, 8, 256], BF16)
            mv = A.sb(es, "mv", [128, 2, 1024], BF16)
            ones = A.sb(es, "ones5", [128, 128], BF16)
            A.S.op("pool", lambda e: e.memset(ones[:], 1.0), [], [ones])
            with contextlib.ExitStack() as es2:
                A.S.begin_const()
                gb = A.sb(es2, "gbm", [128, D], F32, const=True)
                A.dma("sp", gb[:], A.mem_norm.broadcast_to([128, D]), gb, [], [gb])
                A.tsc("dve", gb[:], gb[:], float(np.sqrt(D)), None, ALU.mult, None, [gb], [gb])
                gk = A.sb(es2, "gk", [128, 256], F32, const=True)
                A.dma("sp", gk[:], A.mem_k_norm.broadcast_to([128, 256]), gk, [], [gk])
                A.tsc("dve", gk[:], gk[:], float(np.sqrt(256.0)), None, ALU.mult, None, [gk], [gk])
                A.S.end_const()
                xt = A.sb(es2, "xm", [128, D], F32)
                ss = A.sb(es2, "ssm", [128, 1], F32)
                rs = A.sb(es2, "rsm", [128, 1], F32)
                h = A.sb(es2, "hm", [128, D], BF16)
                mhT = A.sb(es2, "mhT", [128, 16, 256], BF16)
                for b in range(2):
                    A.dma("sp", xt[:], A.mem[128 * b:128 * (b + 1), :], xt, [], [xt])
                    A.norm_tile(xt, gb, h, ss, rs, h, D, [])
                    A.transpose_to(mhT, b * 128, h)
                Wb = [A.sb(es2, f"Wm{i}", [128, 16, 512], BF16) for i in range(2)]
                stage = [A.sb(es2, f"wstm{i}", [128, 16, 128], F32) for i in range(2)]
                sq = A.sb(es2, "sqm", [128, 512], F32)
                ssq = A.sb(es2, "ssqm", [128, 2], F32)
                rs2 = A.sb(es2, "rs2m", [128, 2], F32)
                yraw = A.sb(es2, "yrawm", [128, 512], F32)
                y = A.sb(es2, "ym", [128, 512], F32)
                yb = A.sb(es2, "ybm", [128, 512], BF16)
                for blk in range(4):
                    w = Wb[blk % 2]
                    A.load_w_bf16(w, 0, A.w_mem_kv, blk * 512, 512, stage)
                    for b in range(2):
                        p = A.next_ps()
                        for c in range(16):
                            A.mm(p[:], mhT[:, c, b * 128:(b + 1) * 128], w[:, c, :], c == 0, c == 15, [w, mhT], [p])
                        if blk < 2:
                            A.act(sq[:], p[:], AF.Square, [p], [sq])
                            A.S.op("dve", lambda e, o=ssq[:, 0:2], i=sq[:].rearrange("p (h d) -> p h d", h=2):
                                   e.tensor_reduce(out=o, in_=i, axis=AX.X, op=ALU.add), [sq], [ssq])
                            A.cp("dve", yraw[:], p[:], [p], [yraw])
                            A.rsqrt(rs2[:], ssq[:], 3, rs2, ssq)
                            y3 = y[:].rearrange("p (h d) -> p h d", h=2)
                            A.tt("dve", y3, yraw[:].rearrange("p (h d) -> p h d", h=2),
                                 rs2[:].unsqueeze(2).broadcast_to([128, 2, 256]), ALU.mult, [yraw, rs2], [y])
                            A.tt("pool", yb[:].rearrange("p (h d) -> p h d", h=2), y3,
                                 gk[:].unsqueeze(1).broadcast_to([128, 2, 256]), ALU.mult, [y, gk], [yb])
                            p2 = A.next_ps()
                            pv = p2[:].bitcast(BF16).rearrange("p (a b) -> p a b", a=8)
                            for hh in range(4):
                                A.tr(pv[:, hh, :], yb[:, hh * 128:(hh + 1) * 128], A.ident[:], [yb, A.ident], [p2])
                            A.evac(mkT[:, blk * 4:(blk + 1) * 4, b * 128:(b + 1) * 128], pv[:, 0:4, :], [p2], [mkT])
                        else:
                            A.evac(mv[:, b, (blk - 2) * 512:(blk - 1) * 512], p[:], [p], [mv])
                A.S.barrier()
            qm = [A.sb(es, f"qm{i}", [128, 2, NQ], BF16) for i in range(2)]
            pT = [A.sb(es, f"pTm{i}", [128, 512], BF16) for i in range(4)]
            rinv = A.sb(es, "rinvm", [128, 512], F32)
            ost = [A.sb(es, f"ostm{i}", [128, 2, NQ], BF16) for i in range(2)]
            n = 0
            for hd in range(4):
                q_, o_ = qm[hd % 2], ost[hd % 2]
                A.dma("sp", q_[:], A.qmT.h[2 * hd:2 * hd + 2].rearrange("h d t -> d h t"), q_, [A.qmT], [q_])
                for (t0, tw) in ttiles:
                    po = [A.next_ps(), A.next_ps()]
                    pr = A.next_ps()
                    acc = po + [pr]
                    for mb in range(2):
                        ps = A.ps_free(acc)
                        for dc in range(2):
                            A.mm(ps[:, 0:tw], mkT[:, hd * 2 + dc, mb * 128:(mb + 1) * 128], q_[:, dc, t0:t0 + tw], dc == 0, dc == 1, [mkT, q_], [ps])
                        pt = pT[n % 4]
                        n += 1
                        A.act(pt[:, 0:tw], ps[:, 0:tw], AF.Exp, [ps], [pt], scale=SCM)
                        for dc in range(2):
                            A.mm(po[dc][:, 0:tw], mv[:, mb, hd * 256 + dc * 128: hd * 256 + (dc + 1) * 128], pt[:, 0:tw], mb == 0, mb == 1, [mv, pt], [po[dc]])
                        A.mm(pr[:, 0:tw], ones[:], pt[:, 0:tw], mb == 0, mb == 1, [ones, pt], [pr])
                    A.S.op("dve", lambda e, tw=tw, pr=pr: e.reciprocal(out=rinv[:, 0:tw], in_=pr[:, 0:tw]), [pr], [rinv])
                    for dc in range(2):
                        A.tt("dve", o_[:, dc, t0:t0 + tw], po[dc][:, 0:tw], rinv[:, 0:tw], ALU.mult, [po[dc], rinv], [o_])
                A.dma("pool", A.omemT.h[2 * hd:2 * hd + 2].rearrange("h d t -> d h t"), o_[:], o_, [o_], [A.omemT])

    def phase_p6(self):
        A, nc, cfg = self, self.nc, self.cfg
        A.mergedT = A.dscratch("mergedT", [128, 16, NQ])
        A.x1 = A.dscratch("x1", [NQ, D], F32)
        A.h2T = A.dscratch("h2T", [128, 16, NQ])
        subt = [(0, 512), (512, 512), (1024, 128)]
        srcs = (A.osbT, A.odsaT, A.omemT)
        with contextlib.ExitStack() as es:
            A.S.begin_const()
            bg = A.sb(es, "bg", [128, 48], F32, const=True)
            A.dma("sp", bg[:], A.bg_pt[:, :], bg, [], [bg])
            A.S.end_const()
            oT = [A.sb(es, f"oT{i}", [128, 8, QW], BF16) for i in range(3)]
            hq = A.sb(es, "hq6", [128, 16, QW], BF16)
            mT = A.sb(es, "mT", [128, 16, QW], BF16)
            Wbr = [[A.sb(es, f"Wbr{i}_{j}", [128, 8, 128], BF16) for i in range(3)] for j in range(2)]
            Wg = [[A.sb(es, f"Wg{i}_{j}", [128, 16, 128], BF16) for i in range(3)] for j in range(2)]
            stage = [A.sb(es, f"wst6{i}", [128, 16, 128], F32) for i in range(2)]
            gsb = [A.sb(es, f"gsb{i}", [128, 512], F32) for i in range(2)]
            macc = [A.sb(es, f"macc{i}", [128, 512], F32) for i in range(2)]
            tmp = [A.sb(es, f"tmp6{i}", [128, 512], F32) for i in range(2)]
            n = 0
            for slot in range(2):
                q0 = slot * QW
                for i in range(3):
                    A.dma("sp" if i % 2 else "act", oT[i][:], srcs[i].h.rearrange("h d t -> d h t")[:, :, q0:q0 + QW], oT[i], [srcs[i]], [oT[i]])
                A.dma("sp", hq[:], A.hTq.h[:, :, q0:q0 + QW], hq, [A.hTq], [hq])
                for ft in range(16):
                    j = (slot * 16 + ft) % 2
                    for i in range(3):
                        A.load_w_bf16(Wbr[j][i], 0, A.w_branch[i], ft * 128, 128, stage)
                        A.load_w_bf16(Wg[j][i], 0, A.w_in, 7752 + i * 2048 + ft * 128, 128, stage)
                    for (t0, tw) in subt:
                        mc = macc[n % 2]
                        for i in range(3):
                            pb = A.next_ps()
                            for c in range(8):
                                A.mm(pb[:, 0:tw], Wbr[j][i][:, c, :], oT[i][:, c, t0:t0 + tw], c == 0, c == 7, [Wbr[j][i], oT[i]], [pb])
                            pg = A.next_ps()
                            for c in range(16):
                                A.mm(pg[:, 0:tw], Wg[j][i][:, c, :], hq[:, c, t0:t0 + tw], c == 0, c == 15, [Wg[j][i], hq], [pg])
                            g_ = gsb[(n * 3 + i) % 2]
                            A.act(g_[:, 0:tw], pg[:, 0:tw], AF.Sigmoid, [pg, bg], [g_], bias=bg[:, i * 16 + ft:i * 16 + ft + 1])
                            if i == 0:
                                A.tt("dve", mc[:, 0:tw], g_[:, 0:tw], pb[:, 0:tw], ALU.mult, [g_, pb], [mc])
                            else:
                                t_ = tmp[i % 2]
                                A.tt("dve", t_[:, 0:tw], g_[:, 0:tw], pb[:, 0:tw], ALU.mult, [g_, pb], [t_])
                                if i == 1:
                                    A.tt("pool", mc[:, 0:tw], mc[:, 0:tw], t_[:, 0:tw], ALU.add, [mc, t_], [mc])
                                else:
                                    A.tt("pool", mT[:, ft, t0:t0 + tw], mc[:, 0:tw], t_[:, 0:tw], ALU.add, [mc, t_], [mT])
                        n += 1
                A.dma("pool", A.mergedT.h[:, :, q0:q0 + QW], mT[:], mT, [mT], [A.mergedT])
            A.S.barrier()
        with contextlib.ExitStack() as es:
            Wout = A.sb(es, "Wout", [128, 16, D], BF16)
            with contextlib.ExitStack() as es2:
                stage = [A.sb(es2, f"wst6b{i}", [128, 16, 256], F32) for i in range(2)]
                A.load_w_bf16(Wout, 0, A.w_out, 0, D, stage)
                A.S.barrier()
            A.S.begin_const()
            gbf = A.sb(es, "gbf", [128, D], F32, const=True)
            A.dma("sp", gbf[:], A.ffn_norm.broadcast_to([128, D]), gbf, [], [gbf])
            A.tsc("dve", gbf[:], gbf[:], float(np.sqrt(D)), None, ALU.mult, None, [gbf], [gbf])
            A.S.end_const()
            mTt = [A.sb(es, f"mTt{i}", [128, 16, 128], BF16) for i in range(2)]
            xt = [A.sb(es, f"x6{i}", [128, D], F32) for i in range(2)]
            x1t = [A.sb(es, f"x1t{i}", [128, D], F32) for i in range(2)]
            ss = [A.sb(es, f"ss6{i}", [128, 1], F32) for i in range(2)]
            rs = [A.sb(es, f"rs6{i}", [128, 1], F32) for i in range(2)]
            h2 = [A.sb(es, f"h26{i}", [128, D], BF16) for i in range(2)]
            h2t = [A.sb(es, f"h2t{i}", [128, 16, 128], BF16) for i in range(2)]
            for b in range(NQ // 128):
                m_, x_, x1_, h_, ht_ = mTt[b % 2], xt[b % 2], x1t[b % 2], h2[b % 2], h2t[b % 2]
                A.dma("sp", m_[:], A.mergedT.h[:, :, b * 128:(b + 1) * 128], m_, [A.mergedT], [m_])
                A.dma("act", x_[:], A.x_q[b * 128:(b + 1) * 128, :], x_, [], [x_])
                for ct in range(4):
                    p = A.next_ps()
                    for ft in range(16):
                        A.mm(p[:], m_[:, ft, :], Wout[:, ft, ct * 512:(ct + 1) * 512], ft == 0, ft == 15, [m_, Wout], [p])
                    A.tt("dve", x1_[:, ct * 512:(ct + 1) * 512], p[:], x_[:, ct * 512:(ct + 1) * 512], ALU.add, [p, x_], [x1_])
                A.dma("pool", A.x1.h[b * 128:(b + 1) * 128, :], x1_[:], x1_, [x1_], [A.x1])
                A.norm_tile(x1_, gbf, h_, ss[b % 2], rs[b % 2], h_, D, [])
                A.transpose_to(ht_, 0, h_)
                A.dma("pool", A.h2T.h[:, :, b * 128:(b + 1) * 128], ht_[:], ht_, [ht_], [A.h2T])

    def phase_p7(self):
        A, nc, cfg = self, self.nc, self.cfg
        NFF = D_FF // 128
        with contextlib.ExitStack() as es:
            actT = A.sb(es, "actT", [128, NFF, SEG], BF16)
            for slot in range(2):
                q0 = slot * QW
                with contextlib.ExitStack() as es2:
                    h2 = A.sb(es2, "h2seg", [128, 16, QW], BF16)
                    A.dma("sp", h2[:], A.h2T.h[:, :, q0:q0 + QW], h2, [A.h2T], [h2])
                    A.S.begin_const()
                    fl = A.sb(es2, "flag", [128, 2], F32, const=True)
                    A.dma("sp", fl[:], A.flags.broadcast_to([128, 2]), fl, [], [fl])
                    cwb = A.sb(es2, "cwb", [128, 2 * NFF, 4], F32, const=True)
                    A.dma("sp", cwb[:], A.cwb[:, :, :], cwb, [], [cwb])
                    A.S.end_const()
                    A.tsc("dve", h2[:, :, 126:128], h2[:, :, 126:128], fl[:, slot:slot + 1], None, ALU.mult, None, [h2, fl], [h2])
                    W = [A.sb(es2, f"Wup{i}", [128, 16, 128], BF16) for i in range(4)]
                    stage = [A.sb(es2, f"wst7{i}", [128, 16, 128], F32) for i in range(2)]
                    pre = [A.sb(es2, f"pre{i}", [128, SEG + 2], F32) for i in range(2)]
                    u = [A.sb(es2, f"u{i}", [128, SEG], F32) for i in range(2)]
                    sg = A.sb(es2, "sg", [128, SEG], F32)
                    n = 0
                    for i in range(NFF):
                        for which in range(2):
                            tile = which * NFF + i
                            w = W[n % 4]
                            pr_, u_ = pre[n % 2], u[n % 2]
                            n += 1
                            A.load_w_bf16(w, 0, A.w_up, tile * 128, 128, stage)
                            ph = A.next_ps()
                            for c in range(16):
                                A.mm(ph[:, 0:2], w[:, c, :], h2[:, c, 126:128], c == 0, c == 15, [w, h2], [ph])
                            A.evac(pr_[:, 0:2], ph[:, 0:2], [ph], [pr_])
                            for t0 in (0, 512):
                                p = A.next_ps()
                                for c in range(16):
                                    A.mm(p[:], w[:, c, :], h2[:, c, 128 + t0:128 + t0 + 512], c == 0, c == 15, [w, h2], [p])
                                A.evac(pr_[:, 2 + t0:2 + t0 + 512], p[:], [p], [pr_])
                            A.tsc("dve", u_[:], pr_[:, 0:SEG], cwb[:, tile, 0:1], cwb[:, tile, 3:4], ALU.mult, ALU.add, [pr_, cwb], [u_])
                            A.stt("dve", u_[:], pr_[:, 1:SEG + 1], cwb[:, tile, 1:2], u_[:], ALU.mult, ALU.add, [pr_, cwb, u_], [u_])
                            A.stt("dve", u_[:], pr_[:, 2:SEG + 2], cwb[:, tile, 2:3], u_[:], ALU.mult, ALU.add, [pr_, cwb, u_], [u_])
                            if which == 0:
                                A.act(sg[:], u_[:], AF.Silu, [u_], [sg])
                            else:
                                A.tt("pool", actT[:, i, :], sg[:], u_[:], ALU.mult, [sg, u_], [actT])
                    A.S.barrier()
                with contextlib.ExitStack() as es2:
                    Wd = A.sb(es2, "Wd", [128, NFF, 512], BF16)
                    stage = [A.sb(es2, f"wst7d{i}", [128, NFF, 128], F32) for i in range(2)]
                    xt = [A.sb(es2, f"x7{i}", [128, 512], F32) for i in range(2)]
                    ot = [A.sb(es2, f"o7{i}", [128, 512], F32) for i in range(2)]
                    n = 0
                    for ct in range(4):
                        A.load_w_bf16(Wd, 0, A.w_down, ct * 512, 512, stage)
                        for tb in range(SEG // 128):
                            row0 = q0 + 128 + tb * 128
                            x_, o_ = xt[n % 2], ot[n % 2]
                            n += 1
                            A.dma("act", x_[:], A.x1.h[row0:row0 + 128, ct * 512:(ct + 1) * 512], x_, [A.x1], [x_])
                            p = A.next_ps()
                            for i in range(NFF):
                                A.mm(p[:], actT[:, i, tb * 128:(tb + 1) * 128], Wd[:, i, :], i == 0, i == NFF - 1, [actT, Wd], [p])
                            A.tt("dve", o_[:], p[:], x_[:], ALU.add, [p, x_], [o_])
                            orow = slot * SEG + tb * 128
                            A.dma("pool", A.out[orow:orow + 128, ct * 512:(ct + 1) * 512], o_[:], o_, [o_], [])
                    A.S.barrier()


def core_tokens(c):
    sA, sB = c, 15 - c
    hA = np.arange(SEG * sA - 128, SEG * sA) if sA > 0 else np.arange(0, 128)
    hB = np.arange(SEG * sB - 128, SEG * sB)
    idx = np.concatenate([hA, np.arange(SEG * sA, SEG * (sA + 1)), hB, np.arange(SEG * sB, SEG * (sB + 1))])
    flags = np.array([[1.0 if sA > 0 else 0.0, 1.0]], np.float32)
    return idx, flags


def host_inputs(inp, c, seq=SEQ):
    f = np.float32
    idx, flags = core_tokens(c)
    idx = np.minimum(idx, seq - 1) if seq < SEQ else idx
    x = inp["x"][0]
    pos = inp["positions"][0]
    m = {}
    m["x_all"] = np.ascontiguousarray(x[:seq])
    pp = np.zeros((128, 128), np.int32)
    pp[:, :seq // 128] = pos[:seq].reshape(-1, 128).T
    m["pos_pt"] = pp
    m["x_q"] = np.ascontiguousarray(x[idx])
    m["posq_pt"] = np.ascontiguousarray(pos[idx].reshape(-1, 128).T.astype(np.int32))
    m["qidx_row"] = idx.astype(f)[None, :]
    m["qidx_pt"] = np.ascontiguousarray(idx.astype(f).reshape(-1, 128).T)
    m["qlim_pt"] = np.ascontiguousarray(((idx // 64 + 1) * 64).astype(f).reshape(-1, 128).T)
    kk = np.arange(SEQ, dtype=f)
    m["kidx_pt"] = np.ascontiguousarray(kk.reshape(-1, 128).T)
    m["kidx_row"] = kk[None, :seq].copy()
    m["flags"] = flags
    m["c_ident"] = np.eye(128, dtype=f)
    m["c_tri"] = np.tril(np.ones((128, 128), f))
    m["c_inv64"] = (10000.0 ** (-np.arange(64) / 64.0) / (2 * np.pi)).astype(f)[None, :]
    m["mem"] = np.ascontiguousarray(inp["mem"][0])
    m["bg_pt"] = np.ascontiguousarray(inp["b_gate"].reshape(48, 128).T)
    cw = np.concatenate([inp["conv_w"][0], inp["conv_b"].reshape(1, -1)], 0)
    m["cwb"] = np.ascontiguousarray(cw.reshape(4, 86, 128).transpose(2, 1, 0))
    for k in ("attn_norm", "mem_norm", "b_gate", "dsa_q_norm", "dsa_k_norm", "mem_q_norm", "mem_k_norm",
              "ffn_norm", "conv_b"):
        m[k] = np.ascontiguousarray(inp[k]).reshape(1, -1)
    for k in ("w_in", "w_mem_kv", "w_branch", "w_out", "w_up", "conv_w", "w_down"):
        m[k] = np.ascontiguousarray(inp[k][0])
    return m


def full_cfg():
    return dict(phases=["p1a", "p1b", "p2", "p3", "p4", "p5", "p6", "p7"])


def kernel(**inputs):
    inp = {k: np.asarray(v) for k, v in inputs.items()}
    B = Builder(full_cfg())
    nc = B.build()
    in_maps = [host_inputs(inp, c) for c in range(NCORE)]
    res = run_bass_kernel_spmd(nc, in_maps, core_ids=list(range(NCORE)))
    out = np.zeros((1, SEQ, D), np.float32)
    for c in range(NCORE):
        o = np.asarray(res.results[c]["out"], np.float32)
        out[0, SEG * c:SEG * (c + 1)] = o[:SEG]
        out[0, SEG * (15 - c):SEG * (16 - c)] = o[SEG:]
    return out
```

```python
import contextlib
import numpy as np
import concourse.bass as bass
import concourse.mybir as mybir
from concourse.bass_utils import run_bass_kernel_spmd

F32 = mybir.dt.float32
BF16 = mybir.dt.bfloat16
I32 = mybir.dt.int32
AF = mybir.ActivationFunctionType
ALU = mybir.AluOpType
AX = mybir.AxisListType

D = 2048
SEQ = 16384
D_IN = 13896
D_FF = 5504
EPS = 1e-6
NCORE = 8
SEG = 1024
QW = 128 + SEG
NQ = 2 * QW

ENGS = ["pe", "act", "dve", "pool", "sp"]
EPOCH = 30000
ATTACH_WAITS = True
SAME_ENGINE_INORDER = ("pe",)


class Buf:
    __slots__ = ("name", "writes", "reads", "dsem", "ndma", "fresh", "qs", "swq", "grp")

    def __init__(self, name):
        self.name = name
        self.writes = {}
        self.reads = {}
        self.dsem = None
        self.ndma = 0
        self.fresh = False
        self.qs = set()
        self.swq = False
        self.grp = None


class TT:
    def __init__(self, h, name):
        self.h = h
        self.b = Buf(name)

    def __getitem__(self, k):
        return self.h[k]


class Sched:
    def __init__(self, nc, sems):
        self.nc = nc
        self.free_sems = list(sems)
        self.stream = {e: [] for e in ENGS}
        self.cnt = {e: 0 for e in ENGS}
        self.esems = {e: [] for e in ENGS}
        self.waited = {e: {} for e in ENGS}
        self.dtok = {}
        self.recycled = []
        self.dead_keys = set()
        self.grp_cur = {}
        self.snap = {e: [] for e in ENGS}
        self.dsnap = {}
        self.deferred = None
        self.cgrp = Buf("constgroup")
        self.free_sw = []
        self.free_hw = []
        self.sem_inc = {}
        self.barbufs = [Buf("bar_" + e) for e in ENGS]
        self.nops = 0

    def _sem_of(self, key):
        if key[0] == "e":
            lst = self.esems[key[1]]
            while len(lst) <= key[2]:
                lst.append(self.free_sems.pop())
            return lst[key[2]]
        return key[1]

    def _need(self, eng, deps):
        w = self.waited[eng]
        for key, val in sorted(deps.items(), key=lambda kv: -kv[1]):
            if w.get(key, 0) >= val:
                continue
            w[key] = val
            self.stream[eng].append(("wait", self._sem_of(key), val))
            if key[0] == "e":
                sn = self.snap[key[1]][key[2] * EPOCH + val - 1]
            else:
                sn = self.dsnap.get((key, val))
            if sn:
                for k2, v2 in sn.items():
                    if w.get(k2, 0) < v2:
                        w[k2] = v2

    def _collect(self, reads, writes, eng):
        deps = {}
        for b in reads:
            for k, v in b.writes.items():
                if eng == "pe" and k[0] == "e" and k[1] == "pe":
                    continue
                if deps.get(k, 0) < v:
                    deps[k] = v
        for b in writes:
            for src in (b.writes, b.reads):
                for k, v in src.items():
                    if eng in SAME_ENGINE_INORDER and k[0] == "e" and k[1] == eng:
                        continue
                    if deps.get(k, 0) < v:
                        deps[k] = v
        for k in deps:
            if k in self.grp_cur:
                deps[k] = self.grp_cur[k]
        return deps

    deferred_started = False

    def begin_const(self):
        self.deferred = []
        self.deferred_started = False

    def end_const(self):
        d, self.deferred = self.deferred, None
        for a in d:
            self.op(*a)

    def op(self, eng, fn, reads=(), writes=()):
        if self.deferred is not None:
            self.deferred.append((eng, fn, reads, writes))
            return
        reads = [r.b if isinstance(r, TT) else r for r in reads]
        writes = [r.b if isinstance(r, TT) else r for r in writes]
        self._need(eng, self._collect(reads, writes, eng))
        self.cnt[eng] += 1
        c = self.cnt[eng]
        key = ("e", eng, (c - 1) // EPOCH)
        val = (c - 1) % EPOCH + 1
        self.stream[eng].append(("op", fn, self._sem_of(key)))
        self.snap[eng].append(dict(self.waited[eng]))
        self.nops += 1
        for b in reads:
            if b.reads.get(key, 0) < val:
                b.reads[key] = val
        for b in writes:
            b.writes = {key: val}
            b.reads = {}

    def dma(self, q, out_ap, in_ap, sb, reads=(), writes=()):
        reads = [r.b if isinstance(r, TT) else r for r in reads]
        writes = [r.b if isinstance(r, TT) else r for r in writes]
        sb = sb.b if isinstance(sb, TT) else sb
        self._need(q, self._collect(reads, writes, None))
        own = sb.grp if sb.grp is not None else sb
        if own.dsem is None:
            own.dsem = self.free_sems.pop()
        if sb.grp is not None:
            assert self.deferred is not None, "constant loads must be inside begin_const/end_const"
            if own.ndma:
                self._need(q, {("d", own.dsem, 0): 16 * own.ndma} if not self.deferred_started else {})
                self.deferred_started = True
        own.ndma += 1
        key = ("d", own.dsem, 0)
        val = 16 * own.ndma
        self.dtok[key] = val
        if sb.grp is not None:
            self.grp_cur[key] = val
        self.stream[q].append(("dma", out_ap, in_ap, own.dsem))
        self.dsnap[(key, val)] = dict(self.waited[q])
        self.nops += 1
        for b in reads:
            if b.reads.get(key, 0) < val:
                b.reads[key] = val
        for b in writes:
            keep = {k: v for k, v in b.writes.items() if k[0] == "d"}
            keep[key] = val
            b.writes = keep
            b.reads = {}

    def _all_tokens(self):
        deps = dict(self.dtok)
        for e in ENGS:
            c = self.cnt[e]
            if c:
                deps[("e", e, (c - 1) // EPOCH)] = (c - 1) % EPOCH + 1
        return deps

    def barrier(self):
        deps = self._all_tokens()
        for e in ENGS:
            self._need(e, deps)

    def recycle(self, b):
        return

    def emit(self):
        nc = self.nc
        names = {"pe": "tensor", "act": "scalar", "dve": "vector", "pool": "gpsimd", "sp": "sync"}
        with nc.Block() as block:
            def run(name):
                def body(e):
                    pend = None
                    for it in self.stream[name]:
                        if it[0] == "wait":
                            if pend is not None:
                                e.wait_ge(pend[0], pend[1])
                            pend = (it[1], it[2])
                            continue
                        if pend is not None and not ATTACH_WAITS:
                            e.wait_ge(pend[0], pend[1])
                            pend = None
                        if it[0] == "op":
                            ins = it[1](e)
                        else:
                            ins = e.dma_start(out=it[1], in_=it[2])
                        if pend is not None:
                            ins._wait_ge(pend[0], pend[1])
                            pend = None
                        ins.then_inc(it[-1], 1 if it[0] == "op" else 16)
                    if pend is not None:
                        e.wait_ge(pend[0], pend[1])
                return body
            for name in ENGS:
                if self.stream[name]:
                    getattr(block, names[name])(run(name))
        self.stream = {e: [] for e in ENGS}


class Builder:
    def __init__(self, cfg):
        self.cfg = cfg
        self.nc = bass.Bass("TRN2", target_bir_lowering=False)
        self.rr = 0
        self.SEQ = cfg.get("seq", SEQ)

    def din(self, name, shape, dt=F32):
        return self.nc.dram_tensor(name, list(shape), dt, kind="ExternalInput").ap()

    def dscratch(self, name, shape, dt=BF16):
        kind = "ExternalOutput" if name in self.cfg.get("debug_out", ()) else "Internal"
        t = self.nc.dram_tensor(name, list(shape), dt, kind=kind).ap()
        return TT(t, name)

    def sb(self, es, name, shape, dt, const=False):
        self.uid = getattr(self, "uid", 0) + 1
        name = f"{name}_{self.uid}"
        t = TT(es.enter_context(self.nc.sbuf_tensor(name, list(shape), dt)), name)
        if const:
            t.b.grp = self.S.cgrp
        return t

    def next_ps(self):
        p = self.ps[self.rr % len(self.ps)]
        self.rr += 1
        return p

    def mm(self, out, lhsT, rhs, start, stop, R, W):
        self.S.op("pe", lambda e: e.matmul(out, lhsT, rhs, start=start, stop=stop), R, W)

    def tr(self, out, in_, ident, R, W):
        self.S.op("pe", lambda e: e.transpose(out, in_, ident), R, W)

    def act(self, out, in_, func, R, W, **kw):
        self.S.op("act", lambda e: e.activation(out=out, in_=in_, func=func, **kw), R, W)

    def tsc(self, eng, out, in0, s1, s2, op0, op1, R, W, **kw):
        if op1 is None:
            self.S.op(eng, lambda e: e.tensor_scalar(out=out, in0=in0, scalar1=s1, scalar2=None, op0=op0, **kw), R, W)
        else:
            self.S.op(eng, lambda e: e.tensor_scalar(out=out, in0=in0, scalar1=s1, scalar2=s2, op0=op0, op1=op1, **kw), R, W)

    def tt(self, eng, out, in0, in1, op, R, W):
        self.S.op(eng, lambda e: e.tensor_tensor(out=out, in0=in0, in1=in1, op=op), R, W)

    def stt(self, eng, out, in0, scalar, in1, op0, op1, R, W):
        self.S.op(eng, lambda e: e.scalar_tensor_tensor(out=out, in0=in0, scalar=scalar, in1=in1, op0=op0, op1=op1), R, W)

    def cp(self, eng, out, in_, R, W):
        if eng == "act":
            self.S.op("act", lambda e: e.copy(out=out, in_=in_), R, W)
        else:
            self.S.op(eng, lambda e: e.tensor_copy(out=out, in_=in_), R, W)

    def dma(self, q, out, in_, sbt, R, W):
        self.S.dma(q, out, in_, sbt, R, W)

    def evac(self, out, in_, R, W):
        self.ev = getattr(self, "ev", 0) + 1
        self.cp("act" if self.ev % 2 else "dve", out, in_, R, W)

    def build(self):
        nc, cfg = self.nc, self.cfg
        A = self
        A.x_all = A.din("x_all", [A.SEQ, D])
        A.pos_pt = A.din("pos_pt", [128, 128], I32)
        A.x_q = A.din("x_q", [NQ, D])
        A.posq_pt = A.din("posq_pt", [128, NQ // 128], I32)
        A.qidx_row = A.din("qidx_row", [1, NQ])
        A.qidx_pt = A.din("qidx_pt", [128, NQ // 128])
        A.kidx_pt = A.din("kidx_pt", [128, 128])
        A.kidx_row = A.din("kidx_row", [1, A.SEQ])
        A.flags = A.din("flags", [1, 2])
        A.bg_pt = A.din("bg_pt", [128, 48])
        A.cwb = A.din("cwb", [128, 86, 4])
        A.qlim_pt = A.din("qlim_pt", [128, NQ // 128])
        A.c_ident = A.din("c_ident", [128, 128])
        A.c_tri = A.din("c_tri", [128, 128])
        A.c_inv64 = A.din("c_inv64", [1, 64])
        A.mem = A.din("mem", [256, D])
        A.attn_norm = A.din("attn_norm", [1, D])
        A.mem_norm = A.din("mem_norm", [1, D])
        A.w_in = A.din("w_in", [D, D_IN])
        A.b_gate = A.din("b_gate", [1, 3 * D])
        A.dsa_q_norm = A.din("dsa_q_norm", [1, 128])
        A.dsa_k_norm = A.din("dsa_k_norm", [1, 128])
        A.mem_q_norm = A.din("mem_q_norm", [1, 256])
        A.mem_k_norm = A.din("mem_k_norm", [1, 256])
        A.w_mem_kv = A.din("w_mem_kv", [D, 2048])
        A.w_branch = A.din("w_branch", [3, 1024, D])
        A.w_out = A.din("w_out", [D, D])
        A.ffn_norm = A.din("ffn_norm", [1, D])
        A.w_up = A.din("w_up", [D, 2 * D_FF])
        A.conv_w = A.din("conv_w", [3, 2 * D_FF])
        A.conv_b = A.din("conv_b", [1, 2 * D_FF])
        A.w_down = A.din("w_down", [D_FF, D])
        A.out = nc.dram_tensor("out", [2 * SEG, D], F32, kind="ExternalOutput").ap()
        A.kaT = A.dscratch("kaT", [8, 128, A.SEQ])
        A.va = A.dscratch("va", [A.SEQ, 1024])
        A.kbT = A.dscratch("kbT", [8, 128, A.SEQ])
        A.vb = A.dscratch("vb", [A.SEQ, 1024])
        A.kiT = A.dscratch("kiT", [64, A.SEQ])

        with contextlib.ExitStack() as top:
            sems = [top.enter_context(nc.semaphore(f"s{i}")) for i in range(101)]
            A.S = Sched(nc, sems)
            A.ps = [TT(top.enter_context(nc.psum_tensor(f"ps{i}", [128, 512], F32)), f"ps{i}") for i in range(8)]
            A.S.begin_const()
            A.ident = A.sb(top, "ident", [128, 128], BF16)
            A.identf = A.sb(top, "identf", [128, 128], F32, const=True)
            A.dma("sp", A.identf[:], A.c_ident[:, :], A.identf, [], [A.identf])
            A.cp("dve", A.ident[:], A.identf[:], [A.identf], [A.ident])
            A.cst = A.sb(top, "cst", [128, 4], F32)
            for j, v in enumerate((-float(np.pi), D * EPS, 128 * EPS, 256 * EPS)):
                A.S.op("pool", lambda e, j=j, v=v: e.memset(A.cst[:, j:j + 1], float(v)), [], [A.cst])
            A.invt = A.sb(top, "invt", [128, 64], F32, const=True)
            A.dma("sp", A.invt[:], A.c_inv64.broadcast_to([128, 64]), A.invt, [], [A.invt])
            A.S.end_const()
            for ph in cfg["phases"]:
                getattr(A, "phase_" + ph)()
                A.S.barrier()
                A.S.emit()
        return nc

    def load_w_bf16(self, dst, dst_cols, w_ap, col0, ncols, stage):
        A = self
        wv = w_ap.rearrange("(c p) n -> p c n", p=128)
        nchunk = wv.shape[1]
        step = stage[0].h.shape[2]
        i = getattr(A, "_wst", 0)
        for j0 in range(0, ncols, step):
            n = min(step, ncols - j0)
            st = stage[i % len(stage)]
            i += 1
            for cc0 in range(0, nchunk, 16):
                cc1 = min(nchunk, cc0 + 16)
                A.dma("sp" if i % 2 else "act", st[:, cc0:cc1, 0:n], wv[:, cc0:cc1, col0 + j0:col0 + j0 + n], st, [], [st])
            A.cp("pool", dst[:, 0:nchunk, dst_cols + j0:dst_cols + j0 + n], st[:, 0:nchunk, 0:n], [st], [dst])
        A._wst = i

    def rope_tables(self, T, pos4, R, gains=None):
        A = self
        v = lambda t: t[:].rearrange("p (s j) -> p s j", s=4)
        t, ti, tf, fr, frc, c, sn, cs = (T[k] for k in ("t", "ti", "tf", "fr", "frc", "c", "sin", "cos"))
        A.tt("dve", v(t), A.invt[:].unsqueeze(1).broadcast_to([128, 4, 64]),
             pos4.unsqueeze(2).broadcast_to([128, 4, 64]), ALU.mult, list(R) + [A.invt], [t])
        A.cp("dve", ti[:], t[:], [t], [ti])
        A.cp("dve", tf[:], ti[:], [ti], [tf])
        A.tt("dve", fr[:], t[:], tf[:], ALU.subtract, [t, tf], [fr])
        A.tsc("dve", frc[:], fr[:], 0.25, None, ALU.add, None, [fr], [frc])
        A.tsc("dve", c[:], frc[:], 0.5, None, ALU.is_gt, None, [frc], [c])
        A.tt("dve", frc[:], frc[:], c[:], ALU.subtract, [frc, c], [frc])
        A.act(sn[:], fr[:], AF.Sin, [fr], [sn], scale=float(2 * np.pi))
        A.act(cs[:], frc[:], AF.Sin, [frc], [cs], scale=float(2 * np.pi))
        if gains is not None:
            gt, gap = gains
            g1 = gap[:, 0:64].unsqueeze(1).broadcast_to([128, 4, 64])
            g2 = gap[:, 64:128].unsqueeze(1).broadcast_to([128, 4, 64])
            A.tt("pool", v(T["A"]), v(cs), g1, ALU.mult, [cs, gt], [T["A"]])
            A.tt("pool", v(T["B"]), v(sn), g2, ALU.mult, [sn, gt], [T["B"]])
            A.tt("pool", v(T["C"]), v(cs), g2, ALU.mult, [cs, gt], [T["C"]])
            A.tt("pool", v(T["D"]), v(sn), g1, ALU.mult, [sn, gt], [T["D"]])

    def rope_apply(self, out, y, nh, hd, tabs, tabR, R, W, tmp):
        A = self
        half = hd // 2
        tA, tB, tC, tD = tabs
        t1, t2 = tmp
        bc = lambda t: t.unsqueeze(1).broadcast_to([128, nh, half])
        y1, y2 = y[:, :, 0:half], y[:, :, half:hd]
        v1 = t1[:, 0:nh * half].rearrange("p (h d) -> p h d", h=nh)
        v2 = t2[:, 0:nh * half].rearrange("p (h d) -> p h d", h=nh)
        tabR = list(tabR)
        A.tt("dve", v1, y1, bc(tA), ALU.mult, list(R) + tabR, [t1])
        A.tt("pool", v2, y2, bc(tB), ALU.mult, list(R) + tabR, [t2])
        A.tt("dve", out[:, :, 0:half], v1, v2, ALU.subtract, [t1, t2], list(W))
        A.tt("dve", v1, y2, bc(tC), ALU.mult, list(R) + tabR, [t1])
        A.tt("pool", v2, y1, bc(tD), ALU.mult, list(R) + tabR, [t2])
        A.tt("dve", out[:, :, half:hd], v1, v2, ALU.add, [t1, t2], list(W))

    def norm_tile(self, xt, gb, junk, ss, rs, h, ncols_scale, R_x):
        A = self
        A.S.op("pool", lambda e: e.memset(ss[:, 0:1], 0.0), [], [ss])
        A.act(junk[:], xt[:], AF.Square, [xt] + list(R_x), [junk, ss], accum_out=ss[:, 0:1])
        A.rsqrt(rs[:, 0:1], ss[:, 0:1], 1, rs, ss)
        A.stt("dve", h[:], xt[:], rs[:, 0:1], gb[:], ALU.mult, ALU.mult, [xt, rs, gb], [h])

    def rsqrt(self, out, in_, eps_col, out_t, in_t):
        A = self
        A.act(out, in_, AF.Sqrt, [in_t, A.cst], [out_t], bias=A.cst[:, eps_col:eps_col + 1])
        A.S.op("dve", lambda e: e.reciprocal(out=out, in_=out), [out_t], [out_t])

    def transpose_to(self, hT, col0, h, nchunks=16):
        A = self
        for c0 in range(0, nchunks, 8):
            p = A.next_ps()
            pv = p[:].bitcast(BF16).rearrange("p (a b) -> p a b", a=8)
            n = min(8, nchunks - c0)
            for c in range(n):
                A.tr(pv[:, c, :], h[:, (c0 + c) * 128:(c0 + c + 1) * 128], A.ident[:], [h, A.ident], [p])
            A.evac(hT[:, c0:c0 + n, col0:col0 + 128], pv[:, 0:n, :], [p], [hT])

    def phase_p1a(self):
        self.kv_pass("a")

    def phase_p1b(self):
        self.kv_pass("b")

    def kv_pass(self, which):
        A, nc, cfg = self, self.nc, self.cfg
        NT = cfg.get("nt1", A.SEQ // 512)
        isb = which == "b"
        ncols = 2112 if isb else 2048
        with contextlib.ExitStack() as es:
            W = A.sb(es, "W1", [128, 16, ncols], BF16)
            A.S.begin_const()
            gb = A.sb(es, "gb", [128, D], F32, const=True)
            A.dma("sp", gb[:], A.attn_norm.broadcast_to([128, D]), gb, [], [gb])
            A.tsc("dve", gb[:], gb[:], float(np.sqrt(D)), None, ALU.mult, None, [gb], [gb])
            A.S.end_const()
            with contextlib.ExitStack() as es2:
                stage = [A.sb(es2, f"wst{i}", [128, 16, 256], F32) for i in range(2)]
                if isb:
                    A.load_w_bf16(W, 0, A.w_in, 4096, 1024, stage)
                    A.load_w_bf16(W, 1024, A.w_in, 5120, 1024, stage)
                    A.load_w_bf16(W, 2048, A.w_in, 7680, 64, stage)
                else:
                    A.load_w_bf16(W, 0, A.w_in, 1024, 1024, stage)
                    A.load_w_bf16(W, 1024, A.w_in, 2048, 1024, stage)
                A.S.barrier()
            xt = [A.sb(es, f"xt{i}", [128, D], F32) for i in range(2)]
            ss = [A.sb(es, f"ss{i}", [128, 1], F32) for i in range(2)]
            rs = [A.sb(es, f"rs{i}", [128, 1], F32) for i in range(2)]
            h = [A.sb(es, f"h{i}", [128, D], BF16) for i in range(2)]
            hT = [A.sb(es, f"hT{i}", [128, 16, 512], BF16) for i in range(2)]
            kst = [A.sb(es, f"kst{i}", [128, 8, 512], BF16) for i in range(2)]
            vst = [A.sb(es, f"vst{i}", [128, 4, 1024], BF16) for i in range(2)]
            if isb:
                A.S.begin_const()
                posi = A.sb(es, "posi", [128, 128], I32, const=True)
                posf = A.sb(es, "posf", [128, 128], F32)
                A.dma("sp", posi[:], A.pos_pt[:, :], posi, [], [posi])
                A.cp("dve", posf[:], posi[:], [posi], [posf])
                A.gains = A.sb(es, "gains", [128, 128], F32, const=True)
                A.dma("sp", A.gains[:], A.dsa_k_norm.broadcast_to([128, 128]), A.gains, [], [A.gains])
                A.tsc("dve", A.gains[:], A.gains[:], float(np.sqrt(128.0)), None, ALU.mult, None, [A.gains], [A.gains])
                A.S.end_const()
                RT = {k: A.sb(es, "rt_" + k, [128, 256], I32 if k == "ti" else F32)
                      for k in ("t", "ti", "tf", "fr", "frc", "c", "sin", "cos", "A", "B", "C", "D")}
                sq = A.sb(es, "sq", [128, 512], F32)
                ssq = A.sb(es, "ssq", [128, 8], F32)
                rs8 = A.sb(es, "rs8", [128, 8], F32)
                yraw = A.sb(es, "yraw", [128, 1024], F32)
                y = A.sb(es, "y", [128, 1024], F32)
                kbr = A.sb(es, "kbr", [128, 1024], BF16)
                t1 = A.sb(es, "t1", [128, 512], F32)
                t2 = A.sb(es, "t2", [128, 512], F32)
                kiraw = A.sb(es, "kiraw", [128, 64], F32)
                kir = A.sb(es, "kir", [128, 64], BF16)
                kist = [A.sb(es, f"kist{i}", [64, 512], BF16) for i in range(2)]
            kT_dst = (A.kbT if isb else A.kaT)
            v_dst = (A.vb if isb else A.va)
            kT_view = kT_dst.h.rearrange("h d t -> d h t")
            for ti in range(NT):
                tok0 = 512 * ti
                hTt = hT[ti % 2]
                for s in range(4):
                    g = 4 * ti + s
                    x_ = xt[g % 2]
                    A.dma("sp", x_[:], A.x_all[tok0 + 128 * s: tok0 + 128 * (s + 1), :], x_, [], [x_])
                    A.norm_tile(x_, gb, h[g % 2], ss[g % 2], rs[g % 2], h[g % 2], D, [])
                    A.transpose_to(hTt, s * 128, h[g % 2])
                ks = kst[ti % 2]
                vs = vst[ti % 2]
                if not isb:
                    for hh in range(8):
                        p = A.next_ps()
                        for c in range(16):
                            A.mm(p[:], W[:, c, hh * 128:(hh + 1) * 128], hTt[:, c, :], c == 0, c == 15, [W, hTt], [p])
                        A.evac(ks[:, hh, :], p[:], [p], [ks])
                else:
                    A.rope_tables(RT, posf[:, 4 * ti:4 * ti + 4], [posf], (A.gains, A.gains))
                    rv = lambda k, s: RT[k][:, s * 64:(s + 1) * 64]
                    for s in range(4):
                        g = 4 * ti + s
                        for cc in range(2):
                            p = A.next_ps()
                            for c in range(16):
                                A.mm(p[:], hTt[:, c, s * 128:(s + 1) * 128], W[:, c, cc * 512:(cc + 1) * 512], c == 0, c == 15, [W, hTt], [p])
                            A.act(sq[:], p[:], AF.Square, [p], [sq])
                            A.S.op("dve", lambda e, o=ssq[:, cc * 4:(cc + 1) * 4], i=sq[:].rearrange("p (h d) -> p h d", h=4):
                                   e.tensor_reduce(out=o, in_=i, axis=AX.X, op=ALU.add), [sq], [ssq])
                            A.cp("dve", yraw[:, cc * 512:(cc + 1) * 512], p[:], [p], [yraw])
                        A.rsqrt(rs8[:], ssq[:], 2, rs8, ssq)
                        y3 = y[:].rearrange("p (h d) -> p h d", h=8)
                        A.tt("dve", y3, yraw[:].rearrange("p (h d) -> p h d", h=8),
                             rs8[:].unsqueeze(2).broadcast_to([128, 8, 128]), ALU.mult, [yraw, rs8], [y])
                        kb3 = kbr[:].rearrange("p (h d) -> p h d", h=8)
                        A.rope_apply(kb3, y3, 8, 128, [rv(k, s) for k in "ABCD"], [RT[k] for k in "ABCD"], [y], [kbr], (t1, t2))
                        p = A.next_ps()
                        pv = p[:].bitcast(BF16).rearrange("p (a b) -> p a b", a=8)
                        for hh in range(8):
                            A.tr(pv[:, hh, :], kbr[:, hh * 128:(hh + 1) * 128], A.ident[:], [kbr, A.ident], [p])
                        A.evac(ks[:, :, s * 128:(s + 1) * 128], pv, [p], [ks])
                        c32 = RT["cos"][:, s * 64:(s + 1) * 64:2]
                        s32 = RT["sin"][:, s * 64:(s + 1) * 64:2]
                        p = A.next_ps()
                        for c in range(16):
                            A.mm(p[:, 0:64], hTt[:, c, s * 128:(s + 1) * 128], W[:, c, 2048:2112], c == 0, c == 15, [W, hTt], [p])
                        A.cp("act", kiraw[:], p[:, 0:64], [p], [kiraw])
                        A.rope_apply(kir[:].rearrange("p (h d) -> p h d", h=1), kiraw[:].rearrange("p (h d) -> p h d", h=1),
                                     1, 64, [c32, s32, c32, s32], [RT["cos"], RT["sin"]], [kiraw], [kir], (t1, t2))
                        p = A.next_ps()
                        pv = p[:].bitcast(BF16)
                        A.tr(pv[0:64, 0:128], kir[:], A.ident[:], [kir, A.ident], [p])
                        A.evac(kist[ti % 2][:, s * 128:(s + 1) * 128], pv[0:64, 0:128], [p], [kist[ti % 2]])
                    A.dma("pool", A.kiT.h[:, tok0:tok0 + 512], kist[ti % 2][:], kist[ti % 2], [kist[ti % 2]], [A.kiT])
                A.dma("pool", kT_view[:, :, tok0:tok0 + 512], ks[:], ks, [ks], [kT_dst])
                vc0 = 1024
                for s in range(4):
                    for cc in range(2):
                        p = A.next_ps()
                        for c in range(16):
                            A.mm(p[:], hTt[:, c, s * 128:(s + 1) * 128], W[:, c, vc0 + cc * 512: vc0 + (cc + 1) * 512], c == 0, c == 15, [W, hTt], [p])
                        A.evac(vs[:, s, cc * 512:(cc + 1) * 512], p[:], [p], [vs])
                A.dma("pool", v_dst.h[tok0:tok0 + 512, :].rearrange("(s p) n -> p s n", p=128), vs[:], vs, [vs], [v_dst])

    def phase_p2(self):
        A, nc, cfg = self, self.nc, self.cfg
        NB = NQ // 128
        A.hTq = A.dscratch("hTq", [128, 16, NQ])
        A.qaT = A.dscratch("qaT", [8, 128, NQ])
        A.qbT = A.dscratch("qbT", [8, 128, NQ])
        A.qmT = A.dscratch("qmT", [8, 128, NQ])
        A.qiT = A.dscratch("qiT", [8, 64, NQ])
        A.wi = A.dscratch("wi", [NQ, 8], F32)
        ttiles = [(t0, min(512, NQ - t0)) for t0 in range(0, NQ, 512)]
        with contextlib.ExitStack() as es:
            hTq = A.sb(es, "hTq", [128, 16, NQ], BF16)
            Wb = [A.sb(es, f"Wb{i}", [128, 16, 512], BF16) for i in range(2)]
            stage = [A.sb(es, f"wst{i}", [128, 16, 128], F32) for i in range(2)]
            TB = {k: A.sb(es, "qt_" + k, [128, NB * 64], F32) for k in ("sin", "cos", "A", "B", "C", "D")}
            A.S.begin_const()
            gq = A.sb(es, "gq", [128, 128], F32, const=True)
            gm = A.sb(es, "gm", [128, 256], F32, const=True)
            A.dma("sp", gq[:], A.dsa_q_norm.broadcast_to([128, 128]), gq, [], [gq])
            A.tsc("dve", gq[:], gq[:], float(np.sqrt(128.0)), None, ALU.mult, None, [gq], [gq])
            A.dma("sp", gm[:], A.mem_q_norm.broadcast_to([128, 256]), gm, [], [gm])
            A.tsc("dve", gm[:], gm[:], float(np.sqrt(256.0)), None, ALU.mult, None, [gm], [gm])
            with contextlib.ExitStack() as es2:
                posi = A.sb(es2, "posqi", [128, NB], I32, const=True)
                posf = A.sb(es2, "posqf", [128, NB], F32)
                A.dma("sp", posi[:], A.posq_pt[:, :], posi, [], [posi])
                A.cp("dve", posf[:], posi[:], [posi], [posf])
                gb = A.sb(es2, "gbq", [128, D], F32, const=True)
                A.dma("sp", gb[:], A.attn_norm.broadcast_to([128, D]), gb, [], [gb])
                A.tsc("dve", gb[:], gb[:], float(np.sqrt(D)), None, ALU.mult, None, [gb], [gb])
                A.S.end_const()
                RT = {k: A.sb(es2, "rq_" + k, [128, 256], I32 if k == "ti" else F32)
                      for k in ("t", "ti", "tf", "fr", "frc", "c", "sin", "cos", "A", "B", "C", "D")}
                for b0 in range(0, NB, 4):
                    n = min(4, NB - b0)
                    pos4 = posf[:, b0:b0 + 4] if n == 4 else posf[:, NB - 4:NB]
                    bb = b0 if n == 4 else NB - 4
                    A.rope_tables(RT, pos4, [posf], (gq, gq))
                    for k in TB:
                        A.cp("pool", TB[k][:, bb * 64:(bb + 4) * 64], RT[k][:], [RT[k]], [TB[k]])
                xt = [A.sb(es2, f"xq{i}", [128, D], F32) for i in range(2)]
                ss = [A.sb(es2, f"ssq{i}", [128, 1], F32) for i in range(2)]
                rs = [A.sb(es2, f"rsq{i}", [128, 1], F32) for i in range(2)]
                h = [A.sb(es2, f"hq{i}", [128, D], BF16) for i in range(2)]
                for b in range(NB):
                    x_ = xt[b % 2]
                    A.dma("sp", x_[:], A.x_q[128 * b:128 * (b + 1), :], x_, [], [x_])
                    A.norm_tile(x_, gb, h[b % 2], ss[b % 2], rs[b % 2], h[b % 2], D, [])
                    A.transpose_to(hTq, b * 128, h[b % 2])
                A.dma("pool", A.hTq.h[:, :, :], hTq[:], hTq, [hTq], [A.hTq])
                A.S.barrier()
            qst = A.sb(es, "qst", [128, 4, NQ], BF16)
            sq = A.sb(es, "sqq", [128, 512], F32)
            ssq = A.sb(es, "ssqq", [128, 4], F32)
            rs4 = A.sb(es, "rs4", [128, 4], F32)
            yraw = A.sb(es, "yrawq", [128, 512], F32)
            y = A.sb(es, "yq", [128, 512], F32)
            yb = A.sb(es, "ybq", [128, 512], BF16)
            t1 = A.sb(es, "t1q", [128, 256], F32)
            t2 = A.sb(es, "t2q", [128, 256], F32)
            qist = [A.sb(es, f"qist{i}", [64, 8, 128], BF16) for i in range(2)]
            wist = A.sb(es, "wist", [128, NB, 8], F32)
            wcount = [0]

            def load_block(col0, ncols):
                w = Wb[wcount[0] % 2]
                wcount[0] += 1
                A.load_w_bf16(w, 0, A.w_in, col0, ncols, stage)
                return w

            def tok_major(w, ncols, b):
                p = A.next_ps()
                for c in range(16):
                    A.mm(p[:, 0:ncols], hTq[:, c, b * 128:(b + 1) * 128], w[:, c, 0:ncols], c == 0, c == 15, [w, hTq], [p])
                return p

            for blk in range(2):
                w = load_block(blk * 512, 512)
                for hh in range(4):
                    for (t0, tw) in ttiles:
                        p = A.next_ps()
                        for c in range(16):
                            A.mm(p[:, 0:tw], w[:, c, hh * 128:(hh + 1) * 128], hTq[:, c, t0:t0 + tw], c == 0, c == 15, [w, hTq], [p])
                        A.evac(qst[:, hh, t0:t0 + tw], p[:, 0:tw], [p], [qst])
                A.dma("pool", A.qaT.h[4 * blk:4 * blk + 4].rearrange("h d t -> d h t"), qst[:], qst, [qst], [A.qaT])
            for blk in range(2):
                w = load_block(3072 + blk * 512, 512)
                for b in range(NB):
                    p = tok_major(w, 512, b)
                    A.act(sq[:], p[:], AF.Square, [p], [sq])
                    A.S.op("dve", lambda e, o=ssq[:], i=sq[:].rearrange("p (h d) -> p h d", h=4):
                           e.tensor_reduce(out=o, in_=i, axis=AX.X, op=ALU.add), [sq], [ssq])
                    A.cp("dve", yraw[:], p[:], [p], [yraw])
                    A.rsqrt(rs4[:], ssq[:], 2, rs4, ssq)
                    y3 = y[:].rearrange("p (h d) -> p h d", h=4)
                    A.tt("dve", y3, yraw[:].rearrange("p (h d) -> p h d", h=4),
                         rs4[:].unsqueeze(2).broadcast_to([128, 4, 128]), ALU.mult, [yraw, rs4], [y])
                    yb3 = yb[:].rearrange("p (h d) -> p h d", h=4)
                    tabs = [TB[k][:, b * 64:(b + 1) * 64] for k in "ABCD"]
                    A.rope_apply(yb3, y3, 4, 128, tabs, [TB[k] for k in "ABCD"], [y], [yb], (t1, t2))
                    p2 = A.next_ps()
                    pv = p2[:].bitcast(BF16).rearrange("p (a b) -> p a b", a=8)
                    for hh in range(4):
                        A.tr(pv[:, hh, :], yb[:, hh * 128:(hh + 1) * 128], A.ident[:], [yb, A.ident], [p2])
                    A.evac(qst[:, :, b * 128:(b + 1) * 128], pv[:, 0:4, :], [p2], [qst])
                A.dma("pool", A.qbT.h[4 * blk:4 * blk + 4].rearrange("h d t -> d h t"), qst[:], qst, [qst], [A.qbT])
            for blk in range(2):
                w = load_block(6144 + blk * 512, 512)
                for b in range(NB):
                    p = tok_major(w, 512, b)
                    A.act(sq[:], p[:], AF.Square, [p], [sq])
                    A.S.op("dve", lambda e, o=ssq[:, 0:2], i=sq[:].rearrange("p (h d) -> p h d", h=2):
                           e.tensor_reduce(out=o, in_=i, axis=AX.X, op=ALU.add), [sq], [ssq])
                    A.cp("dve", yraw[:], p[:], [p], [yraw])
                    A.rsqrt(rs4[:, 0:2], ssq[:, 0:2], 3, rs4, ssq)
                    y3 = y[:].rearrange("p (h d) -> p h d", h=2)
                    A.tt("dve", y3, yraw[:].rearrange("p (h d) -> p h d", h=2),
                         rs4[:, 0:2].unsqueeze(2).broadcast_to([128, 2, 256]), ALU.mult, [yraw, rs4], [y])
                    A.tt("pool", yb[:].rearrange("p (h d) -> p h d", h=2), y3,
                         gm[:].unsqueeze(1).broadcast_to([128, 2, 256]), ALU.mult, [y, gm], [yb])
                    p2 = A.next_ps()
                    pv = p2[:].bitcast(BF16).rearrange("p (a b) -> p a b", a=8)
                    for hh in range(4):
                        A.tr(pv[:, hh, :], yb[:, hh * 128:(hh + 1) * 128], A.ident[:], [yb, A.ident], [p2])
                    A.evac(qst[:, :, b * 128:(b + 1) * 128], pv[:, 0:4, :], [p2], [qst])
                A.dma("pool", A.qmT.h[4 * blk:4 * blk + 4].rearrange("h d t -> d h t"), qst[:], qst, [qst], [A.qmT])
            w = load_block(7168, 512)
            for b in range(NB):
                p = tok_major(w, 512, b)
                A.cp("act", yraw[:], p[:], [p], [yraw])
                c32 = TB["cos"][:, b * 64:(b + 1) * 64:2]
                s32 = TB["sin"][:, b * 64:(b + 1) * 64:2]
                A.rope_apply(yb[:].rearrange("p (h d) -> p h d", h=8), yraw[:].rearrange("p (h d) -> p h d", h=8),
                             8, 64, [c32, s32, c32, s32], [TB["cos"], TB["sin"]], [yraw], [yb], (t1, t2))
                p2 = A.next_ps()
                pv = p2[:].bitcast(BF16).rearrange("p (a b) -> p a b", a=8)
                for hh in range(8):
                    A.tr(pv[0:64, hh, :], yb[:, hh * 64:(hh + 1) * 64], A.ident[:], [yb, A.ident], [p2])
                qs = qist[b % 2]
                A.evac(qs[:], pv[0:64, :, :], [p2], [qs])
                A.dma("pool", A.qiT.h.rearrange("h d t -> d h t")[:, :, b * 128:(b + 1) * 128], qs[:], qs, [qs], [A.qiT])
            w = load_block(7744, 8)
            for b in range(NB):
                p = tok_major(w, 8, b)
                A.evac(wist[:, b, :], p[:, 0:8], [p], [wist])
            A.dma("pool", A.wi.h.rearrange("(b p) h -> p b h", p=128), wist[:], wist, [wist], [A.wi])

    def q_tiles(self):
        nkt = self.SEQ // 128
        out = []
        for slot, (segmax, segmin) in enumerate(((7, 0), (15, 8))):
            base = slot * QW
            for (off, w, toff) in ((0, 128, -128), (128, 512, 0), (640, 512, 512)):
                qmax = SEG * segmax + toff + w
                qmin = max(SEG * segmin + toff, 0)
                nk = min((qmax + 127) // 128, nkt)
                out.append((base + off, w, nk, min(qmin // 128, nk) if self.SEQ == SEQ else 0))
        return out

    def phase_p3(self):
        A, nc, cfg = self, self.nc, self.cfg
        A.osbT = A.dscratch("osbT", [8, 128, NQ])
        heads = cfg.get("heads", range(8))
        nkt = A.SEQ // 128
        SC = float(128 ** -0.5)
        with contextlib.ExitStack() as es:
            kT = [A.sb(es, f"kTa{i}", [128, A.SEQ], BF16) for i in range(2)]
            vv = [A.sb(es, f"vva{i}", [128, nkt, 128], BF16) for i in range(2)]
            qT = [A.sb(es, f"qTa{i}", [128, NQ], BF16) for i in range(2)]
            ost = [A.sb(es, f"osta{i}", [128, NQ], BF16) for i in range(2)]
            A.S.begin_const()
            qrow = A.sb(es, "qrow", [128, NQ], F32, const=True)
            kidx = A.sb(es, "kidx", [128, 128], F32, const=True)
            A.dma("sp", qrow[:], A.qidx_row.broadcast_to([128, NQ]), qrow, [], [qrow])
            A.dma("sp", kidx[:], A.kidx_pt[:, :], kidx, [], [kidx])
            trif = A.sb(es, "trif", [128, 128], F32, const=True)
            tri = A.sb(es, "tri", [128, 128], BF16)
            ones = A.sb(es, "ones", [128, 128], BF16)
            one1 = A.sb(es, "one1", [128, 1], F32)
            A.dma("sp", trif[:], A.c_tri[:, :], trif, [], [trif])
            A.cp("dve", tri[:], trif[:], [trif], [tri])
            A.S.op("pool", lambda e: e.memset(ones[:], 1.0), [], [ones])
            A.S.op("pool", lambda e: e.memset(one1[:], 1.0), [], [one1])
            A.S.end_const()
            NCH = 3
            eb = [[A.sb(es, f"eb{c}_{i}", [128, 512], F32) for i in range(2)] for c in range(NCH)]
            spb = [[A.sb(es, f"spb{c}_{i}", [128, 512], BF16) for i in range(2)] for c in range(NCH)]
            sps = [[A.sb(es, f"sps{c}_{i}", [128, 512], BF16) for i in range(2)] for c in range(NCH)]
            tb = [[A.sb(es, f"tb{c}_{i}", [128, 512], F32) for i in range(2)] for c in range(NCH)]
            ab = [[A.sb(es, f"ab{c}_{i}", [128, 512], BF16) for i in range(2)] for c in range(NCH)]
            locked = []

            def chain(c, q0, tw, nk, kmask, k_, v_, q_, o_):
                po = A.ps_free(locked)
                locked.append(po)
                for i, kt in enumerate(range(nk - 1, -1, -1)):
                    first, last = i == 0, kt == 0
                    e_, sp_, t_, a_ = eb[c][i % 2], spb[c][i % 2], tb[c][i % 2], ab[c][i % 2]
                    ss_new, ss_old = sps[c][i % 2], sps[c][(i + 1) % 2]
                    pz = A.ps_free(locked)
                    A.mm(pz[:, 0:tw], k_[:, kt * 128:(kt + 1) * 128], q_[:, q0:q0 + tw], True, True, [k_, q_], [pz])
                    A.act(e_[:, 0:tw], pz[:, 0:tw], AF.Exp, [pz], [e_], scale=SC)
                    if kt >= kmask:
                        A.stt("dve", e_[:, 0:tw], qrow[:, q0:q0 + tw], kidx[:, kt:kt + 1], e_[:, 0:tw],
                              ALU.is_gt, ALU.mult, [qrow, kidx, e_], [e_])
                    A.act(sp_[:, 0:tw], e_[:, 0:tw], AF.Ln, [e_, one1], [sp_], bias=one1[:, 0:1])
                    yield
                    pc = A.ps_free(locked)
                    A.mm(pc[:, 0:tw], tri[:], sp_[:, 0:tw], True, first, [tri, sp_], [pc])
                    if first:
                        A.cp("pool", ss_new[:, 0:tw], sp_[:, 0:tw], [sp_], [ss_new])
                    else:
                        A.mm(pc[:, 0:tw], ones[:], ss_old[:, 0:tw], False, True, [ones, ss_old], [pc])
                        if not last:
                            A.tt("pool", ss_new[:, 0:tw], ss_old[:, 0:tw], sp_[:, 0:tw], ALU.add, [ss_old, sp_], [ss_new])
                    A.act(t_[:, 0:tw], pc[:, 0:tw], AF.Exp, [pc], [t_], scale=-1.0)
                    A.tt("dve", a_[:, 0:tw], e_[:, 0:tw], t_[:, 0:tw], ALU.mult, [e_, t_], [a_])
                    yield
                    A.mm(po[:, 0:tw], v_[:, kt, :], a_[:, 0:tw], first, last, [v_, a_], [po])
                A.evac(o_[:, q0:q0 + tw], po[:, 0:tw], [po], [o_])
                locked.remove(po)

            for hi, hh in enumerate(heads):
                k_, v_, q_, o_ = kT[hi % 2], vv[hi % 2], qT[hi % 2], ost[hi % 2]
                nsp = 4
                for j in range(nsp):
                    c0, c1 = A.SEQ * j // nsp, A.SEQ * (j + 1) // nsp
                    A.dma("sp" if j % 2 else "act", k_[:, c0:c1], A.kaT.h[hh, :, c0:c1], k_, [A.kaT], [k_])
                nsv = max(4, nkt // 8)
                for j in range(nsv):
                    t0, t1 = nkt * j // nsv, nkt * (j + 1) // nsv
                    A.dma("act" if j % 2 else "sp", v_[:, t0:t1, :],
                          A.va.h[t0 * 128:t1 * 128, hh * 128:(hh + 1) * 128].rearrange("(t p) d -> p t d", p=128),
                          v_, [A.va], [v_])
                A.dma("sp", q_[:], A.qaT.h[hh, :, :], q_, [A.qaT], [q_])
                qts = A.q_tiles()
                for g0 in range(0, len(qts), NCH):
                    gens = [chain(c, *qt, k_, v_, q_, o_) for c, qt in enumerate(qts[g0:g0 + NCH])]
                    while gens:
                        for g in list(gens):
                            try:
                                next(g)
                            except StopIteration:
                                gens.remove(g)
                A.dma("pool", A.osbT.h[hh, :, :], o_[:], o_, [o_], [A.osbT])

    def q_blocks(self):
        out = []
        for slot, segmax in enumerate((7, 15)):
            for j in range(-1, 8):
                qmax0 = SEG * segmax + 128 * j
                out.append((slot * 9 + j + 1, min(qmax0 + 128, self.SEQ)))
        return out

    def ps_free(self, excl):
        for _ in range(2 * len(self.ps)):
            p = self.next_ps()
            if all(p is not x for x in excl):
                return p
        raise RuntimeError("no free PSUM bank")

    def phase_p4(self):
        A, nc, cfg = self, self.nc, self.cfg
        A.odsaT = A.dscratch("odsaT", [8, 128, NQ])
        NB = NQ // 128
        blocks = [qb for qb in A.q_blocks() if qb[0] in cfg.get("qblocks", range(NB))]
        SCD = float(128 ** -0.5)
        NITER = cfg.get("niter", 22)
        with contextlib.ExitStack() as es:
            A.S.begin_const()
            kiT = A.sb(es, "kiT", [64, A.SEQ], BF16, const=True)
            A.dma("sp", kiT[:], A.kiT.h[:, :], kiT, [A.kiT], [kiT])
            Sb = A.sb(es, "Sb", [128, A.SEQ], F32)
            mask = A.sb(es, "maskb", [128, A.SEQ], BF16)
            iota = A.sb(es, "iota", [128, 512], F32, const=True)
            A.dma("sp", iota[:], A.kidx_row[:, 0:512].broadcast_to([128, 512]), iota, [], [iota])
            qlim = A.sb(es, "qlim", [128, NB], F32, const=True)
            A.dma("sp", qlim[:], A.qlim_pt[:, :], qlim, [], [qlim])
            ones = A.sb(es, "ones4", [128, 128], BF16)
            A.S.op("pool", lambda e: e.memset(ones[:], 1.0), [], [ones])
            zeros = A.sb(es, "zeros4", [128, 512], BF16)
            A.S.op("pool", lambda e: e.memset(zeros[:], 0.0), [], [zeros])
            wi = A.sb(es, "wi_sb", [128, NB, 8], F32, const=True)
            A.dma("sp", wi[:], A.wi.h.rearrange("(b p) h -> p b h", p=128), wi, [A.wi], [wi])
            A.S.end_const()
            qi = [A.sb(es, f"qi{i}", [64, 8, 128], BF16) for i in range(2)]
            qb_ = [A.sb(es, f"qbq{i}", [128, 8, 128], BF16) for i in range(2)]
            Dg = [A.sb(es, f"Dg{i}", [128, 8, 128], BF16) for i in range(2)]
            rl = [A.sb(es, f"rl{i}", [128, 512], BF16) for i in range(3)]
            pen = [A.sb(es, f"pen{i}", [128, 512], F32) for i in range(2)]
            am = A.sb(es, "am", [128, 32], F32)
            st = {k: A.sb(es, "bs_" + k, [128, 1], F32) for k in ("lo", "w", "mid", "cnt", "ge", "amx")}
            kc = [A.sb(es, f"kc{i}", [128, 8, 512], BF16) for i in range(2)]
            vc = [A.sb(es, f"vc{i}", [128, 4, 1024], BF16) for i in range(2)]
            pe_ = [A.sb(es, f"pexp{i}", [128, 512], BF16) for i in range(2)]
            pm = [A.sb(es, f"pm{i}", [128, 512], BF16) for i in range(2)]
            rinv = A.sb(es, "rinv", [128, 512], F32)
            ost = [A.sb(es, f"ostd{i}", [128, 8, 128], BF16) for i in range(2)]
            kT_view = A.kbT.h.rearrange("h d t -> d h t")
            qiT_view = A.qiT.h.rearrange("h d t -> d h t")
            qbT_view = A.qbT.h.rearrange("h d t -> d h t")
            cnt_i = 0
            for bi, (b, L) in enumerate(blocks):
                nch = (L + 511) // 512
                qi_, qq, dg = qi[bi % 2], qb_[bi % 2], Dg[bi % 2]
                A.dma("sp", qi_[:], qiT_view[:, :, b * 128:(b + 1) * 128], qi_, [A.qiT], [qi_])
                A.dma("sp", qq[:], qbT_view[:, :, b * 128:(b + 1) * 128], qq, [A.qbT], [qq])
                for h in range(8):
                    A.tsc("dve", dg[:, h, :], A.ident[:], wi[:, b, h:h + 1], None, ALU.mult, None, [A.ident, wi], [dg])
                for ci in range(nch):
                    c0 = ci * 512
                    cw = min(512, L - c0)
                    pS = A.next_ps()
                    for h in range(8):
                        pl = A.ps_free([pS])
                        A.mm(pl[:, 0:cw], qi_[:, h, :], kiT[:, c0:c0 + cw], True, True, [qi_, kiT], [pl])
                        r_ = rl[cnt_i % 3]
                        cnt_i += 1
                        if h % 2 == 0:
                            A.act(r_[:, 0:cw], pl[:, 0:cw], AF.Relu, [pl], [r_])
                        else:
                            A.tsc("dve", r_[:, 0:cw], pl[:, 0:cw], 0.0, None, ALU.max, None, [pl], [r_])
                        A.mm(pS[:, 0:cw], dg[:, h, :], r_[:, 0:cw], h == 0, h == 7, [dg, r_], [pS])
                    A.S.op("dve", lambda e, o=am[:, ci:ci + 1], i=pS[:, 0:cw]:
                           e.tensor_reduce(out=o, in_=i, axis=AX.X, op=ALU.max, apply_absolute_value=True), [pS], [am])
                    pn = pen[ci % 2]
                    A.tsc("dve", pn[:, 0:cw], iota[:, 0:cw], float(c0), qlim[:, b:b + 1], ALU.add, ALU.is_ge, [iota, qlim], [pn])
                    A.stt("dve", Sb[:, c0:c0 + cw], pn[:, 0:cw], -1.0e30, pS[:, 0:cw], ALU.mult, ALU.add, [pn, pS], [Sb])
                lo, w_, mid, cnt, ge, amx = (st[k] for k in ("lo", "w", "mid", "cnt", "ge", "amx"))
                A.S.op("dve", lambda e, i=am[:, 0:nch]: e.tensor_reduce(out=amx[:], in_=i, axis=AX.X, op=ALU.max), [am], [amx])
                A.tsc("dve", lo[:], amx[:], -1.0001, -1e-6, ALU.mult, ALU.add, [amx], [lo])
                A.tsc("dve", w_[:], amx[:], 2.0004, 4e-6, ALU.mult, ALU.add, [amx], [w_])
                for it in range(NITER):
                    A.tsc("dve", w_[:], w_[:], 0.5, None, ALU.mult, None, [w_], [w_])
                    A.tt("dve", mid[:], lo[:], w_[:], ALU.add, [lo, w_], [mid])
                    A.tsc("dve", mask[:, 0:L], Sb[:, 0:L], mid[:, 0:1], 0.0, ALU.is_ge, ALU.add, [Sb, mid], [mask, cnt], accum_out=cnt[:, 0:1])
                    A.tsc("dve", ge[:], cnt[:], 255.5, None, ALU.is_ge, None, [cnt], [ge])
                    A.stt("dve", lo[:], ge[:], w_[:, 0:1], lo[:], ALU.mult, ALU.add, [ge, w_, lo], [lo])
                A.tsc("dve", mask[:, 0:L], Sb[:, 0:L], lo[:, 0:1], None, ALU.is_ge, None, [Sb, lo], [mask])
                po = [A.next_ps(), A.next_ps()]
                pr = [A.next_ps(), A.next_ps()]
                acc = po + pr
                nkt = L // 128
                for hg in range(2):
                    A.mm(po[hg][:], zeros[:, 0:128], zeros[:], True, False, [zeros], [po[hg]])
                for ci in range(nch):
                    c0 = ci * 512
                    cw = min(512, L - c0)
                    k_, v_ = kc[ci % 2], vc[ci % 2]
                    A.dma("sp", k_[:, :, 0:cw], kT_view[:, :, c0:c0 + cw], k_, [A.kbT], [k_])
                    A.dma("act", v_[:, 0:cw // 128, :], A.vb.h[c0:c0 + cw, :].rearrange("(t p) n -> p t n", p=128), v_, [A.vb], [v_])
                    pmk = A.ps_free(acc)
                    pmv = pmk[:].bitcast(BF16).rearrange("p (a b) -> p a b", a=8)
                    for j in range(cw // 128):
                        A.tr(pmv[:, j, :], mask[:, c0 + j * 128:c0 + (j + 1) * 128], A.ident[:], [mask, A.ident], [pmk])
                    for j in range(cw // 128):
                        kt = ci * 4 + j
                        first, last = kt == 0, kt == nkt - 1
                        for hg in range(2):
                            ps = A.ps_free(acc + [pmk])
                            for hh in range(4):
                                h = hg * 4 + hh
                                A.mm(ps[:, hh * 128:(hh + 1) * 128], k_[:, h, j * 128:(j + 1) * 128], qq[:, h, :], True, True, [k_, qq], [ps])
                            pe1, pm1 = pe_[(kt * 2 + hg) % 2], pm[(kt * 2 + hg) % 2]
                            A.act(pe1[:], ps[:], AF.Exp, [ps], [pe1], scale=SCD)
                            A.tt("dve", pm1[:].rearrange("p (h q) -> p h q", h=4), pe1[:].rearrange("p (h q) -> p h q", h=4),
                                 pmv[:, j, :].unsqueeze(1).broadcast_to([128, 4, 128]), ALU.mult, [pe1, pmk], [pm1])
                            for hh in range(4):
                                h = hg * 4 + hh
                                A.mm(po[hg][:, hh * 128:(hh + 1) * 128], v_[:, j, h * 128:(h + 1) * 128], pm1[:, hh * 128:(hh + 1) * 128],
                                     False, last and hh == 3, [v_, pm1], [po[hg]])
                            A.mm(pr[hg][:], ones[:], pm1[:], first, last, [ones, pm1], [pr[hg]])
                o_ = ost[bi % 2]
                for hg in range(2):
                    A.S.op("dve", lambda e, p_=pr[hg][:]: e.reciprocal(out=rinv[:], in_=p_), [pr[hg]], [rinv])
                    A.tt("dve", o_[:, hg * 4:(hg + 1) * 4, :], po[hg][:].rearrange("p (h q) -> p h q", h=4),
                         rinv[:].rearrange("p (h q) -> p h q", h=4), ALU.mult, [po[hg], rinv], [o_])
                A.dma("pool", A.odsaT.h.rearrange("h d t -> d h t")[:, :, b * 128:(b + 1) * 128], o_[:], o_, [o_], [A.odsaT])

    def phase_p5(self):
        A, nc, cfg = self, self.nc, self.cfg
        A.omemT = A.dscratch("omemT", [8, 128, NQ])
        SCM = float(256 ** -0.5)
        ttiles = [(t0, min(512, NQ - t0)) for t0 in range(0, NQ, 512)]
        with contextlib.ExitStack() as es:
            mkT = A.sb(es, "mkT", [128, 8, 256], BF16)
            mv = A.sb(es, "mv", [128, 2, 1024], BF16)
            ones = A.sb(es, "ones5", [128, 128], BF16)
            A.S.op("pool", lambda e: e.memset(ones[:], 1.0), [], [ones])
            with contextlib.ExitStack() as es2:
                A.S.begin_const()
                gb = A.sb(es2, "gbm", [128, D], F32, const=True)
                A.dma("sp", gb[:], A.mem_norm.broadcast_to([128, D]), gb, [], [gb])
                A.tsc("dve", gb[:], gb[:], float(np.sqrt(D)), None, ALU.mult, None, [gb], [gb])
                gk = A.sb(es2, "gk", [128, 256], F32, const=True)
                A.dma("sp", gk[:], A.mem_k_norm.broadcast_to([128, 256]), gk, [], [gk])
                A.tsc("dve", gk[:], gk[:], float(np.sqrt(256.0)), None, ALU.mult, None, [gk], [gk])
                A.S.end_const()
                xt = A.sb(es2, "xm", [128, D], F32)
                ss = A.sb(es2, "ssm", [128, 1], F32)
                rs = A.sb(es2, "rsm", [128, 1], F32)
                h = A.sb(es2, "hm", [128, D], BF16)
                mhT = A.sb(es2, "mhT", [128, 16, 256], BF16)
                for b in range(2):
                    A.dma("sp", xt[:], A.mem[128 * b:128 * (b + 1), :], xt, [], [xt])
                    A.norm_tile(xt, gb, h, ss, rs, h, D, [])
                    A.transpose_to(mhT, b * 128, h)
                Wb = [A.sb(es2, f"Wm{i}", [128, 16, 512], BF16) for i in range(2)]
                stage = [A.sb(es2, f"wstm{i}", [128, 16, 128], F32) for i in range(2)]
                sq = A.sb(es2, "sqm", [128, 512], F32)
                ssq = A.sb(es2, "ssqm", [128, 2], F32)
                rs2 = A.sb(es2, "rs2m", [128, 2], F32)
                yraw = A.sb(es2, "yrawm", [128, 512], F32)
                y = A.sb(es2, "ym", [128, 512], F32)
                yb = A.sb(es2, "ybm", [128, 512], BF16)
                for blk in range(4):
                    w = Wb[blk % 2]
                    A.load_w_bf16(w, 0, A.w_mem_kv, blk * 512, 512, stage)
                    for b in range(2):
                        p = A.next_ps()
                        for c in range(16):
                            A.mm(p[:], mhT[:, c, b * 128:(b + 1) * 128], w[:, c, :], c == 0, c == 15, [w, mhT], [p])
                        if blk < 2:
                            A.act(sq[:], p[:], AF.Square, [p], [sq])
                            A.S.op("dve", lambda e, o=ssq[:, 0:2], i=sq[:].rearrange("p (h d) -> p h d", h=2):
                                   e.tensor_reduce(out=o, in_=i, axis=AX.X, op=ALU.add), [sq], [ssq])
                            A.cp("dve", yraw[:], p[:], [p], [yraw])
                            A.rsqrt(rs2[:], ssq[:], 3, rs2, ssq)
                            y3 = y[:].rearrange("p (h d) -> p h d", h=2)
                            A.tt("dve", y3, yraw[:].rearrange("p (h d) -> p h d", h=2),
                                 rs2[:].unsqueeze(2).broadcast_to([128, 2, 256]), ALU.mult, [yraw, rs2], [y])
                            A.tt("pool", yb[:].rearrange("p (h d) -> p h d", h=2), y3,
                                 gk[:].unsqueeze(1).broadcast_to([128, 2, 256]), ALU.mult, [y, gk], [yb])
                            p2 = A.next_ps()
                            pv = p2[:].bitcast(BF16).rearrange("p (a b) -> p a b", a=8)
                            for hh in range(4):
                                A.tr(pv[:, hh, :], yb[:, hh * 128:(hh + 1) * 128], A.ident[:], [yb, A.ident], [p2])
                            A.evac(mkT[:, blk * 4:(blk + 1) * 4, b * 128:(b + 1) * 128], pv[:, 0:4, :], [p2], [mkT])
                        else:
                            A.evac(mv[:, b, (blk - 2) * 512:(blk - 1) * 512], p[:], [p], [mv])
                A.S.barrier()
            qm = [A.sb(es, f"qm{i}", [128, 2, NQ], BF16) for i in range(2)]
            pT = [A.sb(es, f"pTm{i}", [128, 512], BF16) for i in range(4)]
            rinv = A.sb(es, "rinvm", [128, 512], F32)
            ost = [A.sb(es, f"ostm{i}", [128, 2, NQ], BF16) for i in range(2)]
            n = 0
            for hd in range(4):
                q_, o_ = qm[hd % 2], ost[hd % 2]
                A.dma("sp", q_[:], A.qmT.h[2 * hd:2 * hd + 2].rearrange("h d t -> d h t"), q_, [A.qmT], [q_])
                for (t0, tw) in ttiles:
                    po = [A.next_ps(), A.next_ps()]
                    pr = A.next_ps()
                    acc = po + [pr]
                    for mb in range(2):
                        ps = A.ps_free(acc)
                        for dc in range(2):
                            A.mm(ps[:, 0:tw], mkT[:, hd * 2 + dc, mb * 128:(mb + 1) * 128], q_[:, dc, t0:t0 + tw], dc == 0, dc == 1, [mkT, q_], [ps])
                        pt = pT[n % 4]
                        n += 1
                        A.act(pt[:, 0:tw], ps[:, 0:tw], AF.Exp, [ps], [pt], scale=SCM)
                        for dc in range(2):
                            A.mm(po[dc][:, 0:tw], mv[:, mb, hd * 256 + dc * 128: hd * 256 + (dc + 1) * 128], pt[:, 0:tw], mb == 0, mb == 1, [mv, pt], [po[dc]])
                        A.mm(pr[:, 0:tw], ones[:], pt[:, 0:tw], mb == 0, mb == 1, [ones, pt], [pr])
                    A.S.op("dve", lambda e, tw=tw, pr=pr: e.reciprocal(out=rinv[:, 0:tw], in_=pr[:, 0:tw]), [pr], [rinv])
                    for dc in range(2):
                        A.tt("dve", o_[:, dc, t0:t0 + tw], po[dc][:, 0:tw], rinv[:, 0:tw], ALU.mult, [po[dc], rinv], [o_])
                A.dma("pool", A.omemT.h[2 * hd:2 * hd + 2].rearrange("h d t -> d h t"), o_[:], o_, [o_], [A.omemT])

    def phase_p6(self):
        A, nc, cfg = self, self.nc, self.cfg
        A.mergedT = A.dscratch("mergedT", [128, 16, NQ])
        A.x1 = A.dscratch("x1", [NQ, D], F32)
        A.h2T = A.dscratch("h2T", [128, 16, NQ])
        subt = [(0, 512), (512, 512), (1024, 128)]
        srcs = (A.osbT, A.odsaT, A.omemT)
        with contextlib.ExitStack() as es:
            A.S.begin_const()
            bg = A.sb(es, "bg", [128, 48], F32, const=True)
            A.dma("sp", bg[:], A.bg_pt[:, :], bg, [], [bg])
            A.S.end_const()
            oT = [A.sb(es, f"oT{i}", [128, 8, QW], BF16) for i in range(3)]
            hq = A.sb(es, "hq6", [128, 16, QW], BF16)
            mT = A.sb(es, "mT", [128, 16, QW], BF16)
            Wbr = [[A.sb(es, f"Wbr{i}_{j}", [128, 8, 128], BF16) for i in range(3)] for j in range(2)]
            Wg = [[A.sb(es, f"Wg{i}_{j}", [128, 16, 128], BF16) for i in range(3)] for j in range(2)]
            stage = [A.sb(es, f"wst6{i}", [128, 16, 128], F32) for i in range(2)]
            gsb = [A.sb(es, f"gsb{i}", [128, 512], F32) for i in range(2)]
            macc = [A.sb(es, f"macc{i}", [128, 512], F32) for i in range(2)]
            tmp = [A.sb(es, f"tmp6{i}", [128, 512], F32) for i in range(2)]
            n = 0
            for slot in range(2):
                q0 = slot * QW
                for i in range(3):
                    A.dma("sp" if i % 2 else "act", oT[i][:], srcs[i].h.rearrange("h d t -> d h t")[:, :, q0:q0 + QW], oT[i], [srcs[i]], [oT[i]])
                A.dma("sp", hq[:], A.hTq.h[:, :, q0:q0 + QW], hq, [A.hTq], [hq])
                for ft in range(16):
                    j = (slot * 16 + ft) % 2
                    for i in range(3):
                        A.load_w_bf16(Wbr[j][i], 0, A.w_branch[i], ft * 128, 128, stage)
                        A.load_w_bf16(Wg[j][i], 0, A.w_in, 7752 + i * 2048 + ft * 128, 128, stage)
                    for (t0, tw) in subt:
                        mc = macc[n % 2]
                        for i in range(3):
                            pb = A.next_ps()
                            for c in range(8):
                                A.mm(pb[:, 0:tw], Wbr[j][i][:, c, :], oT[i][:, c, t0:t0 + tw], c == 0, c == 7, [Wbr[j][i], oT[i]], [pb])
                            pg = A.next_ps()
                            for c in range(16):
                                A.mm(pg[:, 0:tw], Wg[j][i][:, c, :], hq[:, c, t0:t0 + tw], c == 0, c == 15, [Wg[j][i], hq], [pg])
                            g_ = gsb[(n * 3 + i) % 2]
                            A.act(g_[:, 0:tw], pg[:, 0:tw], AF.Sigmoid, [pg, bg], [g_], bias=bg[:, i * 16 + ft:i * 16 + ft + 1])
                            if i == 0:
                                A.tt("dve", mc[:, 0:tw], g_[:, 0:tw], pb[:, 0:tw], ALU.mult, [g_, pb], [mc])
                            else:
                                t_ = tmp[i % 2]
                                A.tt("dve", t_[:, 0:tw], g_[:, 0:tw], pb[:, 0:tw], ALU.mult, [g_, pb], [t_])
                                if i == 1:
                                    A.tt("pool", mc[:, 0:tw], mc[:, 0:tw], t_[:, 0:tw], ALU.add, [mc, t_], [mc])
                                else:
                                    A.tt("pool", mT[:, ft, t0:t0 + tw], mc[:, 0:tw], t_[:, 0:tw], ALU.add, [mc, t_], [mT])
                        n += 1
                A.dma("pool", A.mergedT.h[:, :, q0:q0 + QW], mT[:], mT, [mT], [A.mergedT])
            A.S.barrier()
        with contextlib.ExitStack() as es:
            Wout = A.sb(es, "Wout", [128, 16, D], BF16)
            with contextlib.ExitStack() as es2:
                stage = [A.sb(es2, f"wst6b{i}", [128, 16, 256], F32) for i in range(2)]
                A.load_w_bf16(Wout, 0, A.w_out, 0, D, stage)
                A.S.barrier()
            A.S.begin_const()
            gbf = A.sb(es, "gbf", [128, D], F32, const=True)
            A.dma("sp", gbf[:], A.ffn_norm.broadcast_to([128, D]), gbf, [], [gbf])
            A.tsc("dve", gbf[:], gbf[:], float(np.sqrt(D)), None, ALU.mult, None, [gbf], [gbf])
            A.S.end_const()
            mTt = [A.sb(es, f"mTt{i}", [128, 16, 128], BF16) for i in range(2)]
            xt = [A.sb(es, f"x6{i}", [128, D], F32) for i in range(2)]
            x1t = [A.sb(es, f"x1t{i}", [128, D], F32) for i in range(2)]
            ss = [A.sb(es, f"ss6{i}", [128, 1], F32) for i in range(2)]
            rs = [A.sb(es, f"rs6{i}", [128, 1], F32) for i in range(2)]
            h2 = [A.sb(es, f"h26{i}", [128, D], BF16) for i in range(2)]
            h2t = [A.sb(es, f"h2t{i}", [128, 16, 128], BF16) for i in range(2)]
            for b in range(NQ // 128):
                m_, x_, x1_, h_, ht_ = mTt[b % 2], xt[b % 2], x1t[b % 2], h2[b % 2], h2t[b % 2]
                A.dma("sp", m_[:], A.mergedT.h[:, :, b * 128:(b + 1) * 128], m_, [A.mergedT], [m_])
                A.dma("act", x_[:], A.x_q[b * 128:(b + 1) * 128, :], x_, [], [x_])
                for ct in range(4):
                    p = A.next_ps()
                    for ft in range(16):
                        A.mm(p[:], m_[:, ft, :], Wout[:, ft, ct * 512:(ct + 1) * 512], ft == 0, ft == 15, [m_, Wout], [p])
                    A.tt("dve", x1_[:, ct * 512:(ct + 1) * 512], p[:], x_[:, ct * 512:(ct + 1) * 512], ALU.add, [p, x_], [x1_])
                A.dma("pool", A.x1.h[b * 128:(b + 1) * 128, :], x1_[:], x1_, [x1_], [A.x1])
                A.norm_tile(x1_, gbf, h_, ss[b % 2], rs[b % 2], h_, D, [])
                A.transpose_to(ht_, 0, h_)
                A.dma("pool", A.h2T.h[:, :, b * 128:(b + 1) * 128], ht_[:], ht_, [ht_], [A.h2T])

    def phase_p7(self):
        A, nc, cfg = self, self.nc, self.cfg
        NFF = D_FF // 128
        with contextlib.ExitStack() as es:
            actT = A.sb(es, "actT", [128, NFF, SEG], BF16)
            for slot in range(2):
                q0 = slot * QW
                with contextlib.ExitStack() as es2:
                    h2 = A.sb(es2, "h2seg", [128, 16, QW], BF16)
                    A.dma("sp", h2[:], A.h2T.h[:, :, q0:q0 + QW], h2, [A.h2T], [h2])
                    A.S.begin_const()
                    fl = A.sb(es2, "flag", [128, 2], F32, const=True)
                    A.dma("sp", fl[:], A.flags.broadcast_to([128, 2]), fl, [], [fl])
                    cwb = A.sb(es2, "cwb", [128, 2 * NFF, 4], F32, const=True)
                    A.dma("sp", cwb[:], A.cwb[:, :, :], cwb, [], [cwb])
                    A.S.end_const()
                    A.tsc("dve", h2[:, :, 126:128], h2[:, :, 126:128], fl[:, slot:slot + 1], None, ALU.mult, None, [h2, fl], [h2])
                    W = [A.sb(es2, f"Wup{i}", [128, 16, 128], BF16) for i in range(4)]
                    stage = [A.sb(es2, f"wst7{i}", [128, 16, 128], F32) for i in range(2)]
                    pre = [A.sb(es2, f"pre{i}", [128, SEG + 2], F32) for i in range(2)]
                    u = [A.sb(es2, f"u{i}", [128, SEG], F32) for i in range(2)]
                    sg = A.sb(es2, "sg", [128, SEG], F32)
                    n = 0
                    for i in range(NFF):
                        for which in range(2):
                            tile = which * NFF + i
                            w = W[n % 4]
                            pr_, u_ = pre[n % 2], u[n % 2]
                            n += 1
                            A.load_w_bf16(w, 0, A.w_up, tile * 128, 128, stage)
                            ph = A.next_ps()
                            for c in range(16):
                                A.mm(ph[:, 0:2], w[:, c, :], h2[:, c, 126:128], c == 0, c == 15, [w, h2], [ph])
                            A.evac(pr_[:, 0:2], ph[:, 0:2], [ph], [pr_])
                            for t0 in (0, 512):
                                p = A.next_ps()
                                for c in range(16):
                                    A.mm(p[:], w[:, c, :], h2[:, c, 128 + t0:128 + t0 + 512], c == 0, c == 15, [w, h2], [p])
                                A.evac(pr_[:, 2 + t0:2 + t0 + 512], p[:], [p], [pr_])
                            A.tsc("dve", u_[:], pr_[:, 0:SEG], cwb[:, tile, 0:1], cwb[:, tile, 3:4], ALU.mult, ALU.add, [pr_, cwb], [u_])
                            A.stt("dve", u_[:], pr_[:, 1:SEG + 1], cwb[:, tile, 1:2], u_[:], ALU.mult, ALU.add, [pr_, cwb, u_], [u_])
                            A.stt("dve", u_[:], pr_[:, 2:SEG + 2], cwb[:, tile, 2:3], u_[:], ALU.mult, ALU.add, [pr_, cwb, u_], [u_])
                            if which == 0:
                                A.act(sg[:], u_[:], AF.Silu, [u_], [sg])
                            else:
                                A.tt("pool", actT[:, i, :], sg[:], u_[:], ALU.mult, [sg, u_], [actT])
                    A.S.barrier()
                with contextlib.ExitStack() as es2:
                    Wd = A.sb(es2, "Wd", [128, NFF, 512], BF16)
                    stage = [A.sb(es2, f"wst7d{i}", [128, NFF, 128], F32) for i in range(2)]
                    xt = [A.sb(es2, f"x7{i}", [128, 512], F32) for i in range(2)]
                    ot = [A.sb(es2, f"o7{i}", [128, 512], F32) for i in range(2)]
                    n = 0
                    for ct in range(4):
                        A.load_w_bf16(Wd, 0, A.w_down, ct * 512, 512, stage)
                        for tb in range(SEG // 128):
                            row0 = q0 + 128 + tb * 128
                            x_, o_ = xt[n % 2], ot[n % 2]
                            n += 1
                            A.dma("act", x_[:], A.x1.h[row0:row0 + 128, ct * 512:(ct + 1) * 512], x_, [A.x1], [x_])
                            p = A.next_ps()
                            for i in range(NFF):
                                A.mm(p[:], actT[:, i, tb * 128:(tb + 1) * 128], Wd[:, i, :], i == 0, i == NFF - 1, [actT, Wd], [p])
                            A.tt("dve", o_[:], p[:], x_[:], ALU.add, [p, x_], [o_])
                            orow = slot * SEG + tb * 128
                            A.dma("pool", A.out[orow:orow + 128, ct * 512:(ct + 1) * 512], o_[:], o_, [o_], [])
                    A.S.barrier()


def core_tokens(c):
    sA, sB = c, 15 - c
    hA = np.arange(SEG * sA - 128, SEG * sA) if sA > 0 else np.arange(0, 128)
    hB = np.arange(SEG * sB - 128, SEG * sB)
    idx = np.concatenate([hA, np.arange(SEG * sA, SEG * (sA + 1)), hB, np.arange(SEG * sB, SEG * (sB + 1))])
    flags = np.array([[1.0 if sA > 0 else 0.0, 1.0]], np.float32)
    return idx, flags


def host_inputs(inp, c, seq=SEQ):
    f = np.float32
    idx, flags = core_tokens(c)
    idx = np.minimum(idx, seq - 1) if seq < SEQ else idx
    x = inp["x"][0]
    pos = inp["positions"][0]
    m = {}
    m["x_all"] = np.ascontiguousarray(x[:seq])
    pp = np.zeros((128, 128), np.int32)
    pp[:, :seq // 128] = pos[:seq].reshape(-1, 128).T
    m["pos_pt"] = pp
    m["x_q"] = np.ascontiguousarray(x[idx])
    m["posq_pt"] = np.ascontiguousarray(pos[idx].reshape(-1, 128).T.astype(np.int32))
    m["qidx_row"] = idx.astype(f)[None, :]
    m["qidx_pt"] = np.ascontiguousarray(idx.astype(f).reshape(-1, 128).T)
    m["qlim_pt"] = np.ascontiguousarray(((idx // 64 + 1) * 64).astype(f).reshape(-1, 128).T)
    kk = np.arange(SEQ, dtype=f)
    m["kidx_pt"] = np.ascontiguousarray(kk.reshape(-1, 128).T)
    m["kidx_row"] = kk[None, :seq].copy()
    m["flags"] = flags
    m["c_ident"] = np.eye(128, dtype=f)
    m["c_tri"] = np.tril(np.ones((128, 128), f))
    m["c_inv64"] = (10000.0 ** (-np.arange(64) / 64.0) / (2 * np.pi)).astype(f)[None, :]
    m["mem"] = np.ascontiguousarray(inp["mem"][0])
    m["bg_pt"] = np.ascontiguousarray(inp["b_gate"].reshape(48, 128).T)
    cw = np.concatenate([inp["conv_w"][0], inp["conv_b"].reshape(1, -1)], 0)
    m["cwb"] = np.ascontiguousarray(cw.reshape(4, 86, 128).transpose(2, 1, 0))
    for k in ("attn_norm", "mem_norm", "b_gate", "dsa_q_norm", "dsa_k_norm", "mem_q_norm", "mem_k_norm",
              "ffn_norm", "conv_b"):
        m[k] = np.ascontiguousarray(inp[k]).reshape(1, -1)
    for k in ("w_in", "w_mem_kv", "w_branch", "w_out", "w_up", "conv_w", "w_down"):
        m[k] = np.ascontiguousarray(inp[k][0])
    return m


def full_cfg():
    return dict(phases=["p1a", "p1b", "p2", "p3", "p4", "p5", "p6", "p7"])


def kernel(**inputs):
    inp = {k: np.asarray(v) for k, v in inputs.items()}
    B = Builder(full_cfg())
    nc = B.build()
    in_maps = [host_inputs(inp, c) for c in range(NCORE)]
    res = run_bass_kernel_spmd(nc, in_maps, core_ids=list(range(NCORE)))
    out = np.zeros((1, SEQ, D), np.float32)
    for c in range(NCORE):
        o = np.asarray(res.results[c]["out"], np.float32)
        out[0, SEG * c:SEG * (c + 1)] = o[:SEG]
        out[0, SEG * (15 - c):SEG * (16 - c)] = o[SEG:]
    return out
```

```python
import contextlib
import numpy as np
import concourse.bass as bass
import concourse.mybir as mybir
from concourse.bass_utils import run_bass_kernel_spmd

F32 = mybir.dt.float32
BF16 = mybir.dt.bfloat16
I32 = mybir.dt.int32
AF = mybir.ActivationFunctionType
ALU = mybir.AluOpType
AX = mybir.AxisListType

D = 2048
SEQ = 16384
D_IN = 13896
D_FF = 5504
EPS = 1e-6
NCORE = 8
SEG = 1024
QW = 128 + SEG
NQ = 2 * QW

ENGS = ["pe", "act", "dve", "pool", "sp"]
EPOCH = 30000
ATTACH_WAITS = True
SAME_ENGINE_INORDER = ("pe",)


class Buf:
    __slots__ = ("name", "writes", "reads", "dsem", "ndma", "fresh", "qs", "swq", "grp")

    def __init__(self, name):
        self.name = name
        self.writes = {}
        self.reads = {}
        self.dsem = None
        self.ndma = 0
        self.fresh = False
        self.qs = set()
        self.swq = False
        self.grp = None


class TT:
    def __init__(self, h, name):
        self.h = h
        self.b = Buf(name)

    def __getitem__(self, k):
        return self.h[k]


class Sched:
    def __init__(self, nc, sems):
        self.nc = nc
        self.free_sems = list(sems)
        self.stream = {e: [] for e in ENGS}
        self.cnt = {e: 0 for e in ENGS}
        self.esems = {e: [] for e in ENGS}
        self.waited = {e: {} for e in ENGS}
        self.dtok = {}
        self.recycled = []
        self.dead_keys = set()
        self.grp_cur = {}
        self.snap = {e: [] for e in ENGS}
        self.dsnap = {}
        self.deferred = None
        self.cgrp = Buf("constgroup")
        self.free_sw = []
        self.free_hw = []
        self.sem_inc = {}
        self.barbufs = [Buf("bar_" + e) for e in ENGS]
        self.nops = 0

    def _sem_of(self, key):
        if key[0] == "e":
            lst = self.esems[key[1]]
            while len(lst) <= key[2]:
                lst.append(self.free_sems.pop())
            return lst[key[2]]
        return key[1]

    def _need(self, eng, deps):
        w = self.waited[eng]
        for key, val in sorted(deps.items(), key=lambda kv: -kv[1]):
            if w.get(key, 0) >= val:
                continue
            w[key] = val
            self.stream[eng].append(("wait", self._sem_of(key), val))
            if key[0] == "e":
                sn = self.snap[key[1]][key[2] * EPOCH + val - 1]
            else:
                sn = self.dsnap.get((key, val))
            if sn:
                for k2, v2 in sn.items():
                    if w.get(k2, 0) < v2:
                        w[k2] = v2

    def _collect(self, reads, writes, eng):
        deps = {}
        for b in reads:
            for k, v in b.writes.items():
                if eng == "pe" and k[0] == "e" and k[1] == "pe":
                    continue
                if deps.get(k, 0) < v:
                    deps[k] = v
        for b in writes:
            for src in (b.writes, b.reads):
                for k, v in src.items():
                    if eng in SAME_ENGINE_INORDER and k[0] == "e" and k[1] == eng:
                        continue
                    if deps.get(k, 0) < v:
                        deps[k] = v
        for k in deps:
            if k in self.grp_cur:
                deps[k] = self.grp_cur[k]
        return deps

    deferred_started = False

    def begin_const(self):
        self.deferred = []
        self.deferred_started = False

    def end_const(self):
        d, self.deferred = self.deferred, None
        for a in d:
            self.op(*a)

    def op(self, eng, fn, reads=(), writes=()):
        if self.deferred is not None:
            self.deferred.append((eng, fn, reads, writes))
            return
        reads = [r.b if isinstance(r, TT) else r for r in reads]
        writes = [r.b if isinstance(r, TT) else r for r in writes]
        self._need(eng, self._collect(reads, writes, eng))
        self.cnt[eng] += 1
        c = self.cnt[eng]
        key = ("e", eng, (c - 1) // EPOCH)
        val = (c - 1) % EPOCH + 1
        self.stream[eng].append(("op", fn, self._sem_of(key)))
        self.snap[eng].append(dict(self.waited[eng]))
        self.nops += 1
        for b in reads:
            if b.reads.get(key, 0) < val:
                b.reads[key] = val
        for b in writes:
            b.writes = {key: val}
            b.reads = {}

    def dma(self, q, out_ap, in_ap, sb, reads=(), writes=()):
        reads = [r.b if isinstance(r, TT) else r for r in reads]
        writes = [r.b if isinstance(r, TT) else r for r in writes]
        sb = sb.b if isinstance(sb, TT) else sb
        self._need(q, self._collect(reads, writes, None))
        own = sb.grp if sb.grp is not None else sb
        if own.dsem is None:
            own.dsem = self.free_sems.pop()
        if sb.grp is not None:
            assert self.deferred is not None, "constant loads must be inside begin_const/end_const"
            if own.ndma:
                self._need(q, {("d", own.dsem, 0): 16 * own.ndma} if not self.deferred_started else {})
                self.deferred_started = True
        own.ndma += 1
        key = ("d", own.dsem, 0)
        val = 16 * own.ndma
        self.dtok[key] = val
        if sb.grp is not None:
            self.grp_cur[key] = val
        self.stream[q].append(("dma", out_ap, in_ap, own.dsem))
        self.dsnap[(key, val)] = dict(self.waited[q])
        self.nops += 1
        for b in reads:
            if b.reads.get(key, 0) < val:
                b.reads[key] = val
        for b in writes:
            keep = {k: v for k, v in b.writes.items() if k[0] == "d"}
            keep[key] = val
            b.writes = keep
            b.reads = {}

    def _all_tokens(self):
        deps = dict(self.dtok)
        for e in ENGS:
            c = self.cnt[e]
            if c:
                deps[("e", e, (c - 1) // EPOCH)] = (c - 1) % EPOCH + 1
        return deps

    def barrier(self):
        deps = self._all_tokens()
        for e in ENGS:
            self._need(e, deps)

    def recycle(self, b):
        return

    def emit(self):
        nc = self.nc
        names = {"pe": "tensor", "act": "scalar", "dve": "vector", "pool": "gpsimd", "sp": "sync"}
        with nc.Block() as block:
            def run(name):
                def body(e):
                    pend = None
                    for it in self.stream[name]:
                        if it[0] == "wait":
                            if pend is not None:
                                e.wait_ge(pend[0], pend[1])
                            pend = (it[1], it[2])
                            continue
                        if pend is not None and not ATTACH_WAITS:
                            e.wait_ge(pend[0], pend[1])
                            pend = None
                        if it[0] == "op":
                            ins = it[1](e)
                        else:
                            ins = e.dma_start(out=it[1], in_=it[2])
                        if pend is not None:
                            ins._wait_ge(pend[0], pend[1])
                            pend = None
                        ins.then_inc(it[-1], 1 if it[0] == "op" else 16)
                    if pend is not None:
                        e.wait_ge(pend[0], pend[1])
                return body
            for name in ENGS:
                if self.stream[name]:
                    getattr(block, names[name])(run(name))
        self.stream = {e: [] for e in ENGS}


class Builder:
    def __init__(self, cfg):
        self.cfg = cfg
        self.nc = bass.Bass("TRN2", target_bir_lowering=False)
        self.rr = 0
        self.SEQ = cfg.get("seq", SEQ)

    def din(self, name, shape, dt=F32):
        return self.nc.dram_tensor(name, list(shape), dt, kind="ExternalInput").ap()

    def dscratch(self, name, shape, dt=BF16):
        kind = "ExternalOutput" if name in self.cfg.get("debug_out", ()) else "Internal"
        t = self.nc.dram_tensor(name, list(shape), dt, kind=kind).ap()
        return TT(t, name)

    def sb(self, es, name, shape, dt, const=False):
        self.uid = getattr(self, "uid", 0) + 1
        name = f"{name}_{self.uid}"
        t = TT(es.enter_context(self.nc.sbuf_tensor(name, list(shape), dt)), name)
        if const:
            t.b.grp = self.S.cgrp
        return t

    def next_ps(self):
        p = self.ps[self.rr % len(self.ps)]
        self.rr += 1
        return p

    def mm(self, out, lhsT, rhs, start, stop, R, W):
        self.S.op("pe", lambda e: e.matmul(out, lhsT, rhs, start=start, stop=stop), R, W)

    def tr(self, out, in_, ident, R, W):
        self.S.op("pe", lambda e: e.transpose(out, in_, ident), R, W)

    def act(self, out, in_, func, R, W, **kw):
        self.S.op("act", lambda e: e.activation(out=out, in_=in_, func=func, **kw), R, W)

    def tsc(self, eng, out, in0, s1, s2, op0, op1, R, W, **kw):
        if op1 is None:
            self.S.op(eng, lambda e: e.tensor_scalar(out=out, in0=in0, scalar1=s1, scalar2=None, op0=op0, **kw), R, W)
        else:
            self.S.op(eng, lambda e: e.tensor_scalar(out=out, in0=in0, scalar1=s1, scalar2=s2, op0=op0, op1=op1, **kw), R, W)

    def tt(self, eng, out, in0, in1, op, R, W):
        self.S.op(eng, lambda e: e.tensor_tensor(out=out, in0=in0, in1=in1, op=op), R, W)

    def stt(self, eng, out, in0, scalar, in1, op0, op1, R, W):
        self.S.op(eng, lambda e: e.scalar_tensor_tensor(out=out, in0=in0, scalar=scalar, in1=in1, op0=op0, op1=op1), R, W)

    def cp(self, eng, out, in_, R, W):
        if eng == "act":
            self.S.op("act", lambda e: e.copy(out=out, in_=in_), R, W)
        else:
            self.S.op(eng, lambda e: e.tensor_copy(out=out, in_=in_), R, W)

    def dma(self, q, out, in_, sbt, R, W):
        self.S.dma(q, out, in_, sbt, R, W)

    def evac(self, out, in_, R, W):
        self.ev = getattr(self, "ev", 0) + 1
        self.cp("act" if self.ev % 2 else "dve", out, in_, R, W)

    def build(self):
        nc, cfg = self.nc, self.cfg
        A = self
        A.x_all = A.din("x_all", [A.SEQ, D])
        A.pos_pt = A.din("pos_pt", [128, 128], I32)
        A.x_q = A.din("x_q", [NQ, D])
        A.posq_pt = A.din("posq_pt", [128, NQ // 128], I32)
        A.qidx_row = A.din("qidx_row", [1, NQ])
        A.qidx_pt = A.din("qidx_pt", [128, NQ // 128])
        A.kidx_pt = A.din("kidx_pt", [128, 128])
        A.kidx_row = A.din("kidx_row", [1, A.SEQ])
        A.flags = A.din("flags", [1, 2])
        A.bg_pt = A.din("bg_pt", [128, 48])
        A.cwb = A.din("cwb", [128, 86, 4])
        A.qlim_pt = A.din("qlim_pt", [128, NQ // 128])
        A.c_ident = A.din("c_ident", [128, 128])
        A.c_tri = A.din("c_tri", [128, 128])
        A.c_inv64 = A.din("c_inv64", [1, 64])
        A.mem = A.din("mem", [256, D])
        A.attn_norm = A.din("attn_norm", [1, D])
        A.mem_norm = A.din("mem_norm", [1, D])
        A.w_in = A.din("w_in", [D, D_IN])
        A.b_gate = A.din("b_gate", [1, 3 * D])
        A.dsa_q_norm = A.din("dsa_q_norm", [1, 128])
        A.dsa_k_norm = A.din("dsa_k_norm", [1, 128])
        A.mem_q_norm = A.din("mem_q_norm", [1, 256])
        A.mem_k_norm = A.din("mem_k_norm", [1, 256])
        A.w_mem_kv = A.din("w_mem_kv", [D, 2048])
        A.w_branch = A.din("w_branch", [3, 1024, D])
        A.w_out = A.din("w_out", [D, D])
        A.ffn_norm = A.din("ffn_norm", [1, D])
        A.w_up = A.din("w_up", [D, 2 * D_FF])
        A.conv_w = A.din("conv_w", [3, 2 * D_FF])
        A.conv_b = A.din("conv_b", [1, 2 * D_FF])
        A.w_down = A.din("w_down", [D_FF, D])
        A.out = nc.dram_tensor("out", [2 * SEG, D], F32, kind="ExternalOutput").ap()
        A.kaT = A.dscratch("kaT", [8, 128, A.SEQ])
        A.va = A.dscratch("va", [A.SEQ, 1024])
        A.kbT = A.dscratch("kbT", [8, 128, A.SEQ])
        A.vb = A.dscratch("vb", [A.SEQ, 1024])
        A.kiT = A.dscratch("kiT", [64, A.SEQ])

        with contextlib.ExitStack() as top:
            sems = [top.enter_context(nc.semaphore(f"s{i}")) for i in range(101)]
            A.S = Sched(nc, sems)
            A.ps = [TT(top.enter_context(nc.psum_tensor(f"ps{i}", [128, 512], F32)), f"ps{i}") for i in range(8)]
            A.S.begin_const()
            A.ident = A.sb(top, "ident", [128, 128], BF16)
            A.identf = A.sb(top, "identf", [128, 128], F32, const=True)
            A.dma("sp", A.identf[:], A.c_ident[:, :], A.identf, [], [A.identf])
            A.cp("dve", A.ident[:], A.identf[:], [A.identf], [A.ident])
            A.cst = A.sb(top, "cst", [128, 4], F32)
            for j, v in enumerate((-float(np.pi), D * EPS, 128 * EPS, 256 * EPS)):
                A.S.op("pool", lambda e, j=j, v=v: e.memset(A.cst[:, j:j + 1], float(v)), [], [A.cst])
            A.invt = A.sb(top, "invt", [128, 64], F32, const=True)
            A.dma("sp", A.invt[:], A.c_inv64.broadcast_to([128, 64]), A.invt, [], [A.invt])
            A.S.end_const()
            for ph in cfg["phases"]:
                getattr(A, "phase_" + ph)()
                A.S.barrier()
                A.S.emit()
        return nc

    def load_w_bf16(self, dst, dst_cols, w_ap, col0, ncols, stage):
        A = self
        wv = w_ap.rearrange("(c p) n -> p c n", p=128)
        nchunk = wv.shape[1]
        step = stage[0].h.shape[2]
        i = getattr(A, "_wst", 0)
        for j0 in range(0, ncols, step):
            n = min(step, ncols - j0)
            st = stage[i % len(stage)]
            i += 1
            for cc0 in range(0, nchunk, 16):
                cc1 = min(nchunk, cc0 + 16)
                A.dma("sp" if i % 2 else "act", st[:, cc0:cc1, 0:n], wv[:, cc0:cc1, col0 + j0:col0 + j0 + n], st, [], [st])
            A.cp("pool", dst[:, 0:nchunk, dst_cols + j0:dst_cols + j0 + n], st[:, 0:nchunk, 0:n], [st], [dst])
        A._wst = i

    def rope_tables(self, T, pos4, R, gains=None):
        A = self
        v = lambda t: t[:].rearrange("p (s j) -> p s j", s=4)
        t, ti, tf, fr, frc, c, sn, cs = (T[k] for k in ("t", "ti", "tf", "fr", "frc", "c", "sin", "cos"))
        A.tt("dve", v(t), A.invt[:].unsqueeze(1).broadcast_to([128, 4, 64]),
             pos4.unsqueeze(2).broadcast_to([128, 4, 64]), ALU.mult, list(R) + [A.invt], [t])
        A.cp("dve", ti[:], t[:], [t], [ti])
        A.cp("dve", tf[:], ti[:], [ti], [tf])
        A.tt("dve", fr[:], t[:], tf[:], ALU.subtract, [t, tf], [fr])
        A.tsc("dve", frc[:], fr[:], 0.25, None, ALU.add, None, [fr], [frc])
        A.tsc("dve", c[:], frc[:], 0.5, None, ALU.is_gt, None, [frc], [c])
        A.tt("dve", frc[:], frc[:], c[:], ALU.subtract, [frc, c], [frc])
        A.act(sn[:], fr[:], AF.Sin, [fr], [sn], scale=float(2 * np.pi))
        A.act(cs[:], frc[:], AF.Sin, [frc], [cs], scale=float(2 * np.pi))
        if gains is not None:
            gt, gap = gains
            g1 = gap[:, 0:64].unsqueeze(1).broadcast_to([128, 4, 64])
            g2 = gap[:, 64:128].unsqueeze(1).broadcast_to([128, 4, 64])
            A.tt("pool", v(T["A"]), v(cs), g1, ALU.mult, [cs, gt], [T["A"]])
            A.tt("pool", v(T["B"]), v(sn), g2, ALU.mult, [sn, gt], [T["B"]])
            A.tt("pool", v(T["C"]), v(cs), g2, ALU.mult, [cs, gt], [T["C"]])
            A.tt("pool", v(T["D"]), v(sn), g1, ALU.mult, [sn, gt], [T["D"]])

    def rope_apply(self, out, y, nh, hd, tabs, tabR, R, W, tmp):
        A = self
        half = hd // 2
        tA, tB, tC, tD = tabs
        t1, t2 = tmp
        bc = lambda t: t.unsqueeze(1).broadcast_to([128, nh, half])
        y1, y2 = y[:, :, 0:half], y[:, :, half:hd]
        v1 = t1[:, 0:nh * half].rearrange("p (h d) -> p h d", h=nh)
        v2 = t2[:, 0:nh * half].rearrange("p (h d) -> p h d", h=nh)
        tabR = list(tabR)
        A.tt("dve", v1, y1, bc(tA), ALU.mult, list(R) + tabR, [t1])
        A.tt("pool", v2, y2, bc(tB), ALU.mult, list(R) + tabR, [t2])
        A.tt("dve", out[:, :, 0:half], v1, v2, ALU.subtract, [t1, t2], list(W))
        A.tt("dve", v1, y2, bc(tC), ALU.mult, list(R) + tabR, [t1])
        A.tt("pool", v2, y1, bc(tD), ALU.mult, list(R) + tabR, [t2])
        A.tt("dve", out[:, :, half:hd], v1, v2, ALU.add, [t1, t2], list(W))

    def norm_tile(self, xt, gb, junk, ss, rs, h, ncols_scale, R_x):
        A = self
        A.S.op("pool", lambda e: e.memset(ss[:, 0:1], 0.0), [], [ss])
        A.act(junk[:], xt[:], AF.Square, [xt] + list(R_x), [junk, ss], accum_out=ss[:, 0:1])
        A.rsqrt(rs[:, 0:1], ss[:, 0:1], 1, rs, ss)
        A.stt("dve", h[:], xt[:], rs[:, 0:1], gb[:], ALU.mult, ALU.mult, [xt, rs, gb], [h])

    def rsqrt(self, out, in_, eps_col, out_t, in_t):
        A = self
        A.act(out, in_, AF.Sqrt, [in_t, A.cst], [out_t], bias=A.cst[:, eps_col:eps_col + 1])
        A.S.op("dve", lambda e: e.reciprocal(out=out, in_=out), [out_t], [out_t])

    def transpose_to(self, hT, col0, h, nchunks=16):
        A = self
        for c0 in range(0, nchunks, 8):
            p = A.next_ps()
            pv = p[:].bitcast(BF16).rearrange("p (a b) -> p a b", a=8)
            n = min(8, nchunks - c0)
            for c in range(n):
                A.tr(pv[:, c, :], h[:, (c0 + c) * 128:(c0 + c + 1) * 128], A.ident[:], [h, A.ident], [p])
            A.evac(hT[:, c0:c0 + n, col0:col0 + 128], pv[:, 0:n, :], [p], [hT])

    def phase_p1a(self):
        self.kv_pass("a")

    def phase_p1b(self):
        self.kv_pass("b")

    def kv_pass(self, which):
        A, nc, cfg = self, self.nc, self.cfg
        NT = cfg.get("nt1", A.SEQ // 512)
        isb = which == "b"
        ncols = 2112 if isb else 2048
        with contextlib.ExitStack() as es:
            W = A.sb(es, "W1", [128, 16, ncols], BF16)
            A.S.begin_const()
            gb = A.sb(es, "gb", [128, D], F32, const=True)
            A.dma("sp", gb[:], A.attn_norm.broadcast_to([128, D]), gb, [], [gb])
            A.tsc("dve", gb[:], gb[:], float(np.sqrt(D)), None, ALU.mult, None, [gb], [gb])
            A.S.end_const()
            with contextlib.ExitStack() as es2:
                stage = [A.sb(es2, f"wst{i}", [128, 16, 256], F32) for i in range(2)]
                if isb:
                    A.load_w_bf16(W, 0, A.w_in, 4096, 1024, stage)
                    A.load_w_bf16(W, 1024, A.w_in, 5120, 1024, stage)
                    A.load_w_bf16(W, 2048, A.w_in, 7680, 64, stage)
                else:
                    A.load_w_bf16(W, 0, A.w_in, 1024, 1024, stage)
                    A.load_w_bf16(W, 1024, A.w_in, 2048, 1024, stage)
                A.S.barrier()
            xt = [A.sb(es, f"xt{i}", [128, D], F32) for i in range(2)]
            ss = [A.sb(es, f"ss{i}", [128, 1], F32) for i in range(2)]
            rs = [A.sb(es, f"rs{i}", [128, 1], F32) for i in range(2)]
            h = [A.sb(es, f"h{i}", [128, D], BF16) for i in range(2)]
            hT = [A.sb(es, f"hT{i}", [128, 16, 512], BF16) for i in range(2)]
            kst = [A.sb(es, f"kst{i}", [128, 8, 512], BF16) for i in range(2)]
            vst = [A.sb(es, f"vst{i}", [128, 4, 1024], BF16) for i in range(2)]
            if isb:
                A.S.begin_const()
                posi = A.sb(es, "posi", [128, 128], I32, const=True)
                posf = A.sb(es, "posf", [128, 128], F32)
                A.dma("sp", posi[:], A.pos_pt[:, :], posi, [], [posi])
                A.cp("dve", posf[:], posi[:], [posi], [posf])
                A.gains = A.sb(es, "gains", [128, 128], F32, const=True)
                A.dma("sp", A.gains[:], A.dsa_k_norm.broadcast_to([128, 128]), A.gains, [], [A.gains])
                A.tsc("dve", A.gains[:], A.gains[:], float(np.sqrt(128.0)), None, ALU.mult, None, [A.gains], [A.gains])
                A.S.end_const()
                RT = {k: A.sb(es, "rt_" + k, [128, 256], I32 if k == "ti" else F32)
                      for k in ("t", "ti", "tf", "fr", "frc", "c", "sin", "cos", "A", "B", "C", "D")}
                sq = A.sb(es, "sq", [128, 512], F32)
                ssq = A.sb(es, "ssq", [128, 8], F32)
                rs8 = A.sb(es, "rs8", [128, 8], F32)
                yraw = A.sb(es, "yraw", [128, 1024], F32)
                y = A.sb(es, "y", [128, 1024], F32)
                kbr = A.sb(es, "kbr", [128, 1024], BF16)
                t1 = A.sb(es, "t1", [128, 512], F32)
                t2 = A.sb(es, "t2", [128, 512], F32)
                kiraw = A.sb(es, "kiraw", [128, 64], F32)
                kir = A.sb(es, "kir", [128, 64], BF16)
                kist = [A.sb(es, f"kist{i}", [64, 512], BF16) for i in range(2)]
            kT_dst = (A.kbT if isb else A.kaT)
            v_dst = (A.vb if isb else A.va)
            kT_view = kT_dst.h.rearrange("h d t -> d h t")
            for ti in range(NT):
                tok0 = 512 * ti
                hTt = hT[ti % 2]
                for s in range(4):
                    g = 4 * ti + s
                    x_ = xt[g % 2]
                    A.dma("sp", x_[:], A.x_all[tok0 + 128 * s: tok0 + 128 * (s + 1), :], x_, [], [x_])
                    A.norm_tile(x_, gb, h[g % 2], ss[g % 2], rs[g % 2], h[g % 2], D, [])
                    A.transpose_to(hTt, s * 128, h[g % 2])
                ks = kst[ti % 2]
                vs = vst[ti % 2]
                if not isb:
                    for hh in range(8):
                        p = A.next_ps()
                        for c in range(16):
                            A.mm(p[:], W[:, c, hh * 128:(hh + 1) * 128], hTt[:, c, :], c == 0, c == 15, [W, hTt], [p])
                        A.evac(ks[:, hh, :], p[:], [p], [ks])
                else:
                    A.rope_tables(RT, posf[:, 4 * ti:4 * ti + 4], [posf], (A.gains, A.gains))
                    rv = lambda k, s: RT[k][:, s * 64:(s + 1) * 64]
                    for s in range(4):
                        g = 4 * ti + s
                        for cc in range(2):
                            p = A.next_ps()
                            for c in range(16):
                                A.mm(p[:], hTt[:, c, s * 128:(s + 1) * 128], W[:, c, cc * 512:(cc + 1) * 512], c == 0, c == 15, [W, hTt], [p])
                            A.act(sq[:], p[:], AF.Square, [p], [sq])
                            A.S.op("dve", lambda e, o=ssq[:, cc * 4:(cc + 1) * 4], i=sq[:].rearrange("p (h d) -> p h d", h=4):
                                   e.tensor_reduce(out=o, in_=i, axis=AX.X, op=ALU.add), [sq], [ssq])
                            A.cp("dve", yraw[:, cc * 512:(cc + 1) * 512], p[:], [p], [yraw])
                        A.rsqrt(rs8[:], ssq[:], 2, rs8, ssq)
                        y3 = y[:].rearrange("p (h d) -> p h d", h=8)
                        A.tt("dve", y3, yraw[:].rearrange("p (h d) -> p h d", h=8),
                             rs8[:].unsqueeze(2).broadcast_to([128, 8, 128]), ALU.mult, [yraw, rs8], [y])
                        kb3 = kbr[:].rearrange("p (h d) -> p h d", h=8)
                        A.rope_apply(kb3, y3, 8, 128, [rv(k, s) for k in "ABCD"], [RT[k] for k in "ABCD"], [y], [kbr], (t1, t2))
                        p = A.next_ps()
                        pv = p[:].bitcast(BF16).rearrange("p (a b) -> p a b", a=8)
                        for hh in range(8):
                            A.tr(pv[:, hh, :], kbr[:, hh * 128:(hh + 1) * 128], A.ident[:], [kbr, A.ident], [p])
                        A.evac(ks[:, :, s * 128:(s + 1) * 128], pv, [p], [ks])
                        c32 = RT["cos"][:, s * 64:(s + 1) * 64:2]
                        s32 = RT["sin"][:, s * 64:(s + 1) * 64:2]
                        p = A.next_ps()
                        for c in range(16):
                            A.mm(p[:, 0:64], hTt[:, c, s * 128:(s + 1) * 128], W[:, c, 2048:2112], c == 0, c == 15, [W, hTt], [p])
                        A.cp("act", kiraw[:], p[:, 0:64], [p], [kiraw])
                        A.rope_apply(kir[:].rearrange("p (h d) -> p h d", h=1), kiraw[:].rearrange("p (h d) -> p h d", h=1),
                                     1, 64, [c32, s32, c32, s32], [RT["cos"], RT["sin"]], [kiraw], [kir], (t1, t2))
                        p = A.next_ps()
                        pv = p[:].bitcast(BF16)
                        A.tr(pv[0:64, 0:128], kir[:], A.ident[:], [kir, A.ident], [p])
                        A.evac(kist[ti % 2][:, s * 128:(s + 1) * 128], pv[0:64, 0:128], [p], [kist[ti % 2]])
                    A.dma("pool", A.kiT.h[:, tok0:tok0 + 512], kist[ti % 2][:], kist[ti % 2], [kist[ti % 2]], [A.kiT])
                A.dma("pool", kT_view[:, :, tok0:tok0 + 512], ks[:], ks, [ks], [kT_dst])
                vc0 = 1024
                for s in range(4):
                    for cc in range(2):
                        p = A.next_ps()
                        for c in range(16):
                            A.mm(p[:], hTt[:, c, s * 128:(s + 1) * 128], W[:, c, vc0 + cc * 512: vc0 + (cc + 1) * 512], c == 0, c == 15, [W, hTt], [p])
                        A.evac(vs[:, s, cc * 512:(cc + 1) * 512], p[:], [p], [vs])
                A.dma("pool", v_dst.h[tok0:tok0 + 512, :].rearrange("(s p) n -> p s n", p=128), vs[:], vs, [vs], [v_dst])

    def phase_p2(self):
        A, nc, cfg = self, self.nc, self.cfg
        NB = NQ // 128
        A.hTq = A.dscratch("hTq", [128, 16, NQ])
        A.qaT = A.dscratch("qaT", [8, 128, NQ])
        A.qbT = A.dscratch("qbT", [8, 128, NQ])
        A.qmT = A.dscratch("qmT", [8, 128, NQ])
        A.qiT = A.dscratch("qiT", [8, 64, NQ])
        A.wi = A.dscratch("wi", [NQ, 8], F32)
        ttiles = [(t0, min(512, NQ - t0)) for t0 in range(0, NQ, 512)]
        with contextlib.ExitStack() as es:
            hTq = A.sb(es, "hTq", [128, 16, NQ], BF16)
            Wb = [A.sb(es, f"Wb{i}", [128, 16, 512], BF16) for i in range(2)]
            stage = [A.sb(es, f"wst{i}", [128, 16, 128], F32) for i in range(2)]
            TB = {k: A.sb(es, "qt_" + k, [128, NB * 64], F32) for k in ("sin", "cos", "A", "B", "C", "D")}
            A.S.begin_const()
            gq = A.sb(es, "gq", [128, 128], F32, const=True)
            gm = A.sb(es, "gm", [128, 256], F32, const=True)
            A.dma("sp", gq[:], A.dsa_q_norm.broadcast_to([128, 128]), gq, [], [gq])
            A.tsc("dve", gq[:], gq[:], float(np.sqrt(128.0)), None, ALU.mult, None, [gq], [gq])
            A.dma("sp", gm[:], A.mem_q_norm.broadcast_to([128, 256]), gm, [], [gm])
            A.tsc("dve", gm[:], gm[:], float(np.sqrt(256.0)), None, ALU.mult, None, [gm], [gm])
            with contextlib.ExitStack() as es2:
                posi = A.sb(es2, "posqi", [128, NB], I32, const=True)
                posf = A.sb(es2, "posqf", [128, NB], F32)
                A.dma("sp", posi[:], A.posq_pt[:, :], posi, [], [posi])
                A.cp("dve", posf[:], posi[:], [posi], [posf])
                gb = A.sb(es2, "gbq", [128, D], F32, const=True)
                A.dma("sp", gb[:], A.attn_norm.broadcast_to([128, D]), gb, [], [gb])
                A.tsc("dve", gb[:], gb[:], float(np.sqrt(D)), None, ALU.mult, None, [gb], [gb])
                A.S.end_const()
                RT = {k: A.sb(es2, "rq_" + k, [128, 256], I32 if k == "ti" else F32)
                      for k in ("t", "ti", "tf", "fr", "frc", "c", "sin", "cos", "A", "B", "C", "D")}
                for b0 in range(0, NB, 4):
                    n = min(4, NB - b0)
                    pos4 = posf[:, b0:b0 + 4] if n == 4 else posf[:, NB - 4:NB]
                    bb = b0 if n == 4 else NB - 4
                    A.rope_tables(RT, pos4, [posf], (gq, gq))
                    for k in TB:
                        A.cp("pool", TB[k][:, bb * 64:(bb + 4) * 64], RT[k][:], [RT[k]], [TB[k]])
                xt = [A.sb(es2, f"xq{i}", [128, D], F32) for i in range(2)]
                ss = [A.sb(es2, f"ssq{i}", [128, 1], F32) for i in range(2)]
                rs = [A.sb(es2, f"rsq{i}", [128, 1], F32) for i in range(2)]
                h = [A.sb(es2, f"hq{i}", [128, D], BF16) for i in range(2)]
                for b in range(NB):
                    x_ = xt[b % 2]
                    A.dma("sp", x_[:], A.x_q[128 * b:128 * (b + 1), :], x_, [], [x_])
                    A.norm_tile(x_, gb, h[b % 2], ss[b % 2], rs[b % 2], h[b % 2], D, [])
                    A.transpose_to(hTq, b * 128, h[b % 2])
                A.dma("pool", A.hTq.h[:, :, :], hTq[:], hTq, [hTq], [A.hTq])
                A.S.barrier()
            qst = A.sb(es, "qst", [128, 4, NQ], BF16)
            sq = A.sb(es, "sqq", [128, 512], F32)
            ssq = A.sb(es, "ssqq", [128, 4], F32)
            rs4 = A.sb(es, "rs4", [128, 4], F32)
            yraw = A.sb(es, "yrawq", [128, 512], F32)
            y = A.sb(es, "yq", [128, 512], F32)
            yb = A.sb(es, "ybq", [128, 512], BF16)
            t1 = A.sb(es, "t1q", [128, 256], F32)
            t2 = A.sb(es, "t2q", [128, 256], F32)
            qist = [A.sb(es, f"qist{i}", [64, 8, 128], BF16) for i in range(2)]
            wist = A.sb(es, "wist", [128, NB, 8], F32)
            wcount = [0]

            def load_block(col0, ncols):
                w = Wb[wcount[0] % 2]
                wcount[0] += 1
                A.load_w_bf16(w, 0, A.w_in, col0, ncols, stage)
                return w

            def tok_major(w, ncols, b):
                p = A.next_ps()
                for c in range(16):
                    A.mm(p[:, 0:ncols], hTq[:, c, b * 128:(b + 1) * 128], w[:, c, 0:ncols], c == 0, c == 15, [w, hTq], [p])
                return p

            for blk in range(2):
                w = load_block(blk * 512, 512)
                for hh in range(4):
                    for (t0, tw) in ttiles:
                        p = A.next_ps()
                        for c in range(16):
                            A.mm(p[:, 0:tw], w[:, c, hh * 128:(hh + 1) * 128], hTq[:, c, t0:t0 + tw], c == 0, c == 15, [w, hTq], [p])
                        A.evac(qst[:, hh, t0:t0 + tw], p[:, 0:tw], [p], [qst])
                A.dma("pool", A.qaT.h[4 * blk:4 * blk + 4].rearrange("h d t -> d h t"), qst[:], qst, [qst], [A.qaT])
            for blk in range(2):
                w = load_block(3072 + blk * 512, 512)
                for b in range(NB):
                    p = tok_major(w, 512, b)
                    A.act(sq[:], p[:], AF.Square, [p], [sq])
                    A.S.op("dve", lambda e, o=ssq[:], i=sq[:].rearrange("p (h d) -> p h d", h=4):
                           e.tensor_reduce(out=o, in_=i, axis=AX.X, op=ALU.add), [sq], [ssq])
                    A.cp("dve", yraw[:], p[:], [p], [yraw])
                    A.rsqrt(rs4[:], ssq[:], 2, rs4, ssq)
                    y3 = y[:].rearrange("p (h d) -> p h d", h=4)
                    A.tt("dve", y3, yraw[:].rearrange("p (h d) -> p h d", h=4),
                         rs4[:].unsqueeze(2).broadcast_to([128, 4, 128]), ALU.mult, [yraw, rs4], [y])
                    yb3 = yb[:].rearrange("p (h d) -> p h d", h=4)
                    tabs = [TB[k][:, b * 64:(b + 1) * 64] for k in "ABCD"]
                    A.rope_apply(yb3, y3, 4, 128, tabs, [TB[k] for k in "ABCD"], [y], [yb], (t1, t2))
                    p2 = A.next_ps()
                    pv = p2[:].bitcast(BF16).rearrange("p (a b) -> p a b", a=8)
                    for hh in range(4):
                        A.tr(pv[:, hh, :], yb[:, hh * 128:(hh + 1) * 128], A.ident[:], [yb, A.ident], [p2])
                    A.evac(qst[:, :, b * 128:(b + 1) * 128], pv[:, 0:4, :], [p2], [qst])
                A.dma("pool", A.qbT.h[4 * blk:4 * blk + 4].rearrange("h d t -> d h t"), qst[:], qst, [qst], [A.qbT])
            for blk in range(2):
                w = load_block(6144 + blk * 512, 512)
                for b in range(NB):
                    p = tok_major(w, 512, b)
                    A.act(sq[:], p[:], AF.Square, [p], [sq])
                    A.S.op("dve", lambda e, o=ssq[:, 0:2], i=sq[:].rearrange("p (h d) -> p h d", h=2):
                           e.tensor_reduce(out=o, in_=i, axis=AX.X, op=ALU.add), [sq], [ssq])
                    A.cp("dve", yraw[:], p[:], [p], [yraw])
                    A.rsqrt(rs4[:, 0:2], ssq[:, 0:2], 3, rs4, ssq)
                    y3 = y[:].rearrange("p (h d) -> p h d", h=2)
                    A.tt("dve", y3, yraw[:].rearrange("p (h d) -> p h d", h=2),
                         rs4[:, 0:2].unsqueeze(2).broadcast_to([128, 2, 256]), ALU.mult, [yraw, rs4], [y])
                    A.tt("pool", yb[:].rearrange("p (h d) -> p h d", h=2), y3,
                         gm[:].unsqueeze(1).broadcast_to([128, 2, 256]), ALU.mult, [y, gm], [yb])
                    p2 = A.next_ps()
                    pv = p2[:].bitcast(BF16).rearrange("p (a b) -> p a b", a=8)
                    for hh in range(4):
                        A.tr(pv[:, hh, :], yb[:, hh * 128:(hh + 1) * 128], A.ident[:], [yb, A.ident], [p2])
                    A.evac(qst[:, :, b * 128:(b + 1) * 128], pv[:, 0:4, :], [p2], [qst])
                A.dma("pool", A.qmT.h[4 * blk:4 * blk + 4].rearrange("h d t -> d h t"), qst[:], qst, [qst], [A.qmT])
            w = load_block(7168, 512)
            for b in range(NB):
                p = tok_major(w, 512, b)
                A.cp("act", yraw[:], p[:], [p], [yraw])
                c32 = TB["cos"][:, b * 64:(b + 1) * 64:2]
                s32 = TB["sin"][:, b * 64:(b + 1) * 64:2]
                A.rope_apply(yb[:].rearrange("p (h d) -> p h d", h=8), yraw[:].rearrange("p (h d) -> p h d", h=8),
                             8, 64, [c32, s32, c32, s32], [TB["cos"], TB["sin"]], [yraw], [yb], (t1, t2))
                p2 = A.next_ps()
                pv = p2[:].bitcast(BF16).rearrange("p (a b) -> p a b", a=8)
                for hh in range(8):
                    A.tr(pv[0:64, hh, :], yb[:, hh * 64:(hh + 1) * 64], A.ident[:], [yb, A.ident], [p2])
                qs = qist[b % 2]
                A.evac(qs[:], pv[0:64, :, :], [p2], [qs])
                A.dma("pool", A.qiT.h.rearrange("h d t -> d h t")[:, :, b * 128:(b + 1) * 128], qs[:], qs, [qs], [A.qiT])
            w = load_block(7744, 8)
            for b in range(NB):
                p = tok_major(w, 8, b)
                A.evac(wist[:, b, :], p[:, 0:8], [p], [wist])
            A.dma("pool", A.wi.h.rearrange("(b p) h -> p b h", p=128), wist[:], wist, [wist], [A.wi])

    def q_tiles(self):
        nkt = self.SEQ // 128
        out = []
        for slot, (segmax, segmin) in enumerate(((7, 0), (15, 8))):
            base = slot * QW
            for (off, w, toff) in ((0, 128, -128), (128, 512, 0), (640, 512, 512)):
                qmax = SEG * segmax + toff + w
                qmin = max(SEG * segmin + toff, 0)
                nk = min((qmax + 127) // 128, nkt)
                out.append((base + off, w, nk, min(qmin // 128, nk) if self.SEQ == SEQ else 0))
        return out

    def phase_p3(self):
        A, nc, cfg = self, self.nc, self.cfg
        A.osbT = A.dscratch("osbT", [8, 128, NQ])
        heads = cfg.get("heads", range(8))
        nkt = A.SEQ // 128
        SC = float(128 ** -0.5)
        with contextlib.ExitStack() as es:
            kT = [A.sb(es, f"kTa{i}", [128, A.SEQ], BF16) for i in range(2)]
            vv = [A.sb(es, f"vva{i}", [128, nkt, 128], BF16) for i in range(2)]
            qT = [A.sb(es, f"qTa{i}", [128, NQ], BF16) for i in range(2)]
            ost = [A.sb(es, f"osta{i}", [128, NQ], BF16) for i in range(2)]
            A.S.begin_const()
            qrow = A.sb(es, "qrow", [128, NQ], F32, const=True)
            kidx = A.sb(es, "kidx", [128, 128], F32, const=True)
            A.dma("sp", qrow[:], A.qidx_row.broadcast_to([128, NQ]), qrow, [], [qrow])
            A.dma("sp", kidx[:], A.kidx_pt[:, :], kidx, [], [kidx])
            trif = A.sb(es, "trif", [128, 128], F32, const=True)
            tri = A.sb(es, "tri", [128, 128], BF16)
            ones = A.sb(es, "ones", [128, 128], BF16)
            one1 = A.sb(es, "one1", [128, 1], F32)
            A.dma("sp", trif[:], A.c_tri[:, :], trif, [], [trif])
            A.cp("dve", tri[:], trif[:], [trif], [tri])
            A.S.op("pool", lambda e: e.memset(ones[:], 1.0), [], [ones])
            A.S.op("pool", lambda e: e.memset(one1[:], 1.0), [], [one1])
            A.S.end_const()
            NCH = 3
            eb = [[A.sb(es, f"eb{c}_{i}", [128, 512], F32) for i in range(2)] for c in range(NCH)]
            spb = [[A.sb(es, f"spb{c}_{i}", [128, 512], BF16) for i in range(2)] for c in range(NCH)]
            sps = [[A.sb(es, f"sps{c}_{i}", [128, 512], BF16) for i in range(2)] for c in range(NCH)]
            tb = [[A.sb(es, f"tb{c}_{i}", [128, 512], F32) for i in range(2)] for c in range(NCH)]
            ab = [[A.sb(es, f"ab{c}_{i}", [128, 512], BF16) for i in range(2)] for c in range(NCH)]
            locked = []

            def chain(c, q0, tw, nk, kmask, k_, v_, q_, o_):
                po = A.ps_free(locked)
                locked.append(po)
                for i, kt in enumerate(range(nk - 1, -1, -1)):
                    first, last = i == 0, kt == 0
                    e_, sp_, t_, a_ = eb[c][i % 2], spb[c][i % 2], tb[c][i % 2], ab[c][i % 2]
                    ss_new, ss_old = sps[c][i % 2], sps[c][(i + 1) % 2]
                    pz = A.ps_free(locked)
                    A.mm(pz[:, 0:tw], k_[:, kt * 128:(kt + 1) * 128], q_[:, q0:q0 + tw], True, True, [k_, q_], [pz])
                    A.act(e_[:, 0:tw], pz[:, 0:tw], AF.Exp, [pz], [e_], scale=SC)
                    if kt >= kmask:
                        A.stt("dve", e_[:, 0:tw], qrow[:, q0:q0 + tw], kidx[:, kt:kt + 1], e_[:, 0:tw],
                              ALU.is_gt, ALU.mult, [qrow, kidx, e_], [e_])
                    A.act(sp_[:, 0:tw], e_[:, 0:tw], AF.Ln, [e_, one1], [sp_], bias=one1[:, 0:1])
                    yield
                    pc = A.ps_free(locked)
                    A.mm(pc[:, 0:tw], tri[:], sp_[:, 0:tw], True, first, [tri, sp_], [pc])
                    if first:
                        A.cp("pool", ss_new[:, 0:tw], sp_[:, 0:tw], [sp_], [ss_new])
                    else:
                        A.mm(pc[:, 0:tw], ones[:], ss_old[:, 0:tw], False, True, [ones, ss_old], [pc])
                        if not last:
                            A.tt("pool", ss_new[:, 0:tw], ss_old[:, 0:tw], sp_[:, 0:tw], ALU.add, [ss_old, sp_], [ss_new])
                    A.act(t_[:, 0:tw], pc[:, 0:tw], AF.Exp, [pc], [t_], scale=-1.0)
                    A.tt("dve", a_[:, 0:tw], e_[:, 0:tw], t_[:, 0:tw], ALU.mult, [e_, t_], [a_])
                    yield
                    A.mm(po[:, 0:tw], v_[:, kt, :], a_[:, 0:tw], first, last, [v_, a_], [po])
                A.evac(o_[:, q0:q0 + tw], po[:, 0:tw], [po], [o_])
                locked.remove(po)

            for hi, hh in enumerate(heads):
                k_, v_, q_, o_ = kT[hi % 2], vv[hi % 2], qT[hi % 2], ost[hi % 2]
                nsp = 4
                for j in range(nsp):
                    c0, c1 = A.SEQ * j // nsp, A.SEQ * (j + 1) // nsp
                    A.dma("sp" if j % 2 else "act", k_[:, c0:c1], A.kaT.h[hh, :, c0:c1], k_, [A.kaT], [k_])
                nsv = max(4, nkt // 8)
                for j in range(nsv):
                    t0, t1 = nkt * j // nsv, nkt * (j + 1) // nsv
                    A.dma("act" if j % 2 else "sp", v_[:, t0:t1, :],
                          A.va.h[t0 * 128:t1 * 128, hh * 128:(hh + 1) * 128].rearrange("(t p) d -> p t d", p=128),
                          v_, [A.va], [v_])
                A.dma("sp", q_[:], A.qaT.h[hh, :, :], q_, [A.qaT], [q_])
                qts = A.q_tiles()
                for g0 in range(0, len(qts), NCH):
                    gens = [chain(c, *qt, k_, v_, q_, o_) for c, qt in enumerate(qts[g0:g0 + NCH])]
                    while gens:
                        for g in list(gens):
                            try:
                                next(g)
                            except StopIteration:
                                gens.remove(g)
                A.dma("pool", A.osbT.h[hh, :, :], o_[:], o_, [o_], [A.osbT])

    def q_blocks(self):
        out = []
        for slot, segmax in enumerate((7, 15)):
            for j in range(-1, 8):
                qmax0 = SEG * segmax + 128 * j
                out.append((slot * 9 + j + 1, min(qmax0 + 128, self.SEQ)))
        return out

    def ps_free(self, excl):
        for _ in range(2 * len(self.ps)):
            p = self.next_ps()
            if all(p is not x for x in excl):
                return p
        raise RuntimeError("no free PSUM bank")

    def phase_p4(self):
        A, nc, cfg = self, self.nc, self.cfg
        A.odsaT = A.dscratch("odsaT", [8, 128, NQ])
        NB = NQ // 128
        blocks = [qb for qb in A.q_blocks() if qb[0] in cfg.get("qblocks", range(NB))]
        SCD = float(128 ** -0.5)
        NITER = cfg.get("niter", 22)
        with contextlib.ExitStack() as es:
            A.S.begin_const()
            kiT = A.sb(es, "kiT", [64, A.SEQ], BF16, const=True)
            A.dma("sp", kiT[:], A.kiT.h[:, :], kiT, [A.kiT], [kiT])
            Sb = A.sb(es, "Sb", [128, A.SEQ], F32)
            mask = A.sb(es, "maskb", [128, A.SEQ], BF16)
            iota = A.sb(es, "iota", [128, 512], F32, const=True)
            A.dma("sp", iota[:], A.kidx_row[:, 0:512].broadcast_to([128, 512]), iota, [], [iota])
            qlim = A.sb(es, "qlim", [128, NB], F32, const=True)
            A.dma("sp", qlim[:], A.qlim_pt[:, :], qlim, [], [qlim])
            ones = A.sb(es, "ones4", [128, 128], BF16)
            A.S.op("pool", lambda e: e.memset(ones[:], 1.0), [], [ones])
            zeros = A.sb(es, "zeros4", [128, 512], BF16)
            A.S.op("pool", lambda e: e.memset(zeros[:], 0.0), [], [zeros])
            wi = A.sb(es, "wi_sb", [128, NB, 8], F32, const=True)
            A.dma("sp", wi[:], A.wi.h.rearrange("(b p) h -> p b h", p=128), wi, [A.wi], [wi])
            A.S.end_const()
            qi = [A.sb(es, f"qi{i}", [64, 8, 128], BF16) for i in range(2)]
            qb_ = [A.sb(es, f"qbq{i}", [128, 8, 128], BF16) for i in range(2)]
            Dg = [A.sb(es, f"Dg{i}", [128, 8, 128], BF16) for i in range(2)]
            rl = [A.sb(es, f"rl{i}", [128, 512], BF16) for i in range(4)]
            pen = [A.sb(es, f"pen{i}", [128, 512], F32) for i in range(2)]
            am = A.sb(es, "am", [128, 32], F32)
            st = {k: A.sb(es, "bs_" + k, [128, 1], F32) for k in ("lo", "w", "mid", "cnt", "ge", "amx")}
            kc = [A.sb(es, f"kc{i}", [128, 8, 512], BF16) for i in range(2)]
            vc = [A.sb(es, f"vc{i}", [128, 4, 1024], BF16) for i in range(2)]
            pe_ = [A.sb(es, f"pexp{i}", [128, 512], BF16) for i in range(3)]
            pm = [A.sb(es, f"pm{i}", [128, 512], BF16) for i in range(3)]
            rinv = A.sb(es, "rinv", [128, 512], F32)
            ost = [A.sb(es, f"ostd{i}", [128, 8, 128], BF16) for i in range(2)]
            kT_view = A.kbT.h.rearrange("h d t -> d h t")
            qiT_view = A.qiT.h.rearrange("h d t -> d h t")
            qbT_view = A.qbT.h.rearrange("h d t -> d h t")
            cnt_i = 0
            for bi, (b, L) in enumerate(blocks):
                nch = (L + 511) // 512
                qi_, qq, dg = qi[bi % 2], qb_[bi % 2], Dg[bi % 2]
                A.dma("sp", qi_[:], qiT_view[:, :, b * 128:(b + 1) * 128], qi_, [A.qiT], [qi_])
                A.dma("sp", qq[:], qbT_view[:, :, b * 128:(b + 1) * 128], qq, [A.qbT], [qq])
                for h in range(8):
                    A.tsc("dve", dg[:, h, :], A.ident[:], wi[:, b, h:h + 1], None, ALU.mult, None, [A.ident, wi], [dg])
                for ci in range(nch):
                    c0 = ci * 512
                    cw = min(512, L - c0)
                    pS = A.next_ps()
                    pls = {}

                    def stage_l(h):
                        pl = A.ps_free([pS] + list(pls.values()))
                        pls[h] = pl
                        A.mm(pl[:, 0:cw], qi_[:, h, :], kiT[:, c0:c0 + cw], True, True, [qi_, kiT], [pl])
                        r_ = rl[(cnt_i + h) % 4]
                        if h % 2 == 0:
                            A.act(r_[:, 0:cw], pl[:, 0:cw], AF.Relu, [pl], [r_])
                        else:
                            A.tsc("dve", r_[:, 0:cw], pl[:, 0:cw], 0.0, None, ALU.max, None, [pl], [r_])
                        return r_

                    rs_ = {0: stage_l(0), 1: stage_l(1)}
                    for h in range(8):
                        if h + 2 < 8:
                            rs_[h + 2] = stage_l(h + 2)
                        A.mm(pS[:, 0:cw], dg[:, h, :], rs_[h][:, 0:cw], h == 0, h == 7, [dg, rs_[h]], [pS])
                        del pls[h]
                    cnt_i += 8
                    A.S.op("dve", lambda e, o=am[:, ci:ci + 1], i=pS[:, 0:cw]:
                           e.tensor_reduce(out=o, in_=i, axis=AX.X, op=ALU.max, apply_absolute_value=True), [pS], [am])
                    pn = pen[ci % 2]
                    A.tsc("dve", pn[:, 0:cw], iota[:, 0:cw], float(c0), qlim[:, b:b + 1], ALU.add, ALU.is_ge, [iota, qlim], [pn])
                    A.stt("dve", Sb[:, c0:c0 + cw], pn[:, 0:cw], -1.0e30, pS[:, 0:cw], ALU.mult, ALU.add, [pn, pS], [Sb])
                lo, w_, mid, cnt, ge, amx = (st[k] for k in ("lo", "w", "mid", "cnt", "ge", "amx"))
                A.S.op("dve", lambda e, i=am[:, 0:nch]: e.tensor_reduce(out=amx[:], in_=i, axis=AX.X, op=ALU.max), [am], [amx])
                A.tsc("dve", lo[:], amx[:], -1.0001, -1e-6, ALU.mult, ALU.add, [amx], [lo])
                A.tsc("dve", w_[:], amx[:], 2.0004, 4e-6, ALU.mult, ALU.add, [amx], [w_])
                for it in range(NITER):
                    A.tsc("dve", w_[:], w_[:], 0.5, None, ALU.mult, None, [w_], [w_])
                    A.tt("dve", mid[:], lo[:], w_[:], ALU.add, [lo, w_], [mid])
                    A.tsc("dve", mask[:, 0:L], Sb[:, 0:L], mid[:, 0:1], 0.0, ALU.is_ge, ALU.add, [Sb, mid], [mask, cnt], accum_out=cnt[:, 0:1])
                    A.tsc("dve", ge[:], cnt[:], 255.5, None, ALU.is_ge, None, [cnt], [ge])
                    A.stt("dve", lo[:], ge[:], w_[:, 0:1], lo[:], ALU.mult, ALU.add, [ge, w_, lo], [lo])
                A.tsc("dve", mask[:, 0:L], Sb[:, 0:L], lo[:, 0:1], None, ALU.is_ge, None, [Sb, lo], [mask])
                po = [A.next_ps(), A.next_ps()]
                pr = [A.next_ps(), A.next_ps()]
                acc = po + pr
                nkt = L // 128
                for hg in range(2):
                    A.mm(po[hg][:], zeros[:, 0:128], zeros[:], True, False, [zeros], [po[hg]])
                units = [(ci, j, hg) for ci in range(nch) for j in range(min(512, L - ci * 512) // 128) for hg in range(2)]
                chunk = {}
                inflight = {}

                def stage_a(u):
                    ci, j, hg = u
                    c0 = ci * 512
                    cw = min(512, L - c0)
                    if ci not in chunk:
                        k_, v_ = kc[ci % 2], vc[ci % 2]
                        A.dma("sp", k_[:, :, 0:cw], kT_view[:, :, c0:c0 + cw], k_, [A.kbT], [k_])
                        A.dma("act", v_[:, 0:cw // 128, :], A.vb.h[c0:c0 + cw, :].rearrange("(t p) n -> p t n", p=128), v_, [A.vb], [v_])
                        busy = acc + [c[2] for c in chunk.values()] + [x[0] for x in inflight.values()]
                        pmk = A.ps_free(busy)
                        pmv = pmk[:].bitcast(BF16).rearrange("p (a b) -> p a b", a=8)
                        for jj in range(cw // 128):
                            A.tr(pmv[:, jj, :], mask[:, c0 + jj * 128:c0 + (jj + 1) * 128], A.ident[:], [mask, A.ident], [pmk])
                        chunk[ci] = (k_, v_, pmk, pmv)
                        for old in [c for c in chunk if c < ci - 1]:
                            del chunk[old]
                    k_, v_, pmk, pmv = chunk[ci]
                    busy = acc + [c[2] for c in chunk.values()] + [x[0] for x in inflight.values()]
                    ps = A.ps_free(busy)
                    for hh in range(4):
                        h = hg * 4 + hh
                        A.mm(ps[:, hh * 128:(hh + 1) * 128], k_[:, h, j * 128:(j + 1) * 128], qq[:, h, :], True, True, [k_, qq], [ps])
                    n = len(inflight_n)
                    inflight_n.append(u)
                    pe1, pm1 = pe_[n % 3], pm[n % 3]
                    A.act(pe1[:], ps[:], AF.Exp, [ps], [pe1], scale=SCD)
                    A.tt("dve", pm1[:].rearrange("p (h q) -> p h q", h=4), pe1[:].rearrange("p (h q) -> p h q", h=4),
                         pmv[:, j, :].unsqueeze(1).broadcast_to([128, 4, 128]), ALU.mult, [pe1, pmk], [pm1])
                    inflight[u] = (ps, pm1, v_)

                def stage_c(u):
                    ci, j, hg = u
                    ps, pm1, v_ = inflight.pop(u)
                    kt = ci * 4 + j
                    first, last = kt == 0, kt == nkt - 1
                    for hh in range(4):
                        h = hg * 4 + hh
                        A.mm(po[hg][:, hh * 128:(hh + 1) * 128], v_[:, j, h * 128:(h + 1) * 128], pm1[:, hh * 128:(hh + 1) * 128],
                             False, last and hh == 3, [v_, pm1], [po[hg]])
                    A.mm(pr[hg][:], ones[:], pm1[:], first, last, [ones, pm1], [pr[hg]])

                inflight_n = []
                LA = 1
                for i in range(len(units) + LA):
                    if i < len(units):
                        stage_a(units[i])
                    if i - LA >= 0:
                        stage_c(units[i - LA])
                o_ = ost[bi % 2]
                for hg in range(2):
                    A.S.op("dve", lambda e, p_=pr[hg][:]: e.reciprocal(out=rinv[:], in_=p_), [pr[hg]], [rinv])
                    A.tt("dve", o_[:, hg * 4:(hg + 1) * 4, :], po[hg][:].rearrange("p (h q) -> p h q", h=4),
                         rinv[:].rearrange("p (h q) -> p h q", h=4), ALU.mult, [po[hg], rinv], [o_])
                A.dma("pool", A.odsaT.h.rearrange("h d t -> d h t")[:, :, b * 128:(b + 1) * 128], o_[:], o_, [o_], [A.odsaT])

    def phase_p5(self):
        A, nc, cfg = self, self.nc, self.cfg
        A.omemT = A.dscratch("omemT", [8, 128, NQ])
        SCM = float(256 ** -0.5)
        ttiles = [(t0, min(512, NQ - t0)) for t0 in range(0, NQ, 512)]
        with contextlib.ExitStack() as es:
            mkT = A.sb(es, "mkT", [128, 8, 256], BF16)
            mv = A.sb(es, "mv", [128, 2, 1024], BF16)
            ones = A.sb(es, "ones5", [128, 128], BF16)
            A.S.op("pool", lambda e: e.memset(ones[:], 1.0), [], [ones])
            with contextlib.ExitStack() as es2:
                A.S.begin_const()
                gb = A.sb(es2, "gbm", [128, D], F32, const=True)
                A.dma("sp", gb[:], A.mem_norm.broadcast_to([128, D]), gb, [], [gb])
                A.tsc("dve", gb[:], gb[:], float(np.sqrt(D)), None, ALU.mult, None, [gb], [gb])
                gk = A.sb(es2, "gk", [128, 256], F32, const=True)
                A.dma("sp", gk[:], A.mem_k_norm.broadcast_to([128, 256]), gk, [], [gk])
                A.tsc("dve", gk[:], gk[:], float(np.sqrt(256.0)), None, ALU.mult, None, [gk], [gk])
                A.S.end_const()
                xt = A.sb(es2, "xm", [128, D], F32)
                ss = A.sb(es2, "ssm", [128, 1], F32)
                rs = A.sb(es2, "rsm", [128, 1], F32)
                h = A.sb(es2, "hm", [128, D], BF16)
                mhT = A.sb(es2, "mhT", [128, 16, 256], BF16)
                for b in range(2):
                    A.dma("sp", xt[:], A.mem[128 * b:128 * (b + 1), :], xt, [], [xt])
                    A.norm_tile(xt, gb, h, ss, rs, h, D, [])
                    A.transpose_to(mhT, b * 128, h)
                Wb = [A.sb(es2, f"Wm{i}", [128, 16, 512], BF16) for i in range(2)]
                stage = [A.sb(es2, f"wstm{i}", [128, 16, 128], F32) for i in range(2)]
                sq = A.sb(es2, "sqm", [128, 512], F32)
                ssq = A.sb(es2, "ssqm", [128, 2], F32)
                rs2 = A.sb(es2, "rs2m", [128, 2], F32)
                yraw = A.sb(es2, "yrawm", [128, 512], F32)
                y = A.sb(es2, "ym", [128, 512], F32)
                yb = A.sb(es2, "ybm", [128, 512], BF16)
                for blk in range(4):
                    w = Wb[blk % 2]
                    A.load_w_bf16(w, 0, A.w_mem_kv, blk * 512, 512, stage)
                    for b in range(2):
                        p = A.next_ps()
                        for c in range(16):
                            A.mm(p[:], mhT[:, c, b * 128:(b + 1) * 128], w[:, c, :], c == 0, c == 15, [w, mhT], [p])
                        if blk < 2:
                            A.act(sq[:], p[:], AF.Square, [p], [sq])
                            A.S.op("dve", lambda e, o=ssq[:, 0:2], i=sq[:].rearrange("p (h d) -> p h d", h=2):
                                   e.tensor_reduce(out=o, in_=i, axis=AX.X, op=ALU.add), [sq], [ssq])
                            A.cp("dve", yraw[:], p[:], [p], [yraw])
                            A.rsqrt(rs2[:], ssq[:], 3, rs2, ssq)
                            y3 = y[:].rearrange("p (h d) -> p h d", h=2)
                            A.tt("dve", y3, yraw[:].rearrange("p (h d) -> p h d", h=2),
                                 rs2[:].unsqueeze(2).broadcast_to([128, 2, 256]), ALU.mult, [yraw, rs2], [y])
                            A.tt("pool", yb[:].rearrange("p (h d) -> p h d", h=2), y3,
                                 gk[:].unsqueeze(1).broadcast_to([128, 2, 256]), ALU.mult, [y, gk], [yb])
                            p2 = A.next_ps()
                            pv = p2[:].bitcast(BF16).rearrange("p (a b) -> p a b", a=8)
                            for hh in range(4):
                                A.tr(pv[:, hh, :], yb[:, hh * 128:(hh + 1) * 128], A.ident[:], [yb, A.ident], [p2])
                            A.evac(mkT[:, blk * 4:(blk + 1) * 4, b * 128:(b + 1) * 128], pv[:, 0:4, :], [p2], [mkT])
                        else:
                            A.evac(mv[:, b, (blk - 2) * 512:(blk - 1) * 512], p[:], [p], [mv])
                A.S.barrier()
            qm = [A.sb(es, f"qm{i}", [128, 2, NQ], BF16) for i in range(2)]
            pT = [A.sb(es, f"pTm{i}", [128, 512], BF16) for i in range(4)]
            rinv = A.sb(es, "rinvm", [128, 512], F32)
            ost = [A.sb(es, f"ostm{i}", [128, 2, NQ], BF16) for i in range(2)]
            n = 0
            for hd in range(4):
                q_, o_ = qm[hd % 2], ost[hd % 2]
                A.dma("sp", q_[:], A.qmT.h[2 * hd:2 * hd + 2].rearrange("h d t -> d h t"), q_, [A.qmT], [q_])
                for (t0, tw) in ttiles:
                    po = [A.next_ps(), A.next_ps()]
                    pr = A.next_ps()
                    acc = po + [pr]
                    for mb in range(2):
                        ps = A.ps_free(acc)
                        for dc in range(2):
                            A.mm(ps[:, 0:tw], mkT[:, hd * 2 + dc, mb * 128:(mb + 1) * 128], q_[:, dc, t0:t0 + tw], dc == 0, dc == 1, [mkT, q_], [ps])
                        pt = pT[n % 4]
                        n += 1
                        A.act(pt[:, 0:tw], ps[:, 0:tw], AF.Exp, [ps], [pt], scale=SCM)
                        for dc in range(2):
                            A.mm(po[dc][:, 0:tw], mv[:, mb, hd * 256 + dc * 128: hd * 256 + (dc + 1) * 128], pt[:, 0:tw], mb == 0, mb == 1, [mv, pt], [po[dc]])
                        A.mm(pr[:, 0:tw], ones[:], pt[:, 0:tw], mb == 0, mb == 1, [ones, pt], [pr])
                    A.S.op("dve", lambda e, tw=tw, pr=pr: e.reciprocal(out=rinv[:, 0:tw], in_=pr[:, 0:tw]), [pr], [rinv])
                    for dc in range(2):
                        A.tt("dve", o_[:, dc, t0:t0 + tw], po[dc][:, 0:tw], rinv[:, 0:tw], ALU.mult, [po[dc], rinv], [o_])
                A.dma("pool", A.omemT.h[2 * hd:2 * hd + 2].rearrange("h d t -> d h t"), o_[:], o_, [o_], [A.omemT])

    def phase_p6(self):
        A, nc, cfg = self, self.nc, self.cfg
        A.mergedT = A.dscratch("mergedT", [128, 16, NQ])
        A.x1 = A.dscratch("x1", [NQ, D], F32)
        A.h2T = A.dscratch("h2T", [128, 16, NQ])
        subt = [(0, 512), (512, 512), (1024, 128)]
        srcs = (A.osbT, A.odsaT, A.omemT)
        with contextlib.ExitStack() as es:
            A.S.begin_const()
            bg = A.sb(es, "bg", [128, 48], F32, const=True)
            A.dma("sp", bg[:], A.bg_pt[:, :], bg, [], [bg])
            A.S.end_const()
            oT = [A.sb(es, f"oT{i}", [128, 8, QW], BF16) for i in range(3)]
            hq = A.sb(es, "hq6", [128, 16, QW], BF16)
            mT = A.sb(es, "mT", [128, 16, QW], BF16)
            Wbr = [[A.sb(es, f"Wbr{i}_{j}", [128, 8, 128], BF16) for i in range(3)] for j in range(2)]
            Wg = [[A.sb(es, f"Wg{i}_{j}", [128, 16, 128], BF16) for i in range(3)] for j in range(2)]
            stage = [A.sb(es, f"wst6{i}", [128, 16, 128], F32) for i in range(2)]
            gsb = [A.sb(es, f"gsb{i}", [128, 512], F32) for i in range(2)]
            macc = [A.sb(es, f"macc{i}", [128, 512], F32) for i in range(2)]
            tmp = [A.sb(es, f"tmp6{i}", [128, 512], F32) for i in range(2)]
            n = 0
            for slot in range(2):
                q0 = slot * QW
                for i in range(3):
                    A.dma("sp" if i % 2 else "act", oT[i][:], srcs[i].h.rearrange("h d t -> d h t")[:, :, q0:q0 + QW], oT[i], [srcs[i]], [oT[i]])
                A.dma("sp", hq[:], A.hTq.h[:, :, q0:q0 + QW], hq, [A.hTq], [hq])
                for ft in range(16):
                    j = (slot * 16 + ft) % 2
                    for i in range(3):
                        A.load_w_bf16(Wbr[j][i], 0, A.w_branch[i], ft * 128, 128, stage)
                        A.load_w_bf16(Wg[j][i], 0, A.w_in, 7752 + i * 2048 + ft * 128, 128, stage)
                    for (t0, tw) in subt:
                        mc = macc[n % 2]
                        for i in range(3):
                            pb = A.next_ps()
                            for c in range(8):
                                A.mm(pb[:, 0:tw], Wbr[j][i][:, c, :], oT[i][:, c, t0:t0 + tw], c == 0, c == 7, [Wbr[j][i], oT[i]], [pb])
                            pg = A.next_ps()
                            for c in range(16):
                                A.mm(pg[:, 0:tw], Wg[j][i][:, c, :], hq[:, c, t0:t0 + tw], c == 0, c == 15, [Wg[j][i], hq], [pg])
                            g_ = gsb[(n * 3 + i) % 2]
                            A.act(g_[:, 0:tw], pg[:, 0:tw], AF.Sigmoid, [pg, bg], [g_], bias=bg[:, i * 16 + ft:i * 16 + ft + 1])
                            if i == 0:
                                A.tt("dve", mc[:, 0:tw], g_[:, 0:tw], pb[:, 0:tw], ALU.mult, [g_, pb], [mc])
                            else:
                                t_ = tmp[i % 2]
                                A.tt("dve", t_[:, 0:tw], g_[:, 0:tw], pb[:, 0:tw], ALU.mult, [g_, pb], [t_])
                                if i == 1:
                                    A.tt("pool", mc[:, 0:tw], mc[:, 0:tw], t_[:, 0:tw], ALU.add, [mc, t_], [mc])
                                else:
                                    A.tt("pool", mT[:, ft, t0:t0 + tw], mc[:, 0:tw], t_[:, 0:tw], ALU.add, [mc, t_], [mT])
                        n += 1
                A.dma("pool", A.mergedT.h[:, :, q0:q0 + QW], mT[:], mT, [mT], [A.mergedT])
            A.S.barrier()
        with contextlib.ExitStack() as es:
            Wout = A.sb(es, "Wout", [128, 16, D], BF16)
            with contextlib.ExitStack() as es2:
                stage = [A.sb(es2, f"wst6b{i}", [128, 16, 256], F32) for i in range(2)]
                A.load_w_bf16(Wout, 0, A.w_out, 0, D, stage)
                A.S.barrier()
            A.S.begin_const()
            gbf = A.sb(es, "gbf", [128, D], F32, const=True)
            A.dma("sp", gbf[:], A.ffn_norm.broadcast_to([128, D]), gbf, [], [gbf])
            A.tsc("dve", gbf[:], gbf[:], float(np.sqrt(D)), None, ALU.mult, None, [gbf], [gbf])
            A.S.end_const()
            mTt = [A.sb(es, f"mTt{i}", [128, 16, 128], BF16) for i in range(2)]
            xt = [A.sb(es, f"x6{i}", [128, D], F32) for i in range(2)]
            x1t = [A.sb(es, f"x1t{i}", [128, D], F32) for i in range(2)]
            ss = [A.sb(es, f"ss6{i}", [128, 1], F32) for i in range(2)]
            rs = [A.sb(es, f"rs6{i}", [128, 1], F32) for i in range(2)]
            h2 = [A.sb(es, f"h26{i}", [128, D], BF16) for i in range(2)]
            h2t = [A.sb(es, f"h2t{i}", [128, 16, 128], BF16) for i in range(2)]
            for b in range(NQ // 128):
                m_, x_, x1_, h_, ht_ = mTt[b % 2], xt[b % 2], x1t[b % 2], h2[b % 2], h2t[b % 2]
                A.dma("sp", m_[:], A.mergedT.h[:, :, b * 128:(b + 1) * 128], m_, [A.mergedT], [m_])
                A.dma("act", x_[:], A.x_q[b * 128:(b + 1) * 128, :], x_, [], [x_])
                for ct in range(4):
                    p = A.next_ps()
                    for ft in range(16):
                        A.mm(p[:], m_[:, ft, :], Wout[:, ft, ct * 512:(ct + 1) * 512], ft == 0, ft == 15, [m_, Wout], [p])
                    A.tt("dve", x1_[:, ct * 512:(ct + 1) * 512], p[:], x_[:, ct * 512:(ct + 1) * 512], ALU.add, [p, x_], [x1_])
                A.dma("pool", A.x1.h[b * 128:(b + 1) * 128, :], x1_[:], x1_, [x1_], [A.x1])
                A.norm_tile(x1_, gbf, h_, ss[b % 2], rs[b % 2], h_, D, [])
                A.transpose_to(ht_, 0, h_)
                A.dma("pool", A.h2T.h[:, :, b * 128:(b + 1) * 128], ht_[:], ht_, [ht_], [A.h2T])

    def phase_p7(self):
        A, nc, cfg = self, self.nc, self.cfg
        NFF = D_FF // 128
        with contextlib.ExitStack() as es:
            actT = A.sb(es, "actT", [128, NFF, SEG], BF16)
            for slot in range(2):
                q0 = slot * QW
                with contextlib.ExitStack() as es2:
                    h2 = A.sb(es2, "h2seg", [128, 16, QW], BF16)
                    A.dma("sp", h2[:], A.h2T.h[:, :, q0:q0 + QW], h2, [A.h2T], [h2])
                    A.S.begin_const()
                    fl = A.sb(es2, "flag", [128, 2], F32, const=True)
                    A.dma("sp", fl[:], A.flags.broadcast_to([128, 2]), fl, [], [fl])
                    cwb = A.sb(es2, "cwb", [128, 2 * NFF, 4], F32, const=True)
                    A.dma("sp", cwb[:], A.cwb[:, :, :], cwb, [], [cwb])
                    A.S.end_const()
                    A.tsc("dve", h2[:, :, 126:128], h2[:, :, 126:128], fl[:, slot:slot + 1], None, ALU.mult, None, [h2, fl], [h2])
                    W = [A.sb(es2, f"Wup{i}", [128, 16, 128], BF16) for i in range(4)]
                    stage = [A.sb(es2, f"wst7{i}", [128, 16, 128], F32) for i in range(2)]
                    pre = [A.sb(es2, f"pre{i}", [128, SEG + 2], F32) for i in range(2)]
                    u = [A.sb(es2, f"u{i}", [128, SEG], F32) for i in range(2)]
                    sg = A.sb(es2, "sg", [128, SEG], F32)
                    n = 0
                    for i in range(NFF):
                        for which in range(2):
                            tile = which * NFF + i
                            w = W[n % 4]
                            pr_, u_ = pre[n % 2], u[n % 2]
                            n += 1
                            A.load_w_bf16(w, 0, A.w_up, tile * 128, 128, stage)
                            ph = A.next_ps()
                            for c in range(16):
                                A.mm(ph[:, 0:2], w[:, c, :], h2[:, c, 126:128], c == 0, c == 15, [w, h2], [ph])
                            A.evac(pr_[:, 0:2], ph[:, 0:2], [ph], [pr_])
                            for t0 in (0, 512):
                                p = A.next_ps()
                                for c in range(16):
                                    A.mm(p[:], w[:, c, :], h2[:, c, 128 + t0:128 + t0 + 512], c == 0, c == 15, [w, h2], [p])
                                A.evac(pr_[:, 2 + t0:2 + t0 + 512], p[:], [p], [pr_])
                            A.tsc("dve", u_[:], pr_[:, 0:SEG], cwb[:, tile, 0:1], cwb[:, tile, 3:4], ALU.mult, ALU.add, [pr_, cwb], [u_])
                            A.stt("dve", u_[:], pr_[:, 1:SEG + 1], cwb[:, tile, 1:2], u_[:], ALU.mult, ALU.add, [pr_, cwb, u_], [u_])
                            A.stt("dve", u_[:], pr_[:, 2:SEG + 2], cwb[:, tile, 2:3], u_[:], ALU.mult, ALU.add, [pr_, cwb, u_], [u_])
                            if which == 0:
                                A.act(sg[:], u_[:], AF.Silu, [u_], [sg])
                            else:
                                A.tt("pool", actT[:, i, :], sg[:], u_[:], ALU.mult, [sg, u_], [actT])
                    A.S.barrier()
                with contextlib.ExitStack() as es2:
                    Wd = A.sb(es2, "Wd", [128, NFF, 512], BF16)
                    stage = [A.sb(es2, f"wst7d{i}", [128, NFF, 128], F32) for i in range(2)]
                    xt = [A.sb(es2, f"x7{i}", [128, 512], F32) for i in range(2)]
                    ot = [A.sb(es2, f"o7{i}", [128, 512], F32) for i in range(2)]
                    n = 0
                    for ct in range(4):
                        A.load_w_bf16(Wd, 0, A.w_down, ct * 512, 512, stage)
                        for tb in range(SEG // 128):
                            row0 = q0 + 128 + tb * 128
                            x_, o_ = xt[n % 2], ot[n % 2]
                            n += 1
                            A.dma("act", x_[:], A.x1.h[row0:row0 + 128, ct * 512:(ct + 1) * 512], x_, [A.x1], [x_])
                            p = A.next_ps()
                            for i in range(NFF):
                                A.mm(p[:], actT[:, i, tb * 128:(tb + 1) * 128], Wd[:, i, :], i == 0, i == NFF - 1, [actT, Wd], [p])
                            A.tt("dve", o_[:], p[:], x_[:], ALU.add, [p, x_], [o_])
                            orow = slot * SEG + tb * 128
                            A.dma("pool", A.out[orow:orow + 128, ct * 512:(ct + 1) * 512], o_[:], o_, [o_], [])
                    A.S.barrier()


def core_tokens(c):
    sA, sB = c, 15 - c
    hA = np.arange(SEG * sA - 128, SEG * sA) if sA > 0 else np.arange(0, 128)
    hB = np.arange(SEG * sB - 128, SEG * sB)
    idx = np.concatenate([hA, np.arange(SEG * sA, SEG * (sA + 1)), hB, np.arange(SEG * sB, SEG * (sB + 1))])
    flags = np.array([[1.0 if sA > 0 else 0.0, 1.0]], np.float32)
    return idx, flags


def host_inputs(inp, c, seq=SEQ):
    f = np.float32
    idx, flags = core_tokens(c)
    idx = np.minimum(idx, seq - 1) if seq < SEQ else idx
    x = inp["x"][0]
    pos = inp["positions"][0]
    m = {}
    m["x_all"] = np.ascontiguousarray(x[:seq])
    pp = np.zeros((128, 128), np.int32)
    pp[:, :seq // 128] = pos[:seq].reshape(-1, 128).T
    m["pos_pt"] = pp
    m["x_q"] = np.ascontiguousarray(x[idx])
    m["posq_pt"] = np.ascontiguousarray(pos[idx].reshape(-1, 128).T.astype(np.int32))
    m["qidx_row"] = idx.astype(f)[None, :]
    m["qidx_pt"] = np.ascontiguousarray(idx.astype(f).reshape(-1, 128).T)
    m["qlim_pt"] = np.ascontiguousarray(((idx // 64 + 1) * 64).astype(f).reshape(-1, 128).T)
    kk = np.arange(SEQ, dtype=f)
    m["kidx_pt"] = np.ascontiguousarray(kk.reshape(-1, 128).T)
    m["kidx_row"] = kk[None, :seq].copy()
    m["flags"] = flags
    m["c_ident"] = np.eye(128, dtype=f)
    m["c_tri"] = np.tril(np.ones((128, 128), f))
    m["c_inv64"] = (10000.0 ** (-np.arange(64) / 64.0) / (2 * np.pi)).astype(f)[None, :]
    m["mem"] = np.ascontiguousarray(inp["mem"][0])
    m["bg_pt"] = np.ascontiguousarray(inp["b_gate"].reshape(48, 128).T)
    cw = np.concatenate([inp["conv_w"][0], inp["conv_b"].reshape(1, -1)], 0)
    m["cwb"] = np.ascontiguousarray(cw.reshape(4, 86, 128).transpose(2, 1, 0))
    for k in ("attn_norm", "mem_norm", "b_gate", "dsa_q_norm", "dsa_k_norm", "mem_q_norm", "mem_k_norm",
              "ffn_norm", "conv_b"):
        m[k] = np.ascontiguousarray(inp[k]).reshape(1, -1)
    for k in ("w_in", "w_mem_kv", "w_branch", "w_out", "w_up", "conv_w", "w_down"):
        m[k] = np.ascontiguousarray(inp[k][0])
    return m


def full_cfg():
    return dict(phases=["p1a", "p1b", "p2", "p3", "p4", "p5", "p6", "p7"])


def kernel(**inputs):
    inp = {k: np.asarray(v) for k, v in inputs.items()}
    B = Builder(full_cfg())
    nc = B.build()
    in_maps = [host_inputs(inp, c) for c in range(NCORE)]
    res = run_bass_kernel_spmd(nc, in_maps, core_ids=list(range(NCORE)))
    out = np.zeros((1, SEQ, D), np.float32)
    for c in range(NCORE):
        o = np.asarray(res.results[c]["out"], np.float32)
        out[0, SEG * c:SEG * (c + 1)] = o[:SEG]
        out[0, SEG * (15 - c):SEG * (16 - c)] = o[SEG:]
    return out
```
